# Optimizing a Trainium2 kernel written in Bass

```python
import math
import jax, jax.numpy as jnp
from jax import lax
import numpy as np

D_MODEL = 1024
BATCH = 8
SEQ = 2048
DEPTH = 4

RET_HEADS = 4
RET_DK = 128
RET_DV = 256
RET_QK = RET_HEADS * RET_DK
RET_V = RET_HEADS * RET_DV
RET_CHUNK = 128
MOBA_HEADS = 8
MOBA_DH = 128
MOBA_W = MOBA_HEADS * MOBA_DH
MOBA_BLOCK = 256
MOBA_TOPK = 3
MOBA_QCHUNK = 16
IN_WIDTHS = (RET_QK, RET_QK, RET_V, RET_V, MOBA_W, MOBA_W, MOBA_W, MOBA_W, D_MODEL, D_MODEL)
D_IN = sum(IN_WIDTHS)
NORM_EPS = 1e-6

kernel_name = "hybrid_retention_moba_gated"


def rmsnorm(x, w):
    xf = x.astype(jnp.float32)
    xf = xf * lax.rsqrt(jnp.mean(xf * xf, axis=-1, keepdims=True) + NORM_EPS)
    return (xf * w.astype(jnp.float32)).astype(x.dtype)


def split_heads(t, n):
    b, s, _ = t.shape
    return t.reshape(b, s, n, -1).transpose(0, 2, 1, 3)


def merge_heads(t):
    b, n, s, d = t.shape
    return t.transpose(0, 2, 1, 3).reshape(b, s, n * d)


def retention(q, k, v):
    b, h, s, dk = q.shape
    dv = v.shape[-1]
    c = RET_CHUNK
    nc = s // c
    q = q.astype(jnp.float32)
    k = k.astype(jnp.float32) * (dk ** -0.5)
    v = v.astype(jnp.float32)
    log_g = jnp.log1p(-jnp.exp2(-5.0 - jnp.arange(h, dtype=jnp.float32)))
    n = jnp.arange(c, dtype=jnp.float32)
    rel = n[:, None] - n[None, :]
    decay_in = jnp.where(rel[None] >= 0, jnp.exp(jnp.maximum(rel, 0.0)[None] * log_g[:, None, None]), 0.0)
    xi = jnp.exp((n + 1.0)[None, :] * log_g[:, None])[None, :, :, None]
    zeta = jnp.exp((c - 1.0 - n)[None, :] * log_g[:, None])[None, :, :, None]
    g_chunk = jnp.exp(c * log_g)[None, :, None, None]

    def to_chunks(t):
        return jnp.moveaxis(t.reshape(b, h, nc, c, t.shape[-1]), 2, 0)

    def step(state, xs):
        qi, ki, vi = xs
        inner = jnp.einsum('bhnd,bhmd->bhnm', qi, ki) * decay_in
        o = jnp.einsum('bhnm,bhmv->bhnv', inner, vi) + jnp.einsum('bhnd,bhdv->bhnv', qi * xi, state)
        state = g_chunk * state + jnp.einsum('bhmd,bhmv->bhdv', ki * zeta, vi)
        return state, o

    state0 = jnp.zeros((b, h, dk, dv), jnp.float32)
    _, o = lax.scan(step, state0, (to_chunks(q), to_chunks(k), to_chunks(v)))
    return jnp.moveaxis(o, 0, 2).reshape(b, h, s, dv)


def head_groupnorm(o, w):
    mu = jnp.mean(o, axis=-1, keepdims=True)
    var = jnp.mean(jnp.square(o - mu), axis=-1, keepdims=True)
    o = (o - mu) * lax.rsqrt(var + NORM_EPS)
    return merge_heads(o) * w.astype(jnp.float32)


def moba_attention(q, k, v):
    b, h, s, dh = q.shape
    bs = MOBA_BLOCK
    nb = -(-s // bs)
    sp = nb * bs
    if sp > s:
        pad = ((0, 0), (0, 0), (0, sp - s), (0, 0))
        q, k, v = jnp.pad(q, pad), jnp.pad(k, pad), jnp.pad(v, pad)
    scale = dh ** -0.5
    topk = min(MOBA_TOPK, nb)
    slopes = jnp.exp2(-8.0 * (jnp.arange(h, dtype=jnp.float32) + 1.0) / h)
    kb = k.reshape(b, h, nb, bs, dh)
    vb = v.reshape(b, h, nb, bs, dh)
    kmean = jnp.mean(kb.astype(jnp.float32), axis=3)
    gate = jnp.einsum('bhtd,bhjd->bhtj', q.astype(jnp.float32), kmean)
    qblk = jnp.arange(sp) // bs
    past = jnp.arange(nb)[None, :] < qblk[:, None]
    gate = jnp.where(past[None, None], gate, -jnp.inf)
    _, idx = lax.top_k(gate, topk)
    valid = jnp.arange(topk)[None, :] < jnp.minimum(qblk, topk)[:, None]

    nq = sp // MOBA_QCHUNK
    qc_all = jnp.moveaxis(q.reshape(b, h, nq, MOBA_QCHUNK, dh), 2, 0)
    ic_all = jnp.moveaxis(idx.reshape(b, h, nq, MOBA_QCHUNK, topk), 2, 0)
    vc_all = valid.reshape(nq, MOBA_QCHUNK, topk)
    bi = jnp.arange(b)[:, None, None, None]
    hi = jnp.arange(h)[None, :, None, None]
    koff = jnp.arange(bs)

    def chunk(xs):
        qc, ic, vc, cid = xs
        tpos = cid * MOBA_QCHUNK + jnp.arange(MOBA_QCHUNK)
        blk = (cid * MOBA_QCHUNK) // bs
        ks = kb[bi, hi, ic]
        vs = vb[bi, hi, ic]
        s_sel = jnp.einsum('bhqd,bhqjkd->bhqjk', qc, ks).astype(jnp.float32) * scale
        dist_sel = (tpos[None, None, :, None, None] - (ic[..., None] * bs + koff)).astype(jnp.float32)
        s_sel = jnp.where(vc[None, None, :, :, None], s_sel - slopes[None, :, None, None, None] * dist_sel, -jnp.inf)
        s_sel = s_sel.reshape(b, h, MOBA_QCHUNK, topk * bs)
        k_own = lax.dynamic_index_in_dim(kb, blk, axis=2, keepdims=False)
        v_own = lax.dynamic_index_in_dim(vb, blk, axis=2, keepdims=False)
        s_own = jnp.einsum('bhqd,bhkd->bhqk', qc, k_own).astype(jnp.float32) * scale
        dist_own = tpos[:, None] - (blk * bs + koff)[None, :]
        s_own = jnp.where(dist_own[None, None] >= 0,
                          s_own - slopes[None, :, None, None] * dist_own.astype(jnp.float32)[None, None], -jnp.inf)
        p = jax.nn.softmax(jnp.concatenate([s_sel, s_own], axis=-1), axis=-1).astype(v.dtype)
        p_sel, p_own = p[..., :topk * bs], p[..., topk * bs:]
        o = jnp.einsum('bhqn,bhqnd->bhqd', p_sel, vs.reshape(b, h, MOBA_QCHUNK, topk * bs, dh))
        return o + jnp.einsum('bhqk,bhkd->bhqd', p_own, v_own)

    o = lax.map(chunk, (qc_all, ic_all, vc_all, jnp.arange(nq)))
    return jnp.moveaxis(o, 0, 2).reshape(b, h, sp, dh)[:, :, :s]


def hybrid_layer(x, ln_w, w_in, ret_gn_w, w_ret_o, w_moba_o, w_out):
    h = rmsnorm(x, ln_w)
    proj = jnp.einsum('bsd,de->bse', h, w_in)
    splits = [int(o) for o in np.cumsum(IN_WIDTHS)[:-1]]
    rq, rk, rv, rg, mq, mk, mv, mg, gr, gm = jnp.split(proj, splits, axis=-1)
    ret = retention(split_heads(rq, RET_HEADS), split_heads(rk, RET_HEADS), split_heads(rv, RET_HEADS))
    ret = head_groupnorm(ret, ret_gn_w).astype(x.dtype) * jax.nn.silu(rg)
    ret = jnp.einsum('bsv,vd->bsd', ret, w_ret_o)
    mo = moba_attention(split_heads(mq, MOBA_HEADS), split_heads(mk, MOBA_HEADS), split_heads(mv, MOBA_HEADS))
    mo = merge_heads(mo) * jax.nn.silu(mg)
    mo = jnp.einsum('bsv,vd->bsd', mo, w_moba_o)
    y = jax.nn.sigmoid(gr) * ret + jax.nn.sigmoid(gm) * mo
    return x + jnp.einsum('bsd,de->bse', y, w_out)


def setup_inputs(seed: int = 0) -> dict:
    key = jax.random.key(seed)
    ks = jax.random.split(key, 9)
    d = D_MODEL
    x = jax.random.normal(ks[0], (BATCH, SEQ, d), jnp.float32)
    ln_w = 1.0 + 0.02 * jax.random.normal(ks[1], (DEPTH, d), jnp.float32)
    w_in = jax.random.normal(ks[2], (DEPTH, d, D_IN), jnp.float32) * d ** -0.5
    ret_gn_w = 1.0 + 0.02 * jax.random.normal(ks[3], (DEPTH, RET_V), jnp.float32)
    w_ret_o = jax.random.normal(ks[4], (DEPTH, RET_V, d), jnp.float32) * RET_V ** -0.5
    w_moba_o = jax.random.normal(ks[5], (DEPTH, MOBA_W, d), jnp.float32) * MOBA_W ** -0.5
    w_out = jax.random.normal(ks[6], (DEPTH, d, d), jnp.float32) * d ** -0.5
    final_norm_w = 1.0 + 0.02 * jax.random.normal(ks[7], (d,), jnp.float32)
    return {"x": x, "ln_w": ln_w, "w_in": w_in, "ret_gn_w": ret_gn_w, "w_ret_o": w_ret_o,
            "w_moba_o": w_moba_o, "w_out": w_out, "final_norm_w": final_norm_w}


def reference(x, ln_w, w_in, ret_gn_w, w_ret_o, w_moba_o, w_out, final_norm_w):
    for layer in range(DEPTH):
        x = hybrid_layer(x, ln_w[layer], w_in[layer], ret_gn_w[layer], w_ret_o[layer],
                         w_moba_o[layer], w_out[layer])
    return rmsnorm(x, final_norm_w)
```

```python
from contextlib import ExitStack
import numpy as np
import concourse.bass as bass
import concourse.mybir as mybir
from concourse.bass_utils import run_bass_kernel_spmd

F32 = mybir.dt.float32
BF16 = mybir.dt.bfloat16
ALU = mybir.AluOpType
AF = mybir.ActivationFunctionType
AX = mybir.AxisListType

ENGS = ("pe", "act", "dve", "pool", "sp")


class _Op:
    __slots__ = ("eng", "fn", "deps", "mark", "rank", "kind", "stream", "ndma", "epoch", "idx")


class Prog:
    def __init__(self, nc):
        self.nc = nc
        self.ops = {e: [] for e in ENGS}
        self.lastw = {}
        self.readers = {}
        self.stream_cnt = {}
        self.epoch = 0
        self.final_waits = []

    def _deps(self, r, w):
        deps = set()
        for k in r:
            t = self.lastw.get(k)
            if t is not None:
                deps.add(t + ("raw",))
        for k in w:
            t = self.lastw.get(k)
            if t is not None:
                deps.add(t + ("waw",))
            for t in self.readers.get(k, ()):
                deps.add(t + ("war",))
        return deps

    def _commit(self, tok, r, w):
        for k in r:
            self.readers.setdefault(k, []).append(tok)
        for k in w:
            self.lastw[k] = tok
            self.readers[k] = []

    def retire(self, old_keys, new_keys):
        toks = []
        for k in old_keys:
            t = self.lastw.get(k)
            if t is not None:
                toks.append(t)
            toks.extend(self.readers.get(k, ()))
        toks = list(dict.fromkeys(toks))
        for k in new_keys:
            cur = list(self.readers.get(k, ()))
            t = self.lastw.get(k)
            if t is not None:
                cur.append(t)
            self.lastw[k] = None
            self.readers[k] = list(dict.fromkeys(cur + toks))

    def op(self, eng, fn, r=(), w=()):
        o = _Op()
        o.eng, o.fn, o.kind, o.mark, o.epoch = eng, fn, "c", False, self.epoch
        o.deps = self._deps(r, w)
        xk = [("Bx", k[1]) for k in list(r) + list(w) if isinstance(k, tuple) and k[0] == "B"] if eng != "pe" else []
        for k in xk:
            t = self.lastw.get(k)
            if t is not None:
                o.deps.add(t + ("war",))
        o.idx = len(self.ops[eng])
        self.ops[eng].append(o)
        tok = ("e", eng, o.idx)
        self._commit(tok, r, w)
        for k in xk:
            self.lastw[k] = tok
        return o

    def dma(self, q, fn, stream, ndma, r=(), w=()):
        o = _Op()
        o.eng, o.fn, o.kind, o.mark, o.epoch = q, fn, "d", False, self.epoch
        o.stream, o.ndma = stream, ndma
        o.deps = self._deps(r, w)
        o.idx = len(self.ops[q])
        self.ops[q].append(o)
        c = self.stream_cnt.get(stream, 0) + ndma
        self.stream_cnt[stream] = c
        self._commit(("d", stream, c), r, w)
        return o

    def emit(self):
        nc = self.nc
        ops = self.ops
        for e in ENGS:
            for o in ops[e]:
                nd = set()
                for d in o.deps:
                    if d[0] == "e":
                        pe_, idx, kind = d[1], d[2], d[3]
                        if pe_ == e and (e == "pe" or kind == "war"):
                            continue
                        ops[pe_][idx].mark = True
                        nd.add(("e", pe_, idx))
                    else:
                        nd.add(("d", d[1], d[2]))
                o.deps = nd
        nep = self.epoch + 1
        for e in ENGS:
            cnt = [0] * nep
            for o in ops[e]:
                if o.mark:
                    cnt[o.epoch] += 1
                    o.rank = cnt[o.epoch]
        with ExitStack() as st:
            esem = {}
            for e in ("pe", "act", "dve", "pool"):
                for ep in range(nep):
                    esem[(e, ep)] = st.enter_context(nc.semaphore(f"s_{e}_{ep}"))
            ssem = {s: st.enter_context(nc.semaphore(f"d_{s}")) for s in self.stream_cnt}
            block = st.enter_context(nc.Block())

            def run(e, eng):
                waited = {}
                for o in ops[e]:
                    need = {}
                    for d in o.deps:
                        if d[0] == "e":
                            po = ops[d[1]][d[2]]
                            key = ("e", d[1], po.epoch)
                            val = po.rank
                        else:
                            key = ("d", d[1])
                            val = 16 * d[2]
                        if need.get(key, 0) < val:
                            need[key] = val
                    for key, val in need.items():
                        if waited.get(key, 0) >= val:
                            continue
                        waited[key] = val
                        sem = esem[(key[1], key[2])] if key[0] == "e" else ssem[key[1]]
                        eng.wait_ge(sem, val)
                    if o.kind == "c":
                        ins = o.fn(eng)
                        if o.mark:
                            ins.then_inc(esem[(e, o.epoch)], 1)
                    else:
                        lst = o.fn(eng)
                        assert len(lst) == o.ndma, (len(lst), o.ndma)
                        for ins in lst:
                            ins.then_inc(ssem[o.stream], 16)
                for (q, stream) in self.final_waits:
                    if q == e:
                        eng.wait_ge(ssem[stream], 16 * self.stream_cnt[stream])

            @block.tensor
            def _(eng):
                run("pe", eng)

            @block.scalar
            def _(eng):
                run("act", eng)

            @block.vector
            def _(eng):
                run("dve", eng)

            @block.gpsimd
            def _(eng):
                run("pool", eng)

            @block.sync
            def _(eng):
                run("sp", eng)


def sb_ap(t, col, dims, p0=0, npart=128):
    F = 1
    for s in t.shape[1:]:
        F *= s
    return bass.AP(t, p0 * F + col, [[F, npart]] + [list(d) for d in dims])


def K(name, lo, hi):
    return [(name, i) for i in range(lo, hi)]


S = 2048
D = 1024
NT = 16
EPS = 1e-6
RET_H, MOBA_H = 4, 8
C_RQ, C_RK, C_RV, C_RG = 0, 512, 1024, 2048
C_MQ, C_MK, C_MV, C_MG = 3072, 4096, 5120, 6144
C_GR, C_GM = 7168, 8192
DIN = 9216
NEG = -30000.0
GAMMA = [1.0 - 2.0 ** (-5.0 - h) for h in range(RET_H)]
SLOPE = [2.0 ** (-8.0 * (h + 1.0) / MOBA_H) for h in range(MOBA_H)]
SCALE = 128.0 ** -0.5


def _bias_plan():
    table = {}
    plan = {}
    for h in range(MOBA_H):
        for T in range(4):
            for st in range(4 * (T + 1)):
                i = st - 4 * T
                c0 = 128 * i if i >= 0 else 0
                segs = []
                if h == 0:
                    rngs = [(max(c0, 0), 256, 512 * T + 128), (max(c0, 256), 512, 512 * T + 384)]
                else:
                    rngs = [(c0, 512, 512 * T + 256)]
                for lo, hi, ref in rngs:
                    if lo >= hi:
                        continue
                    key = (h, 128 * st - ref)
                    if key not in table:
                        table[key] = len(table)
                    segs.append((lo, hi, table[key]))
                plan[(h, T, st)] = segs
    return plan, table


BIAS_PLAN, BIAS_TABLE = _bias_plan()
NBIAS = len(BIAS_TABLE)

CF_ZCOL = 0
CF_ABIAS = CF_ZCOL + 4
CF_NEGM = CF_ABIAS + NBIAS
CF_PW = CF_NEGM + 64
CF_LNW = CF_PW + 16
CF_GNW = CF_LNW + 32
NCF = CF_GNW + 32
CB_TRI01 = 0
CB_NEGTRI = 128
CB_IDENT = 256
CB_ONES2 = 384
CB_NEGSEL = 512
NCB = CB_NEGSEL + 7 * 128


def host_consts(ln_w, ret_gn_w, n_layers, layer0):
    p = np.arange(128, dtype=np.float64)
    cf = np.zeros((128, NCF), np.float64)
    for h in range(RET_H):
        cf[:, CF_ZCOL + h] = GAMMA[h] ** (127.0 - p) * 128.0 ** -0.5
    for (h, delta), idx in BIAS_TABLE.items():
        cf[:, CF_ABIAS + idx] = SLOPE[h] * (delta + p)
    for i in range(8):
        qb = (8 + i) // 2
        for j in range(8):
            cf[:, CF_NEGM + i * 8 + j] = 0.0 if j < qb else -1e30
    cf[:, CF_PW:CF_PW + 16] = -0.5
    cf = cf.astype(np.float32)
    for l in range(n_layers):
        cf[:, CF_LNW + l * 8:CF_LNW + l * 8 + 8] = ln_w[layer0 + l].reshape(8, 128).T
        cf[:, CF_GNW + l * 8:CF_GNW + l * 8 + 8] = ret_gn_w[layer0 + l].reshape(8, 128).T
    cb = np.zeros((128, NCB), np.float32)
    m = np.arange(128)[:, None]
    n = np.arange(128)[None, :]
    cb[:, CB_TRI01:CB_TRI01 + 128] = (m <= n)
    cb[:, CB_NEGTRI:CB_NEGTRI + 128] = np.where(m > n, NEG, 0.0)
    cb[:, CB_IDENT:CB_IDENT + 128] = (m == n)
    cb[:, CB_ONES2:CB_ONES2 + 128] = 2.0
    for j in range(7):
        cb[j, CB_NEGSEL + j * 128:CB_NEGSEL + (j + 1) * 128] = NEG
    nn = np.arange(128, dtype=np.float64)
    qk = np.zeros((RET_H, 128, 256), np.float32)
    for h in range(RET_H):
        qk[h, :, 0:128] = (GAMMA[h] ** (nn + 1.0))[None, :]
        qk[h, :, 128:256] = (GAMMA[h] ** (127.0 - nn) * 128.0 ** -0.5)[None, :]
    return cf, cb, qk


def build(n_layers=4, final_norm=True):
    nc = bass.Bass("TRN2", target_bir_lowering=False)
    x_d = nc.dram_tensor("x", [S, D], F32, kind="ExternalInput")
    win_d = nc.dram_tensor("w_in", [n_layers, D, DIN], F32, kind="ExternalInput")
    wro_d = nc.dram_tensor("w_ro", [n_layers, D, D], F32, kind="ExternalInput")
    wmo_d = nc.dram_tensor("w_mo", [n_layers, D, D], F32, kind="ExternalInput")
    wout_d = nc.dram_tensor("w_out", [n_layers, D, D], F32, kind="ExternalInput")
    cf_d = nc.dram_tensor("cf", [128, NCF], F32, kind="ExternalInput")
    cb_d = nc.dram_tensor("cb", [128, NCB], F32, kind="ExternalInput")
    qk_d = nc.dram_tensor("qksc", [RET_H, 128, 256], F32, kind="ExternalInput")
    fnw_d = nc.dram_tensor("fnw", [128, D], F32, kind="ExternalInput")
    out_d = nc.dram_tensor("out", [S, D], F32, kind="ExternalOutput")

    with ExitStack() as st:
        def sb(name, shape, dt):
            return st.enter_context(nc.sbuf_tensor(name, shape, dt))

        xres = sb("xres", [128, NT * D], F32)
        hT = sb("hT", [128, 8 * S], BF16)
        GT = sb("GT", [128, 8 * S], BF16)
        yT = sb("yT", [128, 8 * S], BF16)
        wsl = sb("wsl", [128, 4 * 2048], BF16)
        qT = sb("qT", [128, S], BF16)
        kT = sb("kT", [128, S], BF16)
        vtok = sb("vtok", [128, NT * 128], BF16)
        PT = sb("PT", [128, 4 * 512], BF16)
        cf = sb("cf_sb", [128, NCF], F32)
        cb = sb("cb_sb", [128, NCB], BF16)
        qksc = sb("qksc_sb", [128, 256], F32)
        st12 = sb("st12", [128, NT * 12], F32)
        mv = sb("mv", [128, NT * 2], F32)
        ms16 = sb("ms16", [128, 16], F32)
        rstd16 = sb("rstd16", [128, 16], F32)
        gsm = sb("gsm", [128, 48], F32)
        gm = sb("gm", [128, 64], F32)
        ocp = sb("ocp", [128, 3 * 256], BF16)
        m8 = sb("m8", [128, 64], F32)
        nmb = sb("nmb", [128, 64], BF16)
        nmT = sb("nmT", [128, 1024], BF16)
        ksum = sb("ksum", [128, 8], F32)
        kmh = sb("kmh", [128, 8], BF16)
        kml = sb("kml", [128, 8], BF16)
        tmg = sb("tmg", [128, 512], BF16)
        um = sb("um", [128, 512], BF16)
        rl = sb("rl", [128, 512], F32)
        B = [st.enter_context(nc.psum_tensor(f"B{i}", [128, 512], F32)) for i in range(8)]

        Bb = [b[:].bitcast(BF16) for b in B]
        PTf = PT[:].bitcast(F32)
        VTf = vtok[:].bitcast(F32)
        hTf = hT[:].bitcast(F32)
        GTf = GT[:].bitcast(F32)

        ident = cb[:, CB_IDENT:CB_IDENT + 128]
        P = Prog(nc)
        rot = [0]

        def nextA():
            i = rot[0] % 3
            rot[0] += 1
            return i

        def wrows(dten, L):
            return dten.ap()[L].rearrange("(c p) n -> p c n", p=128)

        def load_w(pieces, skeys, stream):
            def f(e):
                r = []
                for (dten, L, c0, n, slot_col, width) in pieces:
                    src = wrows(dten, L)
                    for half in range(2):
                        r.append(e.dma_start(out=sb_ap(wsl, slot_col + half * 4 * width, [[width, 4], [1, n]]),
                                             in_=src[:, half * 4:(half + 1) * 4, c0:c0 + n]))
                return r
            P.dma("pool", f, stream, 2 * len(pieces), w=skeys)

        P.dma("sp", lambda e: [e.dma_start(out=cf[:], in_=cf_d.ap())], "cf", 1, w=["cf"])
        P.dma("sp", lambda e: [e.dma_start(out=GTf[:, 0:NCB], in_=cb_d.ap())], "cbs", 1, w=K("GT", 0, 16))
        P.op("dve", lambda e: e.tensor_copy(cb[:], GTf[:, 0:NCB]), r=K("GT", 0, 16), w=["cb"])
        P.op("pool", lambda e: e.memset(nmT[:], 0.0), w=["nmT"])
        for g in range(4):
            def f(e, g=g):
                return [e.dma_start(out=xres[:, (4 * g + i) * D:(4 * g + i + 1) * D],
                                    in_=x_d.ap()[(4 * g + i) * 128:(4 * g + i + 1) * 128, :]) for i in range(4)]
            P.dma("sp", f, f"xin{g}", 4, w=K("xres", 4 * g, 4 * g + 4))

        def rms_tile(tt):
            for hf in range(2):
                P.op("dve", lambda e, hf=hf: e.bn_stats(
                    st12[:, tt * 12 + hf * 6:tt * 12 + hf * 6 + 6],
                    xres[:, tt * D + hf * 512:tt * D + hf * 512 + 512]), r=[("xres", tt)], w=[("st12", tt)])
            P.op("dve", lambda e: e.bn_aggr(mv[:, tt * 2:tt * 2 + 2], st12[:, tt * 12:tt * 12 + 12]),
                 r=[("st12", tt)], w=[("mv", tt)])
            P.op("dve", lambda e: e.tensor_tensor(ms16[:, tt:tt + 1], mv[:, tt * 2:tt * 2 + 1], mv[:, tt * 2:tt * 2 + 1], ALU.mult),
                 r=[("mv", tt)], w=[("ms16", tt)])
            P.op("dve", lambda e: e.scalar_tensor_tensor(ms16[:, tt:tt + 1], ms16[:, tt:tt + 1], EPS, mv[:, tt * 2 + 1:tt * 2 + 2], ALU.add, ALU.add),
                 r=[("mv", tt), ("ms16", tt)], w=[("ms16", tt)])
            P.op("pool", lambda e: e.tensor_tensor(rstd16[:, tt:tt + 1], ms16[:, tt:tt + 1], cf[:, CF_PW:CF_PW + 1], ALU.pow),
                 r=[("ms16", tt), "cf"], w=[("rstd16", tt)])

        def norm_front(L, tt):
            rms_tile(tt)
            b = tt % 2
            hb = PT[:, b * 1024:(b + 1) * 1024]
            P.op("act", lambda e: e.activation(hb, xres[:, tt * D:(tt + 1) * D], AF.Copy, scale=rstd16[:, tt:tt + 1]),
                 r=[("xres", tt), ("rstd16", tt)], w=[("hb", b)])

        def norm_back(L, tt):
            b = tt % 2
            hb = PT[:, b * 1024:(b + 1) * 1024]
            bk = 6 + b

            def tr(e):
                for c in range(8):
                    ins = e.transpose(Bb[bk][:, c * 128:(c + 1) * 128], hb[:, c * 128:(c + 1) * 128], ident)
                return ins
            P.op("pe", tr, r=[("hb", b), "cb"], w=[("B", bk)])
            P.op("dve", lambda e: e.tensor_tensor(
                sb_ap(hT, tt * 128, [[S, 8], [1, 128]]),
                Bb[bk].rearrange("p (c n) -> p c n", c=8),
                sb_ap(cf, CF_LNW + L * 8, [[1, 8], [0, 128]]), ALU.mult),
                r=[("B", bk), "cf"], w=[("hT", tt)])

        def phaseN(L):
            P.retire(K("PTm", 0, 4) + ["tg0", "tg1"], K("hb", 0, 2))
            for tt in range(NT + 1):
                if tt < NT:
                    norm_front(L, tt)
                if tt >= 1:
                    norm_back(L, tt - 1)

        def loadRA(L, h):
            load_w([(win_d, L, C_RK + 128 * h, 128, 0, 512), (win_d, L, C_RV + 256 * h, 256, 128, 512),
                    (win_d, L, C_RQ + 128 * h, 128, 384, 512)], K("wsl", 0, 2), "wA")
            P.dma("sp", lambda e: [e.dma_start(out=qksc[:], in_=qk_d.ap()[h])], "qksc", 1, w=["qksc"])

        def loadRB(L, h):
            sB = 2 + h % 2
            load_w([(win_d, L, C_RG + 256 * h, 256, sB * 2048, 256)], [("wsl", sB)], f"wB{sB}")

        def loadR(L, h):
            loadRA(L, h)
            loadRB(L, h)

        def vT_ap(j, lo, n):
            return yT[:, j * S + lo:j * S + lo + n]

        def uT_ap(j, lo, n):
            return yT[:, 4096 + j * S + lo:4096 + j * S + lo + n]

        def kvt_ap(c, lo, n):
            return yT[:, 8192 + c * 384 + lo:8192 + c * 384 + lo + n]

        def rtg_ap(i):
            return yT[:, 14336 + i * 512:14336 + (i + 1) * 512]

        def insb_ap(i):
            return yT[:, 15360 + i * 128:15360 + (i + 1) * 128]

        def on_ap(i):
            return yT[:, 15616 + i * 256:15616 + (i + 1) * 256]

        def sbf_ap(c):
            return vtok[:, c * 256:(c + 1) * 256] if c < 8 else PT[:, (c - 8) * 256:(c - 7) * 256]
        R_S = rl[:, 0:256]
        RKEYS_Y = (K("vT", 0, 16) + K("uT", 0, 16) + K("kvt", 0, 16) + ["rtg0", "rtg1"] + K("insb", 0, 2) + K("on", 0, 3))

        def phaseRall(L):
            P.retire(K("hb", 0, 2) + K("PTm", 0, 4) + ["tg0", "tg1"], K("sbf", 8, 15))
            P.retire(K("vtokm", 0, 16), K("sbf", 0, 8))
            P.retire(K("yT", 0, 16), RKEYS_Y)
            P.retire(["rl"], K("Sst", 0, 2))
            pj = [0]
            rcnt = [0]

            def proj_tile(h, T, kind):
                sB = 2 + h % 2
                gnw0 = CF_GNW + L * 8 + 2 * h
                if kind == "q":
                    wfn, wkeys = (lambda cc: cc * 512 + 384), K("wsl", 0, 2)
                elif kind == "k":
                    wfn, wkeys = (lambda cc: cc * 512), K("wsl", 0, 2)
                elif kind in ("v0", "v1"):
                    j = int(kind[1])
                    wfn, wkeys = (lambda cc: cc * 512 + 128 + j * 128), K("wsl", 0, 2)
                else:
                    j = int(kind[1])
                    wfn, wkeys = (lambda cc: sB * 2048 + cc * 256 + j * 128), [("wsl", sB)]
                a = pj[0] % 3
                pj[0] += 1

                def mm(e):
                    for cc in range(8):
                        w0 = wfn(cc)
                        ins = e.matmul(B[a][:, 0:512], wsl[:, w0:w0 + 128],
                                       hT[:, cc * S + T * 512:cc * S + T * 512 + 512], start=(cc == 0), stop=(cc == 7))
                    return ins
                P.op("pe", mm, r=wkeys + K("hT", 4 * T, 4 * T + 4), w=[("B", a)])
                if kind in ("q", "k"):
                    which = 0 if kind == "q" else 1
                    dst = qT if which == 0 else kT
                    dkey = "qT" if which == 0 else "kT"
                    P.op("dve", lambda e: e.tensor_tensor(
                        sb_ap(dst, T * 512, [[128, 4], [1, 128]]),
                        B[a][:, 0:512].rearrange("p (c n) -> p c n", c=4),
                        sb_ap(qksc, which * 128, [[0, 4], [1, 128]]), ALU.mult),
                        r=[("B", a), "qksc"], w=K(dkey, 4 * T, 4 * T + 4))
                elif kind in ("v0", "v1"):
                    P.op("act", lambda e: e.activation(vT_ap(j, T * 512, 512), B[a][:, 0:512], AF.Copy),
                         r=[("B", a)], w=K("vT", 4 * T, 4 * T + 4))
                else:
                    ri = rcnt[0] % 2
                    rcnt[0] += 1
                    P.op("act", lambda e: e.activation(rtg_ap(ri), B[a][:, 0:512], AF.Tanh, scale=0.5),
                         r=[("B", a)], w=[f"rtg{ri}"])
                    P.op("dve", lambda e: e.scalar_tensor_tensor(uT_ap(j, T * 512, 512), rtg_ap(ri), 1.0, B[a][:, 0:512], ALU.add, ALU.mult),
                         r=[("B", a), f"rtg{ri}"], w=K("uT", 4 * T, 4 * T + 4))
                    P.op("act", lambda e: e.activation(uT_ap(j, T * 512, 512), uT_ap(j, T * 512, 512), AF.Copy,
                                                        scale=cf[:, gnw0 + j:gnw0 + j + 1]),
                         r=K("uT", 4 * T, 4 * T + 4) + ["cf"], w=K("uT", 4 * T, 4 * T + 4))

            def st1(h, pr):
                def trkv(e):
                    for q in range(2):
                        c = 2 * pr + q
                        e.transpose(Bb[3][:, q * 384:q * 384 + 128], kT[:, c * 128:(c + 1) * 128], ident)
                        e.transpose(Bb[3][:, q * 384 + 128:q * 384 + 256], vT_ap(0, c * 128, 128), ident)
                        ins = e.transpose(Bb[3][:, q * 384 + 256:q * 384 + 384], vT_ap(1, c * 128, 128), ident)
                    return ins
                P.op("pe", trkv, r=K("kT", 2 * pr, 2 * pr + 2) + K("vT", 2 * pr, 2 * pr + 2) + ["cb"], w=[("B", 3)])
                P.op("act", lambda e: e.activation(kvt_ap(2 * pr, 0, 768), Bb[3][:, 0:768], AF.Copy),
                     r=[("B", 3)], w=K("kvt", 2 * pr, 2 * pr + 2))

            def st2A1(h2, c2, hA, cA):
                do2 = c2 is not None and c2 < NT - 1
                doA = cA is not None
                if not (do2 or doA):
                    return
                r = []
                if do2:
                    r += [("kvt", c2)]
                if doA:
                    r += [("kT", cA), ("qT", cA)]

                def mm(e):
                    ins = None
                    if do2:
                        ins = e.matmul(B[4][:, 0:256], kvt_ap(c2, 0, 128), kvt_ap(c2, 128, 256), start=True, stop=True)
                    if doA:
                        tok = slice(cA * 128, (cA + 1) * 128)
                        ins = e.matmul(B[4][:, 256:384], kT[:, tok], qT[:, tok], start=True, stop=True)
                    return ins
                P.op("pe", mm, r=r, w=[("B", 4)])
                if do2:
                    g2 = GAMMA[h2]
                    Sc = rl[:, (c2 % 2) * 256:(c2 % 2) * 256 + 256]
                    Sp = rl[:, ((c2 + 1) % 2) * 256:((c2 + 1) % 2) * 256 + 256]
                    if c2 == 0:
                        P.op("dve", lambda e: e.tensor_copy(Sc, B[4][:, 0:256]), r=[("B", 4)], w=[("Sst", c2 % 2)])
                    else:
                        P.op("dve", lambda e: e.scalar_tensor_tensor(Sc, Sp, float(g2 ** 128.0), B[4][:, 0:256], ALU.mult, ALU.add),
                             r=[("B", 4), ("Sst", (c2 + 1) % 2)], w=[("Sst", c2 % 2)])
                if doA:
                    gA = GAMMA[hA]
                    i2 = cA % 2
                    P.op("dve", lambda e: e.scalar_tensor_tensor(insb_ap(i2), B[4][:, 256:384], float(gA ** -128.0), cb[:, CB_TRI01:CB_TRI01 + 128], ALU.mult, ALU.mult),
                         r=[("B", 4), "cb"], w=[("insb", i2)])
                if do2:
                    P.op("dve", lambda e: e.tensor_copy(sbf_ap(c2), Sc), r=[("Sst", c2 % 2)], w=[("sbf", c2)])

            def stA2(h, c):
                tok = slice(c * 128, (c + 1) * 128)
                i2 = c % 2
                ob = 5 + c % 2

                def mmo(e):
                    if c > 0:
                        e.matmul(B[ob][:, 0:256], qT[:, tok], sbf_ap(c - 1), start=True, stop=False)
                    return e.matmul(B[ob][:, 0:256], insb_ap(i2), kvt_ap(c, 128, 256), start=(c == 0), stop=True)
                P.op("pe", mmo, r=[("qT", c), ("insb", i2), ("kvt", c)] + ([("sbf", c - 1)] if c > 0 else []), w=[("B", ob)])

            def stB1(h, c):
                ob = 5 + c % 2
                g3 = c % 3
                g0 = g3 * 16
                P.op("dve", lambda e: e.bn_stats(gsm[:, g0:g0 + 6], B[ob][:, 0:256]), r=[("B", ob)], w=[("gsm", g3)])
                P.op("dve", lambda e: e.bn_aggr(gsm[:, g0 + 6:g0 + 8], gsm[:, g0:g0 + 6]), r=[("gsm", g3)], w=[("gsm", g3)])
                P.op("act", lambda e: e.activation(ocp[:, g3 * 256:(g3 + 1) * 256], B[ob][:, 0:256], AF.Copy),
                     r=[("B", ob)], w=[("ocp", g3)])
                P.op("pool", lambda e: e.tensor_scalar(gsm[:, g0 + 8:g0 + 9], gsm[:, g0 + 7:g0 + 8], EPS, 4.0, ALU.add, ALU.mult),
                     r=[("gsm", g3)], w=[("gsm", g3)])
                P.op("pool", lambda e: e.tensor_tensor(gsm[:, g0 + 9:g0 + 10], gsm[:, g0 + 8:g0 + 9], cf[:, CF_PW:CF_PW + 1], ALU.pow),
                     r=[("gsm", g3), "cf"], w=[("gsm", g3)])
                P.op("pool", lambda e: e.tensor_scalar(gsm[:, g0 + 10:g0 + 11], gsm[:, g0 + 6:g0 + 7], -1.0, gsm[:, g0 + 9:g0 + 10], ALU.mult, ALU.mult),
                     r=[("gsm", g3)], w=[("gsm", g3)])

            def stB2a(h, c):
                ob = 5 + c % 2
                g3 = c % 3
                g0 = g3 * 16
                P.op("act", lambda e: e.activation(on_ap(g3), ocp[:, g3 * 256:(g3 + 1) * 256], AF.Identity,
                                                    bias=gsm[:, g0 + 10:g0 + 11], scale=gsm[:, g0 + 9:g0 + 10]),
                     r=[("ocp", g3), ("gsm", g3)], w=[("on", g3)])

            def stB2b(h, c):
                g3 = c % 3

                def trr(e):
                    e.transpose(Bb[7][:, 0:128], on_ap(g3)[:, 0:128], ident)
                    return e.transpose(Bb[7][:, 128:256], on_ap(g3)[:, 128:256], ident)
                P.op("pe", trr, r=[("on", g3), "cb"], w=[("B", 7)])

            def stB3(h, c):
                P.op("dve", lambda e: e.tensor_tensor(
                    sb_ap(GT, 2 * h * S + c * 128, [[S, 2], [1, 128]]),
                    Bb[7][:, 0:256].rearrange("p (j n) -> p j n", j=2),
                    sb_ap(yT, 4096 + c * 128, [[S, 2], [1, 128]]), ALU.mult),
                    r=[("B", 7), ("uT", c)], w=[("GT", c)])

            NG = RET_H * NT
            PROJ_ORDER = [("q", "k"), ("v0", "v1"), ("r0",), ("r1",)]
            stages = [(9, stA2), (10, stB1), (11, stB2a), (12, stB2b), (13, stB3)]
            for gstep in range(NG + 14):
                for (dly, fn) in reversed(stages):
                    gc = gstep - dly
                    if 0 <= gc < NG:
                        fn(gc // NT, gc % NT)
                g2, gA = gstep - 6, gstep - 8
                st2A1(g2 // NT if 0 <= g2 < NG else None, g2 % NT if 0 <= g2 < NG else None,
                      gA // NT if 0 <= gA < NG else None, gA % NT if 0 <= gA < NG else None)
                if gstep % 2 == 0:
                    gc = gstep - 4
                    if 0 <= gc < NG:
                        st1(gc // NT, (gc % NT) // 2)
                if gstep < NG:
                    h, cc_ = gstep // NT, gstep % NT
                    for kind in PROJ_ORDER[cc_ % 4]:
                        proj_tile(h, cc_ // 4, kind)
                    if cc_ == 0 and h + 1 < RET_H:
                        loadRB(L, h + 1)
                    if cc_ == 13 and h + 1 < RET_H:
                        loadRA(L, h + 1)

        def loadXO(L, eb, wd, gcol):
            p = eb % 2
            load_w([(wd, L, eb * 256, 256, (2 * p) * 2048, 256)], [("wsl", 2 * p)], f"wX{2 * p}")
            load_w([(win_d, L, gcol + eb * 256, 256, (2 * p + 1) * 2048, 256)], [("wsl", 2 * p + 1)], f"wX{2 * p + 1}")

        def phaseXO(L, eb, first):
            p = eb % 2
            if eb == 0:
                if first:
                    P.retire(K("sbf", 8, 15), ["tg0", "tg1"])
                    P.retire(RKEYS_Y, K("yT", 0, 16))
                else:
                    P.retire(K("PTm", 0, 4), ["tg0", "tg1"])
            for ec in range(2):
                echunk = 2 * eb + ec
                for T in range(4):
                    aa = nextA()

                    def mma(e, aa=aa, T=T, ec=ec):
                        for vc_ in range(8):
                            ins = e.matmul(B[aa][:, 0:512], wsl[:, (2 * p) * 2048 + vc_ * 256 + ec * 128:(2 * p) * 2048 + vc_ * 256 + ec * 128 + 128],
                                           GT[:, vc_ * S + T * 512:vc_ * S + T * 512 + 512], start=(vc_ == 0), stop=(vc_ == 7))
                        return ins
                    P.op("pe", mma, r=[("wsl", 2 * p)] + K("GT", 4 * T, 4 * T + 4), w=[("B", aa)])
                    ag = nextA()

                    def mmg(e, ag=ag, T=T, ec=ec):
                        for cc in range(8):
                            ins = e.matmul(B[ag][:, 0:512], wsl[:, (2 * p + 1) * 2048 + cc * 256 + ec * 128:(2 * p + 1) * 2048 + cc * 256 + ec * 128 + 128],
                                           hT[:, cc * S + T * 512:cc * S + T * 512 + 512], start=(cc == 0), stop=(cc == 7))
                        return ins
                    P.op("pe", mmg, r=[("wsl", 2 * p + 1)] + K("hT", 4 * T, 4 * T + 4), w=[("B", ag)])
                    tb = (ec * 4 + T) % 2
                    tg = PT[:, tb * 512:(tb + 1) * 512]
                    tkey = f"tg{tb}"
                    P.op("act", lambda e, ag=ag, tg=tg: e.activation(tg, B[ag][:, 0:512], AF.Tanh, scale=0.5),
                         r=[("B", ag)], w=[tkey])
                    ydst = yT[:, echunk * S + T * 512:echunk * S + T * 512 + 512]
                    if first:
                        P.op("dve", lambda e, aa=aa, tg=tg, ydst=ydst: e.scalar_tensor_tensor(ydst, tg, 1.0, B[aa][:, 0:512], ALU.add, ALU.mult),
                             r=[("B", aa), tkey], w=K("yT", 4 * T, 4 * T + 4))
                    else:
                        P.op("dve", lambda e, aa=aa, tg=tg: e.scalar_tensor_tensor(rl[:], tg, 1.0, B[aa][:, 0:512], ALU.add, ALU.mult),
                             r=[("B", aa), tkey], w=["rl"])
                        P.op("dve", lambda e, ydst=ydst: e.tensor_tensor(ydst, rl[:], ydst, ALU.add),
                             r=["rl"] + K("yT", 4 * T, 4 * T + 4), w=K("yT", 4 * T, 4 * T + 4))

        def loadM(L, h):
            p = h % 2
            load_w([(win_d, L, C_MQ + 128 * h, 128, (2 * p) * 2048, 256), (win_d, L, C_MK + 128 * h, 128, (2 * p) * 2048 + 128, 256)],
                   [("wsl", 2 * p)], f"wX{2 * p}")
            load_w([(win_d, L, C_MV + 128 * h, 128, (2 * p + 1) * 2048, 256), (win_d, L, C_MG + 128 * h, 128, (2 * p + 1) * 2048 + 128, 256)],
                   [("wsl", 2 * p + 1)], f"wX{2 * p + 1}")

        def phaseM(L, h):
            p = h % 2
            sQK = (2 * p) * 2048
            sVG = (2 * p + 1) * 2048
            if h == 0:
                P.retire(["tg0", "tg1"], K("PTm", 0, 4))
                P.retire(K("sbf", 0, 8), K("vtokm", 0, 16))
                P.retire(K("Sst", 0, 2), ["rl"])
            for T in range(4):
                a = nextA()

                def mmq(e, a=a, T=T):
                    for cc in range(8):
                        ins = e.matmul(B[a][:, 0:512], wsl[:, sQK + cc * 256:sQK + cc * 256 + 128],
                                       hT[:, cc * S + T * 512:cc * S + T * 512 + 512], start=(cc == 0), stop=(cc == 7))
                    return ins
                P.op("pe", mmq, r=[("wsl", 2 * p)] + K("hT", 4 * T, 4 * T + 4), w=[("B", a)])
                P.op("act", lambda e, a=a, T=T: e.activation(qT[:, T * 512:(T + 1) * 512], B[a][:, 0:512], AF.Copy),
                     r=[("B", a)], w=K("qT", 4 * T, 4 * T + 4))
                a2 = nextA()

                def mmk(e, a2=a2, T=T):
                    for cc in range(8):
                        ins = e.matmul(B[a2][:, 0:512], wsl[:, sQK + cc * 256 + 128:sQK + cc * 256 + 256],
                                       hT[:, cc * S + T * 512:cc * S + T * 512 + 512], start=(cc == 0), stop=(cc == 7))
                    return ins
                P.op("pe", mmk, r=[("wsl", 2 * p)] + K("hT", 4 * T, 4 * T + 4), w=[("B", a2)])
                P.op("act", lambda e, a2=a2, T=T: e.activation(kT[:, T * 512:(T + 1) * 512], B[a2][:, 0:512], AF.Copy),
                     r=[("B", a2)], w=K("kT", 4 * T, 4 * T + 4))
                P.op("dve", lambda e, a2=a2, T=T: e.tensor_reduce(ksum[:, 2 * T:2 * T + 2], B[a2][:, 0:512].rearrange("p (b n) -> p b n", b=2), AX.X, ALU.add),
                     r=[("B", a2)], w=["ksum"])
            for T in range(4):
                a = nextA()

                def mmv(e, a=a, T=T):
                    for cc in range(8):
                        ins = e.matmul(B[a][:, 0:512], wsl[:, sVG + cc * 256:sVG + cc * 256 + 128],
                                       hT[:, cc * S + T * 512:cc * S + T * 512 + 512], start=(cc == 0), stop=(cc == 7))
                    return ins
                P.op("pe", mmv, r=[("wsl", 2 * p + 1)] + K("hT", 4 * T, 4 * T + 4), w=[("B", a)])
                P.op("act", lambda e, a=a: e.activation(tmg[:], B[a][:, 0:512], AF.Copy), r=[("B", a)], w=["tmg"])
                bk = 6 if T % 2 == 0 else 7

                def trv(e, bk=bk):
                    for i in range(4):
                        ins = e.transpose(Bb[bk][:, i * 128:(i + 1) * 128], tmg[:, i * 128:(i + 1) * 128], ident)
                    return ins
                P.op("pe", trv, r=["tmg", "cb"], w=[("B", bk)])
                P.op("dve", lambda e, bk=bk, T=T: e.tensor_copy(vtok[:, T * 512:(T + 1) * 512], Bb[bk][:, 0:512]),
                     r=[("B", bk)], w=K("vtokm", 4 * T, 4 * T + 4))
            def gate1():
                P.op("dve", lambda e: e.tensor_copy(kmh[:], ksum[:]), r=["ksum"], w=["kmh"])
                P.op("dve", lambda e: e.tensor_tensor(kml[:], ksum[:], kmh[:], ALU.subtract), r=["ksum", "kmh"], w=["kml"])

                def mmgate(e):
                    for i in range(8):
                        tt = 8 + i
                        e.matmul(B[7][:, i * 8:(i + 1) * 8], qT[:, tt * 128:(tt + 1) * 128], kmh[:], start=True, stop=False)
                        ins = e.matmul(B[7][:, i * 8:(i + 1) * 8], qT[:, tt * 128:(tt + 1) * 128], kml[:], start=False, stop=True)
                    return ins
                P.op("pe", mmgate, r=K("qT", 8, 16) + ["kmh", "kml"], w=[("B", 7)])
                P.op("dve", lambda e: e.tensor_tensor(gm[:], B[7][:, 0:64], cf[:, CF_NEGM:CF_NEGM + 64], ALU.add),
                     r=[("B", 7), "cf"], w=["gm"])
                for i in range(8):
                    P.op("dve", lambda e, i=i: e.max(m8[:, i * 8:(i + 1) * 8], gm[:, i * 8:(i + 1) * 8]), r=["gm"], w=["m8"])
                P.op("dve", lambda e: e.tensor_tensor(sb_ap(nmb, 0, [[8, 8], [1, 8]]), sb_ap(gm, 0, [[8, 8], [1, 8]]),
                                                       sb_ap(m8, 2, [[8, 8], [0, 8]]), ALU.is_lt),
                     r=["gm", "m8"], w=["nmb"])


            def gate2():
                def trm(e):
                    for i in range(8):
                        ins = e.transpose(Bb[7][0:8, i * 128:(i + 1) * 128], nmb[:, i * 8:(i + 1) * 8], ident)
                    return ins
                P.op("pe", trm, r=["nmb", "cb"], w=[("B", 7)])
                P.op("dve", lambda e: e.tensor_copy(nmT[0:8, :], Bb[7][0:8, 0:1024]), r=[("B", 7)], w=["nmT"])


            for T in range(4):
                if T == 1:
                    gate1()
                if T == 2:
                    gate2()
                def mmmg(e, T=T):
                    for cc in range(8):
                        ins = e.matmul(B[7][:, 0:512], wsl[:, sVG + cc * 256 + 128:sVG + cc * 256 + 256],
                                       hT[:, cc * S + T * 512:cc * S + T * 512 + 512], start=(cc == 0), stop=(cc == 7))
                    return ins
                P.op("pe", mmmg, r=[("wsl", 2 * p + 1)] + K("hT", 4 * T, 4 * T + 4), w=[("B", 7)])
                P.op("act", lambda e: e.activation(tmg[:], B[7][:, 0:512], AF.Tanh, scale=0.5), r=[("B", 7)], w=["tmg"])
                P.op("dve", lambda e: e.scalar_tensor_tensor(um[:], tmg[:], 1.0, B[7][:, 0:512], ALU.add, ALU.mult),
                     r=[("B", 7), "tmg"], w=["um"])
                ob = 3 + T % 2
                lb = 5 + T % 2
                nst = 4 * (T + 1)
                def rec_qk(stl, T=T):
                    i = stl - 4 * T
                    c0 = 128 * i if i >= 0 else 0
                    j = stl // 2
                    a = nextA()
                    need_mask = (T >= 2) and (j <= 2 * T)
                    cm0 = 0 if j < 2 * T else 256

                    def mms(e, a=a, T=T, stl=stl, i=i, c0=c0, j=j, need_mask=need_mask, cm0=cm0):
                        last = not (i >= 0 or need_mask)
                        ins = e.matmul(B[a][:, c0:512], kT[:, stl * 128:(stl + 1) * 128], qT[:, T * 512 + c0:T * 512 + 512],
                                       start=True, stop=last)
                        if i >= 0:
                            ins = e.matmul(B[a][:, c0:c0 + 128], ident, cb[:, CB_NEGTRI:CB_NEGTRI + 128],
                                           start=False, stop=not need_mask)
                        if need_mask:
                            ins = e.matmul(B[a][:, cm0:512], cb[:, CB_NEGSEL + j * 128:CB_NEGSEL + (j + 1) * 128],
                                           nmT[:, T * 512 - 1024 + cm0:T * 512 - 1024 + 512], start=False, stop=True)
                        return ins
                    P.op("pe", mms, r=[("kT", stl)] + K("qT", 4 * T, 4 * T + 4) + ["cb", "nmT"], w=[("B", a)])
                    return a

                def rec_exp_pv(stl, a, T=T, ob=ob, lb=lb, nst=nst, pre=None):
                    i = stl - 4 * T
                    c0 = 128 * i if i >= 0 else 0
                    pb = stl % 4
                    for (lo, hi, bi) in BIAS_PLAN[(h, T, stl)]:
                        P.op("act", lambda e, a=a, pb=pb, lo=lo, hi=hi, bi=bi: e.activation(
                            PT[:, pb * 512 + lo:pb * 512 + hi], B[a][:, lo:hi], AF.Exp,
                            bias=cf[:, CF_ABIAS + bi:CF_ABIAS + bi + 1], scale=SCALE),
                            r=[("B", a), "cf"], w=[("PTm", pb)])
                    if pre is not None:
                        pre()

                    def mmpv(e, pb=pb, c0=c0, stl=stl, ob=ob, lb=lb, nst=nst):
                        e.matmul(B[ob][:, c0:512], vtok[:, stl * 128:(stl + 1) * 128], PT[:, pb * 512 + c0:pb * 512 + 512],
                                 start=(stl == 0), stop=(stl == nst - 1))
                        return e.matmul(B[lb][:, c0:512], cb[:, CB_ONES2:CB_ONES2 + 128], PT[:, pb * 512 + c0:pb * 512 + 512],
                                        start=(stl == 0), stop=(stl == nst - 1))
                    P.op("pe", mmpv, r=[("PTm", pb), ("vtokm", stl), "cb"], w=[("B", ob), ("B", lb)])

                banks = {}
                for stl in range(min(2, nst)):
                    banks[stl] = rec_qk(stl)
                for stl in range(nst):
                    def pre(stl=stl):
                        if stl + 2 < nst:
                            banks[stl + 2] = rec_qk(stl + 2)
                    rec_exp_pv(stl, banks[stl], pre=pre)
                P.op("dve", lambda e, lb=lb: e.reciprocal(rl[:], B[lb][:, 0:512]), r=[("B", lb)], w=["rl"])
                P.op("dve", lambda e: e.tensor_tensor(rl[:], rl[:], um[:], ALU.mult), r=["rl", "um"], w=["rl"])
                P.op("dve", lambda e, ob=ob, T=T: e.tensor_tensor(GT[:, h * S + T * 512:h * S + T * 512 + 512], B[ob][:, 0:512], rl[:], ALU.mult),
                     r=[("B", ob), "rl"], w=K("GT", 4 * T, 4 * T + 4))

        def loadO(L):
            for s in range(4):
                load_w([(wout_d, L, s * 256, 256, s * 2048, 256)], [("wsl", s)], f"wX{s}")

        def phaseO(L):
            last = (L == n_layers - 1)
            if not last:
                P.retire(K("PTm", 0, 4) + ["tg0", "tg1"], K("hb", 0, 2))
            else:
                P.dma("sp", lambda e: [e.dma_start(out=hTf[:, 0:D], in_=fnw_d.ap())], "fnw", 1, r=[], w=K("hT", 0, 16))
            for tt in range(NT):
                for half in range(2):
                    a = nextA()

                    def mmo(e, a=a, tt=tt, half=half):
                        for q in range(2):
                            s = 2 * half + q
                            for ec in range(8):
                                ins = e.matmul(B[a][:, q * 256:(q + 1) * 256], yT[:, ec * S + tt * 128:ec * S + tt * 128 + 128],
                                               wsl[:, s * 2048 + ec * 256:s * 2048 + ec * 256 + 256], start=(ec == 0), stop=(ec == 7))
                        return ins
                    P.op("pe", mmo, r=K("wsl", 2 * half, 2 * half + 2) + [("yT", tt)], w=[("B", a)])
                    xs = xres[:, tt * D + half * 512:tt * D + half * 512 + 512]
                    P.op("dve", lambda e, a=a, xs=xs: e.scalar_tensor_tensor(xs, B[a][:, 0:512], 0.5, xs, ALU.mult, ALU.add),
                         r=[("B", a), ("xres", tt)], w=[("xres", tt)])
                if not last:
                    norm_front(L + 1, tt)
                    if tt >= 1:
                        norm_back(L + 1, tt - 1)
                else:
                    final_tile(tt)
            if not last:
                norm_back(L + 1, NT - 1)
            if last:
                P.final_waits = [("sp", "out0"), ("sp", "out1")]

        def final_tile(tt):
            b = tt % 2
            stg = hTf[:, D + b * D:D + (b + 1) * D]
            if final_norm:
                rms_tile(tt)
                P.op("dve", lambda e: e.scalar_tensor_tensor(stg, xres[:, tt * D:(tt + 1) * D], rstd16[:, tt:tt + 1], hTf[:, 0:D], ALU.mult, ALU.mult),
                     r=[("xres", tt), ("rstd16", tt)] + K("hT", 0, 16), w=[("stg", b)])
                P.dma("sp", lambda e: [e.dma_start(out=out_d.ap()[tt * 128:(tt + 1) * 128, :], in_=stg)],
                      f"out{b}", 1, r=[("stg", b)])
            else:
                P.dma("sp", lambda e: [e.dma_start(out=out_d.ap()[tt * 128:(tt + 1) * 128, :], in_=xres[:, tt * D:(tt + 1) * D])],
                      f"out{b}", 1, r=[("xres", tt)])

        units = []
        for L in range(n_layers):
            if L == 0:
                units.append((set(), None, lambda L=L: phaseN(L), L))
            units.append(({0, 1, 2, 3}, (lambda L=L: loadR(L, 0)), (lambda L=L: phaseRall(L)), L))
            for eb in range(4):
                units.append(({2 * (eb % 2), 2 * (eb % 2) + 1}, (lambda L=L, eb=eb: loadXO(L, eb, wro_d, C_GR)),
                              (lambda L=L, eb=eb: phaseXO(L, eb, True)), L))
            for h in range(MOBA_H):
                units.append(({2 * (h % 2), 2 * (h % 2) + 1}, (lambda L=L, h=h: loadM(L, h)), (lambda L=L, h=h: phaseM(L, h)), L))
            for eb in range(4):
                units.append(({2 * (eb % 2), 2 * (eb % 2) + 1}, (lambda L=L, eb=eb: loadXO(L, eb, wmo_d, C_GM)),
                              (lambda L=L, eb=eb: phaseXO(L, eb, False)), L))
            units.append(({0, 1, 2, 3}, (lambda L=L: loadO(L)), (lambda L=L: phaseO(L)), L))
        loaded = [False] * len(units)
        for i, (slots, ld, comp, L) in enumerate(units):
            P.epoch = L
            if ld is not None and not loaded[i]:
                ld()
                loaded[i] = True
            busy = set(slots)
            for k in range(i + 1, min(i + 3, len(units))):
                s2, ld2, _, _ = units[k]
                if ld2 is None:
                    continue
                if loaded[k]:
                    busy |= s2
                    continue
                if s2 & busy:
                    break
                ld2()
                loaded[k] = True
                busy |= s2
            comp()
        P.emit()
    return nc


_CACHE = {}


def _get_prog(n_layers, final_norm):
    key = (n_layers, final_norm)
    if key not in _CACHE:
        _CACHE[key] = build(n_layers, final_norm)
    return _CACHE[key]


def kernel(x, ln_w, w_in, ret_gn_w, w_ret_o, w_moba_o, w_out, final_norm_w):
    x = np.ascontiguousarray(np.asarray(x, dtype=np.float32))
    ln_w = np.asarray(ln_w, dtype=np.float32)
    ret_gn_w = np.asarray(ret_gn_w, dtype=np.float32)
    w_in = np.ascontiguousarray(np.asarray(w_in, dtype=np.float32))
    w_ret_o = np.ascontiguousarray(np.asarray(w_ret_o, dtype=np.float32))
    w_moba_o = np.ascontiguousarray(np.asarray(w_moba_o, dtype=np.float32))
    w_out = np.ascontiguousarray(np.asarray(w_out, dtype=np.float32))
    fnw = np.ascontiguousarray(np.broadcast_to(np.asarray(final_norm_w, dtype=np.float32)[None, :], (128, D)))
    nL = w_in.shape[0]
    cf, cb, qk = host_consts(ln_w, ret_gn_w, nL, 0)
    nc = _get_prog(nL, True)
    ncores = x.shape[0]
    in_maps = [{"x": x[b], "w_in": w_in, "w_ro": w_ret_o, "w_mo": w_moba_o, "w_out": w_out,
                "cf": cf, "cb": cb, "qksc": qk, "fnw": fnw} for b in range(ncores)]
    res = run_bass_kernel_spmd(nc, in_maps, core_ids=list(range(ncores)))
    return np.stack([np.asarray(r["out"], dtype=np.float32) for r in res.results], axis=0)
```

```python
from contextlib import ExitStack
import numpy as np
import concourse.bass as bass
import concourse.mybir as mybir
from concourse.bass_utils import run_bass_kernel_spmd

F32 = mybir.dt.float32
BF16 = mybir.dt.bfloat16
ALU = mybir.AluOpType
AF = mybir.ActivationFunctionType
AX = mybir.AxisListType

ENGS = ("pe", "act", "dve", "pool", "sp")


class _Op:
    __slots__ = ("eng", "fn", "deps", "mark", "rank", "kind", "stream", "ndma", "epoch", "idx")


class Prog:
    def __init__(self, nc):
        self.nc = nc
        self.ops = {e: [] for e in ENGS}
        self.lastw = {}
        self.readers = {}
        self.stream_cnt = {}
        self.epoch = 0
        self.final_waits = []

    def _deps(self, r, w):
        deps = set()
        for k in r:
            t = self.lastw.get(k)
            if t is not None:
                deps.add(t + ("raw",))
        for k in w:
            t = self.lastw.get(k)
            if t is not None:
                deps.add(t + ("waw",))
            for t in self.readers.get(k, ()):
                deps.add(t + ("war",))
        return deps

    def _commit(self, tok, r, w):
        for k in r:
            self.readers.setdefault(k, []).append(tok)
        for k in w:
            self.lastw[k] = tok
            self.readers[k] = []

    def retire(self, old_keys, new_keys):
        toks = []
        for k in old_keys:
            t = self.lastw.get(k)
            if t is not None:
                toks.append(t)
            toks.extend(self.readers.get(k, ()))
        toks = list(dict.fromkeys(toks))
        for k in new_keys:
            cur = list(self.readers.get(k, ()))
            t = self.lastw.get(k)
            if t is not None:
                cur.append(t)
            self.lastw[k] = None
            self.readers[k] = list(dict.fromkeys(cur + toks))

    def op(self, eng, fn, r=(), w=()):
        o = _Op()
        o.eng, o.fn, o.kind, o.mark, o.epoch = eng, fn, "c", False, self.epoch
        o.deps = self._deps(r, w)
        xk = [("Bx", k[1]) for k in list(r) + list(w) if isinstance(k, tuple) and k[0] == "B"] if eng != "pe" else []
        for k in xk:
            t = self.lastw.get(k)
            if t is not None:
                o.deps.add(t + ("x",))
        o.idx = len(self.ops[eng])
        self.ops[eng].append(o)
        tok = ("e", eng, o.idx)
        self._commit(tok, r, w)
        for k in xk:
            self.lastw[k] = tok
        return o

    def dma(self, q, fn, stream, ndma, r=(), w=()):
        o = _Op()
        o.eng, o.fn, o.kind, o.mark, o.epoch = q, fn, "d", False, self.epoch
        o.stream, o.ndma = stream, ndma
        o.deps = self._deps(r, w)
        o.idx = len(self.ops[q])
        self.ops[q].append(o)
        c = self.stream_cnt.get(stream, 0) + ndma
        self.stream_cnt[stream] = c
        self._commit(("d", stream, c), r, w)
        return o

    def emit(self):
        nc = self.nc
        ops = self.ops
        for e in ENGS:
            for o in ops[e]:
                nd = set()
                for d in o.deps:
                    if d[0] == "e":
                        pe_, idx, kind = d[1], d[2], d[3]
                        if pe_ == e and (e == "pe" or kind == "x"):
                            continue
                        ops[pe_][idx].mark = True
                        nd.add(("e", pe_, idx))
                    else:
                        nd.add(("d", d[1], d[2]))
                o.deps = nd
        nep = self.epoch + 1
        for e in ENGS:
            cnt = [0] * nep
            for o in ops[e]:
                if o.mark:
                    cnt[o.epoch] += 1
                    o.rank = cnt[o.epoch]
        with ExitStack() as st:
            esem = {}
            for e in ("pe", "act", "dve", "pool"):
                for ep in range(nep):
                    esem[(e, ep)] = st.enter_context(nc.semaphore(f"s_{e}_{ep}"))
            ssem = {s: st.enter_context(nc.semaphore(f"d_{s}")) for s in self.stream_cnt}
            block = st.enter_context(nc.Block())

            def run(e, eng):
                waited = {}
                for o in ops[e]:
                    need = {}
                    for d in o.deps:
                        if d[0] == "e":
                            po = ops[d[1]][d[2]]
                            key = ("e", d[1], po.epoch)
                            val = po.rank
                        else:
                            key = ("d", d[1])
                            val = 16 * d[2]
                        if need.get(key, 0) < val:
                            need[key] = val
                    for key, val in need.items():
                        if waited.get(key, 0) >= val:
                            continue
                        waited[key] = val
                        sem = esem[(key[1], key[2])] if key[0] == "e" else ssem[key[1]]
                        eng.wait_ge(sem, val)
                    if o.kind == "c":
                        ins = o.fn(eng)
                        if o.mark:
                            ins.then_inc(esem[(e, o.epoch)], 1)
                    else:
                        lst = o.fn(eng)
                        assert len(lst) == o.ndma, (len(lst), o.ndma)
                        for ins in lst:
                            ins.then_inc(ssem[o.stream], 16)
                for (q, stream) in self.final_waits:
                    if q == e:
                        eng.wait_ge(ssem[stream], 16 * self.stream_cnt[stream])

            @block.tensor
            def _(eng):
                run("pe", eng)

            @block.scalar
            def _(eng):
                run("act", eng)

            @block.vector
            def _(eng):
                run("dve", eng)

            @block.gpsimd
            def _(eng):
                run("pool", eng)

            @block.sync
            def _(eng):
                run("sp", eng)


def sb_ap(t, col, dims, p0=0, npart=128):
    F = 1
    for s in t.shape[1:]:
        F *= s
    return bass.AP(t, p0 * F + col, [[F, npart]] + [list(d) for d in dims])


def K(name, lo, hi):
    return [(name, i) for i in range(lo, hi)]


S = 2048
D = 1024
NT = 16
EPS = 1e-6
RET_H, MOBA_H = 4, 8
C_RQ, C_RK, C_RV, C_RG = 0, 512, 1024, 2048
C_MQ, C_MK, C_MV, C_MG = 3072, 4096, 5120, 6144
C_GR, C_GM = 7168, 8192
DIN = 9216
NEG = -30000.0
GAMMA = [1.0 - 2.0 ** (-5.0 - h) for h in range(RET_H)]
SLOPE = [2.0 ** (-8.0 * (h + 1.0) / MOBA_H) for h in range(MOBA_H)]
SCALE = 128.0 ** -0.5


def _bias_plan():
    table = {}
    plan = {}
    for h in range(MOBA_H):
        for T in range(4):
            for st in range(4 * (T + 1)):
                i = st - 4 * T
                c0 = 128 * i if i >= 0 else 0
                segs = []
                if h == 0:
                    rngs = [(max(c0, 0), 256, 512 * T + 128), (max(c0, 256), 512, 512 * T + 384)]
                else:
                    rngs = [(c0, 512, 512 * T + 256)]
                for lo, hi, ref in rngs:
                    if lo >= hi:
                        continue
                    key = (h, 128 * st - ref)
                    if key not in table:
                        table[key] = len(table)
                    segs.append((lo, hi, table[key]))
                plan[(h, T, st)] = segs
    return plan, table


BIAS_PLAN, BIAS_TABLE = _bias_plan()
NBIAS = len(BIAS_TABLE)

CF_ZCOL = 0
CF_ABIAS = CF_ZCOL + 4
CF_NEGM = CF_ABIAS + NBIAS
CF_PW = CF_NEGM + 64
CF_LNW = CF_PW + 16
CF_GNW = CF_LNW + 32
NCF = CF_GNW + 32
CB_TRI01 = 0
CB_NEGTRI = 128
CB_IDENT = 256
CB_ONES2 = 384
CB_NEGSEL = 512
NCB = CB_NEGSEL + 7 * 128


def host_consts(ln_w, ret_gn_w, n_layers, layer0):
    p = np.arange(128, dtype=np.float64)
    cf = np.zeros((128, NCF), np.float64)
    for h in range(RET_H):
        cf[:, CF_ZCOL + h] = GAMMA[h] ** (127.0 - p) * 128.0 ** -0.5
    for (h, delta), idx in BIAS_TABLE.items():
        cf[:, CF_ABIAS + idx] = SLOPE[h] * (delta + p)
    for i in range(8):
        qb = (8 + i) // 2
        for j in range(8):
            cf[:, CF_NEGM + i * 8 + j] = 0.0 if j < qb else -1e30
    cf[:, CF_PW:CF_PW + 16] = -0.5
    cf = cf.astype(np.float32)
    for l in range(n_layers):
        cf[:, CF_LNW + l * 8:CF_LNW + l * 8 + 8] = ln_w[layer0 + l].reshape(8, 128).T
        cf[:, CF_GNW + l * 8:CF_GNW + l * 8 + 8] = ret_gn_w[layer0 + l].reshape(8, 128).T
    cb = np.zeros((128, NCB), np.float32)
    m = np.arange(128)[:, None]
    n = np.arange(128)[None, :]
    cb[:, CB_TRI01:CB_TRI01 + 128] = (m <= n)
    cb[:, CB_NEGTRI:CB_NEGTRI + 128] = np.where(m > n, NEG, 0.0)
    cb[:, CB_IDENT:CB_IDENT + 128] = (m == n)
    cb[:, CB_ONES2:CB_ONES2 + 128] = 2.0
    for j in range(7):
        cb[j, CB_NEGSEL + j * 128:CB_NEGSEL + (j + 1) * 128] = NEG
    nn = np.arange(128, dtype=np.float64)
    qk = np.zeros((RET_H, 128, 256), np.float32)
    for h in range(RET_H):
        qk[h, :, 0:128] = (GAMMA[h] ** (nn + 1.0))[None, :]
        qk[h, :, 128:256] = (GAMMA[h] ** (127.0 - nn) * 128.0 ** -0.5)[None, :]
    return cf, cb, qk


def build(n_layers=4, final_norm=True):
    nc = bass.Bass("TRN2", target_bir_lowering=False)
    x_d = nc.dram_tensor("x", [S, D], F32, kind="ExternalInput")
    win_d = nc.dram_tensor("w_in", [n_layers, D, DIN], F32, kind="ExternalInput")
    wro_d = nc.dram_tensor("w_ro", [n_layers, D, D], F32, kind="ExternalInput")
    wmo_d = nc.dram_tensor("w_mo", [n_layers, D, D], F32, kind="ExternalInput")
    wout_d = nc.dram_tensor("w_out", [n_layers, D, D], F32, kind="ExternalInput")
    cf_d = nc.dram_tensor("cf", [128, NCF], F32, kind="ExternalInput")
    cb_d = nc.dram_tensor("cb", [128, NCB], F32, kind="ExternalInput")
    qk_d = nc.dram_tensor("qksc", [RET_H, 128, 256], F32, kind="ExternalInput")
    fnw_d = nc.dram_tensor("fnw", [128, D], F32, kind="ExternalInput")
    out_d = nc.dram_tensor("out", [S, D], F32, kind="ExternalOutput")

    with ExitStack() as st:
        def sb(name, shape, dt):
            return st.enter_context(nc.sbuf_tensor(name, shape, dt))

        xres = sb("xres", [128, NT * D], F32)
        hT = sb("hT", [128, 8 * S], BF16)
        GT = sb("GT", [128, 8 * S], BF16)
        yT = sb("yT", [128, 8 * S], BF16)
        wsl = sb("wsl", [128, 4 * 2048], BF16)
        qT = sb("qT", [128, S], BF16)
        kT = sb("kT", [128, S], BF16)
        vtok = sb("vtok", [128, NT * 128], BF16)
        PT = sb("PT", [128, 4 * 512], BF16)
        cf = sb("cf_sb", [128, NCF], F32)
        cb = sb("cb_sb", [128, NCB], BF16)
        qksc = sb("qksc_sb", [128, 256], F32)
        st12 = sb("st12", [128, NT * 12], F32)
        mv = sb("mv", [128, NT * 2], F32)
        ms16 = sb("ms16", [128, 16], F32)
        rstd16 = sb("rstd16", [128, 16], F32)
        gsm = sb("gsm", [128, 48], F32)
        gm = sb("gm", [128, 64], F32)
        ocp = sb("ocp", [128, 3 * 256], BF16)
        m8 = sb("m8", [128, 64], F32)
        nmb = sb("nmb", [128, 64], BF16)
        nmT = sb("nmT", [128, 1024], BF16)
        ksum = sb("ksum", [128, 8], F32)
        kmh = sb("kmh", [128, 8], BF16)
        kml = sb("kml", [128, 8], BF16)
        tmg = sb("tmg", [128, 512], BF16)
        um = sb("um", [128, 512], BF16)
        rl = sb("rl", [128, 512], F32)
        B = [st.enter_context(nc.psum_tensor(f"B{i}", [128, 512], F32)) for i in range(8)]

        Bb = [b[:].bitcast(BF16) for b in B]
        PTf = PT[:].bitcast(F32)
        VTf = vtok[:].bitcast(F32)
        hTf = hT[:].bitcast(F32)
        GTf = GT[:].bitcast(F32)

        ident = cb[:, CB_IDENT:CB_IDENT + 128]
        P = Prog(nc)
        rot = [0]

        def nextA():
            i = rot[0] % 3
            rot[0] += 1
            return i

        def wrows(dten, L):
            return dten.ap()[L].rearrange("(c p) n -> p c n", p=128)

        def load_w(pieces, skeys, stream):
            def f(e):
                r = []
                for (dten, L, c0, n, slot_col, width) in pieces:
                    src = wrows(dten, L)
                    for half in range(2):
                        r.append(e.dma_start(out=sb_ap(wsl, slot_col + half * 4 * width, [[width, 4], [1, n]]),
                                             in_=src[:, half * 4:(half + 1) * 4, c0:c0 + n]))
                return r
            P.dma("pool", f, stream, 2 * len(pieces), w=skeys)

        P.dma("sp", lambda e: [e.dma_start(out=cf[:], in_=cf_d.ap())], "cf", 1, w=["cf"])
        P.dma("sp", lambda e: [e.dma_start(out=GTf[:, 0:NCB], in_=cb_d.ap())], "cbs", 1, w=K("GT", 0, 16))
        P.op("dve", lambda e: e.tensor_copy(cb[:], GTf[:, 0:NCB]), r=K("GT", 0, 16), w=["cb"])
        P.op("pool", lambda e: e.memset(nmT[:], 0.0), w=["nmT"])
        for g in range(4):
            def f(e, g=g):
                return [e.dma_start(out=xres[:, (4 * g + i) * D:(4 * g + i + 1) * D],
                                    in_=x_d.ap()[(4 * g + i) * 128:(4 * g + i + 1) * 128, :]) for i in range(4)]
            P.dma("sp", f, f"xin{g}", 4, w=K("xres", 4 * g, 4 * g + 4))

        def rms_tile(tt):
            for hf in range(2):
                P.op("dve", lambda e, hf=hf: e.bn_stats(
                    st12[:, tt * 12 + hf * 6:tt * 12 + hf * 6 + 6],
                    xres[:, tt * D + hf * 512:tt * D + hf * 512 + 512]), r=[("xres", tt)], w=[("st12", tt)])
            P.op("dve", lambda e: e.bn_aggr(mv[:, tt * 2:tt * 2 + 2], st12[:, tt * 12:tt * 12 + 12]),
                 r=[("st12", tt)], w=[("mv", tt)])
            P.op("dve", lambda e: e.tensor_tensor(ms16[:, tt:tt + 1], mv[:, tt * 2:tt * 2 + 1], mv[:, tt * 2:tt * 2 + 1], ALU.mult),
                 r=[("mv", tt)], w=[("ms16", tt)])
            P.op("dve", lambda e: e.scalar_tensor_tensor(ms16[:, tt:tt + 1], ms16[:, tt:tt + 1], EPS, mv[:, tt * 2 + 1:tt * 2 + 2], ALU.add, ALU.add),
                 r=[("mv", tt), ("ms16", tt)], w=[("ms16", tt)])
            P.op("pool", lambda e: e.tensor_tensor(rstd16[:, tt:tt + 1], ms16[:, tt:tt + 1], cf[:, CF_PW:CF_PW + 1], ALU.pow),
                 r=[("ms16", tt), "cf"], w=[("rstd16", tt)])

        def norm_front(L, tt):
            rms_tile(tt)
            b = tt % 2
            hb = PT[:, b * 1024:(b + 1) * 1024]
            P.op("act", lambda e: e.activation(hb, xres[:, tt * D:(tt + 1) * D], AF.Copy, scale=rstd16[:, tt:tt + 1]),
                 r=[("xres", tt), ("rstd16", tt)], w=[("hb", b)])

        def norm_back(L, tt):
            b = tt % 2
            hb = PT[:, b * 1024:(b + 1) * 1024]
            bk = 6 + b

            def tr(e):
                for c in range(8):
                    ins = e.transpose(Bb[bk][:, c * 128:(c + 1) * 128], hb[:, c * 128:(c + 1) * 128], ident)
                return ins
            P.op("pe", tr, r=[("hb", b), "cb"], w=[("B", bk)])
            def ev(e):
                for c in range(8):
                    ins = e.activation(hT[:, c * S + tt * 128:c * S + (tt + 1) * 128], Bb[bk][:, c * 128:(c + 1) * 128], AF.Copy,
                                       scale=cf[:, CF_LNW + L * 8 + c:CF_LNW + L * 8 + c + 1])
                return ins
            P.op("act", ev, r=[("B", bk), "cf"], w=[("hT", tt)])

        def phaseN(L):
            P.retire(K("PTm", 0, 4) + ["tg0", "tg1"], K("hb", 0, 2))
            for tt in range(NT + 1):
                if tt < NT:
                    norm_front(L, tt)
                if tt >= 1:
                    norm_back(L, tt - 1)

        def loadRA(L, h):
            load_w([(win_d, L, C_RK + 128 * h, 128, 0, 512), (win_d, L, C_RV + 256 * h, 256, 128, 512),
                    (win_d, L, C_RQ + 128 * h, 128, 384, 512)], K("wsl", 0, 2), "wA")
            P.dma("sp", lambda e: [e.dma_start(out=qksc[:], in_=qk_d.ap()[h])], "qksc", 1, w=["qksc"])

        def loadRB(L, h):
            sB = 2 + h % 2
            load_w([(win_d, L, C_RG + 256 * h, 256, sB * 2048, 256)], [("wsl", sB)], f"wB{sB}")

        def loadR(L, h):
            loadRA(L, h)
            loadRB(L, h)

        def vT_ap(j, lo, n):
            return yT[:, j * S + lo:j * S + lo + n]

        def uT_ap(j, lo, n):
            return yT[:, 4096 + j * S + lo:4096 + j * S + lo + n]

        def kvt_ap(c, lo, n):
            return yT[:, 8192 + c * 384 + lo:8192 + c * 384 + lo + n]

        def rtg_ap(i):
            return yT[:, 14336 + i * 512:14336 + (i + 1) * 512]

        def insb_ap(i):
            return yT[:, 15360 + i * 128:15360 + (i + 1) * 128]

        def on_ap(i):
            return yT[:, 15616 + i * 256:15616 + (i + 1) * 256]

        def sbf_ap(c):
            return vtok[:, c * 256:(c + 1) * 256] if c < 8 else PT[:, (c - 8) * 256:(c - 7) * 256]
        R_S = rl[:, 0:256]
        RKEYS_Y = (K("vT", 0, 16) + K("uT", 0, 16) + K("kvt", 0, 16) + ["rtg0", "rtg1"] + K("insb", 0, 2) + K("on", 0, 3))

        def phaseRall(L):
            P.retire(K("hb", 0, 2) + K("PTm", 0, 4) + ["tg0", "tg1"], K("sbf", 8, 15))
            P.retire(K("vtokm", 0, 16), K("sbf", 0, 8))
            P.retire(K("yT", 0, 16), RKEYS_Y)
            P.retire(["rl"], K("Sst", 0, 2))
            pj = [0]
            rcnt = [0]

            def proj_tile(h, T, kind):
                sB = 2 + h % 2
                gnw0 = CF_GNW + L * 8 + 2 * h
                if kind == "q":
                    wfn, wkeys = (lambda cc: cc * 512 + 384), K("wsl", 0, 2)
                elif kind == "k":
                    wfn, wkeys = (lambda cc: cc * 512), K("wsl", 0, 2)
                elif kind in ("v0", "v1"):
                    j = int(kind[1])
                    wfn, wkeys = (lambda cc: cc * 512 + 128 + j * 128), K("wsl", 0, 2)
                else:
                    j = int(kind[1])
                    wfn, wkeys = (lambda cc: sB * 2048 + cc * 256 + j * 128), [("wsl", sB)]
                a = pj[0] % 3
                pj[0] += 1

                def mm(e):
                    for cc in range(8):
                        w0 = wfn(cc)
                        ins = e.matmul(B[a][:, 0:512], wsl[:, w0:w0 + 128],
                                       hT[:, cc * S + T * 512:cc * S + T * 512 + 512], start=(cc == 0), stop=(cc == 7))
                    return ins
                P.op("pe", mm, r=wkeys + K("hT", 4 * T, 4 * T + 4), w=[("B", a)])
                if kind in ("q", "k"):
                    which = 0 if kind == "q" else 1
                    dst = qT if which == 0 else kT
                    dkey = "qT" if which == 0 else "kT"
                    P.op("dve", lambda e: e.tensor_tensor(
                        sb_ap(dst, T * 512, [[128, 4], [1, 128]]),
                        B[a][:, 0:512].rearrange("p (c n) -> p c n", c=4),
                        sb_ap(qksc, which * 128, [[0, 4], [1, 128]]), ALU.mult),
                        r=[("B", a), "qksc"], w=K(dkey, 4 * T, 4 * T + 4))
                elif kind in ("v0", "v1"):
                    P.op("act", lambda e: e.activation(vT_ap(j, T * 512, 512), B[a][:, 0:512], AF.Copy),
                         r=[("B", a)], w=K("vT", 4 * T, 4 * T + 4))
                else:
                    ri = rcnt[0] % 2
                    rcnt[0] += 1
                    P.op("act", lambda e: e.activation(rtg_ap(ri), B[a][:, 0:512], AF.Tanh, scale=0.5),
                         r=[("B", a)], w=[f"rtg{ri}"])
                    P.op("dve", lambda e: e.scalar_tensor_tensor(uT_ap(j, T * 512, 512), rtg_ap(ri), 1.0, B[a][:, 0:512], ALU.add, ALU.mult),
                         r=[("B", a), f"rtg{ri}"], w=K("uT", 4 * T, 4 * T + 4))
                    P.op("act", lambda e: e.activation(uT_ap(j, T * 512, 512), uT_ap(j, T * 512, 512), AF.Copy,
                                                        scale=cf[:, gnw0 + j:gnw0 + j + 1]),
                         r=K("uT", 4 * T, 4 * T + 4) + ["cf"], w=K("uT", 4 * T, 4 * T + 4))

            def st1(h, pr):
                def trkv(e):
                    for q in range(2):
                        c = 2 * pr + q
                        e.transpose(Bb[3][:, q * 384:q * 384 + 128], kT[:, c * 128:(c + 1) * 128], ident)
                        e.transpose(Bb[3][:, q * 384 + 128:q * 384 + 256], vT_ap(0, c * 128, 128), ident)
                        ins = e.transpose(Bb[3][:, q * 384 + 256:q * 384 + 384], vT_ap(1, c * 128, 128), ident)
                    return ins
                P.op("pe", trkv, r=K("kT", 2 * pr, 2 * pr + 2) + K("vT", 2 * pr, 2 * pr + 2) + ["cb"], w=[("B", 3)])
                P.op("act", lambda e: e.activation(kvt_ap(2 * pr, 0, 768), Bb[3][:, 0:768], AF.Copy),
                     r=[("B", 3)], w=K("kvt", 2 * pr, 2 * pr + 2))

            def st2A1(h2, c2, hA, cA):
                do2 = c2 is not None and c2 < NT - 1
                doA = cA is not None
                if not (do2 or doA):
                    return
                r = []
                if do2:
                    r += [("kvt", c2)]
                if doA:
                    r += [("kT", cA), ("qT", cA)]

                def mm(e):
                    ins = None
                    if do2:
                        ins = e.matmul(B[4][:, 0:256], kvt_ap(c2, 0, 128), kvt_ap(c2, 128, 256), start=True, stop=True)
                    if doA:
                        tok = slice(cA * 128, (cA + 1) * 128)
                        ins = e.matmul(B[4][:, 256:384], kT[:, tok], qT[:, tok], start=True, stop=True)
                    return ins
                P.op("pe", mm, r=r, w=[("B", 4)])
                if do2:
                    g2 = GAMMA[h2]
                    Sc = rl[:, (c2 % 2) * 256:(c2 % 2) * 256 + 256]
                    Sp = rl[:, ((c2 + 1) % 2) * 256:((c2 + 1) % 2) * 256 + 256]
                    if c2 == 0:
                        P.op("dve", lambda e: e.tensor_copy(Sc, B[4][:, 0:256]), r=[("B", 4)], w=[("Sst", c2 % 2)])
                    else:
                        P.op("dve", lambda e: e.scalar_tensor_tensor(Sc, Sp, float(g2 ** 128.0), B[4][:, 0:256], ALU.mult, ALU.add),
                             r=[("B", 4), ("Sst", (c2 + 1) % 2)], w=[("Sst", c2 % 2)])
                if doA:
                    gA = GAMMA[hA]
                    i2 = cA % 2
                    P.op("dve", lambda e: e.scalar_tensor_tensor(insb_ap(i2), B[4][:, 256:384], float(gA ** -128.0), cb[:, CB_TRI01:CB_TRI01 + 128], ALU.mult, ALU.mult),
                         r=[("B", 4), "cb"], w=[("insb", i2)])
                if do2:
                    P.op("dve", lambda e: e.tensor_copy(sbf_ap(c2), Sc), r=[("Sst", c2 % 2)], w=[("sbf", c2)])

            def stA2(h, c):
                tok = slice(c * 128, (c + 1) * 128)
                i2 = c % 2
                ob = 5 + c % 2

                def mmo(e):
                    if c > 0:
                        e.matmul(B[ob][:, 0:256], qT[:, tok], sbf_ap(c - 1), start=True, stop=False)
                    return e.matmul(B[ob][:, 0:256], insb_ap(i2), kvt_ap(c, 128, 256), start=(c == 0), stop=True)
                P.op("pe", mmo, r=[("qT", c), ("insb", i2), ("kvt", c)] + ([("sbf", c - 1)] if c > 0 else []), w=[("B", ob)])

            def stB1(h, c):
                ob = 5 + c % 2
                g3 = c % 3
                g0 = g3 * 16
                P.op("dve", lambda e: e.bn_stats(gsm[:, g0:g0 + 6], B[ob][:, 0:256]), r=[("B", ob)], w=[("gsm", g3)])
                P.op("dve", lambda e: e.bn_aggr(gsm[:, g0 + 6:g0 + 8], gsm[:, g0:g0 + 6]), r=[("gsm", g3)], w=[("gsm", g3)])
                P.op("act", lambda e: e.activation(ocp[:, g3 * 256:(g3 + 1) * 256], B[ob][:, 0:256], AF.Copy),
                     r=[("B", ob)], w=[("ocp", g3)])
                P.op("pool", lambda e: e.tensor_scalar(gsm[:, g0 + 8:g0 + 9], gsm[:, g0 + 7:g0 + 8], EPS, 4.0, ALU.add, ALU.mult),
                     r=[("gsm", g3)], w=[("gsm", g3)])
                P.op("pool", lambda e: e.tensor_tensor(gsm[:, g0 + 9:g0 + 10], gsm[:, g0 + 8:g0 + 9], cf[:, CF_PW:CF_PW + 1], ALU.pow),
                     r=[("gsm", g3), "cf"], w=[("gsm", g3)])
                P.op("pool", lambda e: e.tensor_scalar(gsm[:, g0 + 10:g0 + 11], gsm[:, g0 + 6:g0 + 7], -1.0, gsm[:, g0 + 9:g0 + 10], ALU.mult, ALU.mult),
                     r=[("gsm", g3)], w=[("gsm", g3)])

            def stB2a(h, c):
                ob = 5 + c % 2
                g3 = c % 3
                g0 = g3 * 16
                P.op("act", lambda e: e.activation(on_ap(g3), ocp[:, g3 * 256:(g3 + 1) * 256], AF.Identity,
                                                    bias=gsm[:, g0 + 10:g0 + 11], scale=gsm[:, g0 + 9:g0 + 10]),
                     r=[("ocp", g3), ("gsm", g3)], w=[("on", g3)])

            def stB2b(h, c):
                g3 = c % 3

                def trr(e):
                    e.transpose(Bb[7][:, 0:128], on_ap(g3)[:, 0:128], ident)
                    return e.transpose(Bb[7][:, 128:256], on_ap(g3)[:, 128:256], ident)
                P.op("pe", trr, r=[("on", g3), "cb"], w=[("B", 7)])

            def stB3(h, c):
                P.op("dve", lambda e: e.tensor_tensor(
                    sb_ap(GT, 2 * h * S + c * 128, [[S, 2], [1, 128]]),
                    Bb[7][:, 0:256].rearrange("p (j n) -> p j n", j=2),
                    sb_ap(yT, 4096 + c * 128, [[S, 2], [1, 128]]), ALU.mult),
                    r=[("B", 7), ("uT", c)], w=[("GT", c)])

            NG = RET_H * NT
            PROJ_ORDER = [("q", "k"), ("v0", "v1"), ("r0",), ("r1",)]
            stages = [(9, stA2), (10, stB1), (11, stB2a), (12, stB2b), (13, stB3)]
            for gstep in range(NG + 14):
                for (dly, fn) in reversed(stages):
                    gc = gstep - dly
                    if 0 <= gc < NG:
                        fn(gc // NT, gc % NT)
                g2, gA = gstep - 6, gstep - 8
                st2A1(g2 // NT if 0 <= g2 < NG else None, g2 % NT if 0 <= g2 < NG else None,
                      gA // NT if 0 <= gA < NG else None, gA % NT if 0 <= gA < NG else None)
                if gstep % 2 == 0:
                    gc = gstep - 4
                    if 0 <= gc < NG:
                        st1(gc // NT, (gc % NT) // 2)
                if gstep < NG:
                    h, cc_ = gstep // NT, gstep % NT
                    for kind in PROJ_ORDER[cc_ % 4]:
                        proj_tile(h, cc_ // 4, kind)
                    if cc_ == 0 and h + 1 < RET_H:
                        loadRB(L, h + 1)
                    if cc_ == 13 and h + 1 < RET_H:
                        loadRA(L, h + 1)

        def loadXO(L, eb, wd, gcol):
            p = eb % 2
            load_w([(wd, L, eb * 256, 256, (2 * p) * 2048, 256)], [("wsl", 2 * p)], f"wX{2 * p}")
            load_w([(win_d, L, gcol + eb * 256, 256, (2 * p + 1) * 2048, 256)], [("wsl", 2 * p + 1)], f"wX{2 * p + 1}")

        def phaseXO(L, eb, first):
            p = eb % 2
            if eb == 0:
                if first:
                    P.retire(K("sbf", 8, 15), ["tg0", "tg1"])
                    P.retire(RKEYS_Y, K("yT", 0, 16))
                else:
                    P.retire(K("PTm", 0, 4), ["tg0", "tg1"])
            for ec in range(2):
                echunk = 2 * eb + ec
                for T in range(4):
                    aa = nextA()

                    def mma(e, aa=aa, T=T, ec=ec):
                        for vc_ in range(8):
                            ins = e.matmul(B[aa][:, 0:512], wsl[:, (2 * p) * 2048 + vc_ * 256 + ec * 128:(2 * p) * 2048 + vc_ * 256 + ec * 128 + 128],
                                           GT[:, vc_ * S + T * 512:vc_ * S + T * 512 + 512], start=(vc_ == 0), stop=(vc_ == 7))
                        return ins
                    P.op("pe", mma, r=[("wsl", 2 * p)] + K("GT", 4 * T, 4 * T + 4), w=[("B", aa)])
                    ag = nextA()

                    def mmg(e, ag=ag, T=T, ec=ec):
                        for cc in range(8):
                            ins = e.matmul(B[ag][:, 0:512], wsl[:, (2 * p + 1) * 2048 + cc * 256 + ec * 128:(2 * p + 1) * 2048 + cc * 256 + ec * 128 + 128],
                                           hT[:, cc * S + T * 512:cc * S + T * 512 + 512], start=(cc == 0), stop=(cc == 7))
                        return ins
                    P.op("pe", mmg, r=[("wsl", 2 * p + 1)] + K("hT", 4 * T, 4 * T + 4), w=[("B", ag)])
                    tb = (ec * 4 + T) % 2
                    tg = PT[:, tb * 512:(tb + 1) * 512]
                    tkey = f"tg{tb}"
                    P.op("act", lambda e, ag=ag, tg=tg: e.activation(tg, B[ag][:, 0:512], AF.Tanh, scale=0.5),
                         r=[("B", ag)], w=[tkey])
                    ydst = yT[:, echunk * S + T * 512:echunk * S + T * 512 + 512]
                    if first:
                        P.op("dve", lambda e, aa=aa, tg=tg, ydst=ydst: e.scalar_tensor_tensor(ydst, tg, 1.0, B[aa][:, 0:512], ALU.add, ALU.mult),
                             r=[("B", aa), tkey], w=K("yT", 4 * T, 4 * T + 4))
                    else:
                        P.op("dve", lambda e, aa=aa, tg=tg: e.scalar_tensor_tensor(rl[:], tg, 1.0, B[aa][:, 0:512], ALU.add, ALU.mult),
                             r=[("B", aa), tkey], w=["rl"])
                        P.op("dve", lambda e, ydst=ydst: e.tensor_tensor(ydst, rl[:], ydst, ALU.add),
                             r=["rl"] + K("yT", 4 * T, 4 * T + 4), w=K("yT", 4 * T, 4 * T + 4))

        def loadM(L, h):
            p = h % 2
            load_w([(win_d, L, C_MQ + 128 * h, 128, (2 * p) * 2048, 256), (win_d, L, C_MK + 128 * h, 128, (2 * p) * 2048 + 128, 256)],
                   [("wsl", 2 * p)], f"wX{2 * p}")
            load_w([(win_d, L, C_MV + 128 * h, 128, (2 * p + 1) * 2048, 256), (win_d, L, C_MG + 128 * h, 128, (2 * p + 1) * 2048 + 128, 256)],
                   [("wsl", 2 * p + 1)], f"wX{2 * p + 1}")

        def phaseM(L, h):
            p = h % 2
            sQK = (2 * p) * 2048
            sVG = (2 * p + 1) * 2048
            if h == 0:
                P.retire(["tg0", "tg1"], K("PTm", 0, 4))
                P.retire(K("sbf", 0, 8), K("vtokm", 0, 16))
                P.retire(K("Sst", 0, 2), ["rl"])
            for T in range(4):
                a = nextA()

                def mmq(e, a=a, T=T):
                    for cc in range(8):
                        ins = e.matmul(B[a][:, 0:512], wsl[:, sQK + cc * 256:sQK + cc * 256 + 128],
                                       hT[:, cc * S + T * 512:cc * S + T * 512 + 512], start=(cc == 0), stop=(cc == 7))
                    return ins
                P.op("pe", mmq, r=[("wsl", 2 * p)] + K("hT", 4 * T, 4 * T + 4), w=[("B", a)])
                P.op("act", lambda e, a=a, T=T: e.activation(qT[:, T * 512:(T + 1) * 512], B[a][:, 0:512], AF.Copy),
                     r=[("B", a)], w=K("qT", 4 * T, 4 * T + 4))
                a2 = nextA()

                def mmk(e, a2=a2, T=T):
                    for cc in range(8):
                        ins = e.matmul(B[a2][:, 0:512], wsl[:, sQK + cc * 256 + 128:sQK + cc * 256 + 256],
                                       hT[:, cc * S + T * 512:cc * S + T * 512 + 512], start=(cc == 0), stop=(cc == 7))
                    return ins
                P.op("pe", mmk, r=[("wsl", 2 * p)] + K("hT", 4 * T, 4 * T + 4), w=[("B", a2)])
                P.op("act", lambda e, a2=a2, T=T: e.activation(kT[:, T * 512:(T + 1) * 512], B[a2][:, 0:512], AF.Copy),
                     r=[("B", a2)], w=K("kT", 4 * T, 4 * T + 4))
                P.op("dve", lambda e, a2=a2, T=T: e.tensor_reduce(ksum[:, 2 * T:2 * T + 2], B[a2][:, 0:512].rearrange("p (b n) -> p b n", b=2), AX.X, ALU.add),
                     r=[("B", a2)], w=["ksum"])
            for T in range(4):
                a = nextA()

                def mmv(e, a=a, T=T):
                    for cc in range(8):
                        ins = e.matmul(B[a][:, 0:512], wsl[:, sVG + cc * 256:sVG + cc * 256 + 128],
                                       hT[:, cc * S + T * 512:cc * S + T * 512 + 512], start=(cc == 0), stop=(cc == 7))
                    return ins
                P.op("pe", mmv, r=[("wsl", 2 * p + 1)] + K("hT", 4 * T, 4 * T + 4), w=[("B", a)])
                P.op("act", lambda e, a=a: e.activation(tmg[:], B[a][:, 0:512], AF.Copy), r=[("B", a)], w=["tmg"])
                bk = 6 if T % 2 == 0 else 7

                def trv(e, bk=bk):
                    for i in range(4):
                        ins = e.transpose(Bb[bk][:, i * 128:(i + 1) * 128], tmg[:, i * 128:(i + 1) * 128], ident)
                    return ins
                P.op("pe", trv, r=["tmg", "cb"], w=[("B", bk)])
                P.op("dve", lambda e, bk=bk, T=T: e.tensor_copy(vtok[:, T * 512:(T + 1) * 512], Bb[bk][:, 0:512]),
                     r=[("B", bk)], w=K("vtokm", 4 * T, 4 * T + 4))
            def gate1():
                P.op("dve", lambda e: e.tensor_copy(kmh[:], ksum[:]), r=["ksum"], w=["kmh"])
                P.op("dve", lambda e: e.tensor_tensor(kml[:], ksum[:], kmh[:], ALU.subtract), r=["ksum", "kmh"], w=["kml"])

                def mmgate(e):
                    for i in range(8):
                        tt = 8 + i
                        e.matmul(B[7][:, i * 8:(i + 1) * 8], qT[:, tt * 128:(tt + 1) * 128], kmh[:], start=True, stop=False)
                        ins = e.matmul(B[7][:, i * 8:(i + 1) * 8], qT[:, tt * 128:(tt + 1) * 128], kml[:], start=False, stop=True)
                    return ins
                P.op("pe", mmgate, r=K("qT", 8, 16) + ["kmh", "kml"], w=[("B", 7)])
                P.op("dve", lambda e: e.tensor_tensor(gm[:], B[7][:, 0:64], cf[:, CF_NEGM:CF_NEGM + 64], ALU.add),
                     r=[("B", 7), "cf"], w=["gm"])
                for i in range(8):
                    P.op("dve", lambda e, i=i: e.max(m8[:, i * 8:(i + 1) * 8], gm[:, i * 8:(i + 1) * 8]), r=["gm"], w=["m8"])
                P.op("dve", lambda e: e.tensor_tensor(sb_ap(nmb, 0, [[8, 8], [1, 8]]), sb_ap(gm, 0, [[8, 8], [1, 8]]),
                                                       sb_ap(m8, 2, [[8, 8], [0, 8]]), ALU.is_lt),
                     r=["gm", "m8"], w=["nmb"])


            def gate2():
                def trm(e):
                    for i in range(8):
                        ins = e.transpose(Bb[7][0:8, i * 128:(i + 1) * 128], nmb[:, i * 8:(i + 1) * 8], ident)
                    return ins
                P.op("pe", trm, r=["nmb", "cb"], w=[("B", 7)])
                P.op("dve", lambda e: e.tensor_copy(nmT[0:8, :], Bb[7][0:8, 0:1024]), r=[("B", 7)], w=["nmT"])


            for T in range(4):
                if T == 1:
                    gate1()
                if T == 2:
                    gate2()
                def mmmg(e, T=T):
                    for cc in range(8):
                        ins = e.matmul(B[7][:, 0:512], wsl[:, sVG + cc * 256 + 128:sVG + cc * 256 + 256],
                                       hT[:, cc * S + T * 512:cc * S + T * 512 + 512], start=(cc == 0), stop=(cc == 7))
                    return ins
                P.op("pe", mmmg, r=[("wsl", 2 * p + 1)] + K("hT", 4 * T, 4 * T + 4), w=[("B", 7)])
                P.op("act", lambda e: e.activation(tmg[:], B[7][:, 0:512], AF.Tanh, scale=0.5), r=[("B", 7)], w=["tmg"])
                P.op("dve", lambda e: e.scalar_tensor_tensor(um[:], tmg[:], 1.0, B[7][:, 0:512], ALU.add, ALU.mult),
                     r=[("B", 7), "tmg"], w=["um"])
                ob = 3 + T % 2
                lb = 5 + T % 2
                nst = 4 * (T + 1)
                def rec_qk(stl, T=T):
                    i = stl - 4 * T
                    c0 = 128 * i if i >= 0 else 0
                    j = stl // 2
                    a = nextA()
                    need_mask = (T >= 2) and (j <= 2 * T)
                    cm0 = 0 if j < 2 * T else 256

                    def mms(e, a=a, T=T, stl=stl, i=i, c0=c0, j=j, need_mask=need_mask, cm0=cm0):
                        last = not (i >= 0 or need_mask)
                        ins = e.matmul(B[a][:, c0:512], kT[:, stl * 128:(stl + 1) * 128], qT[:, T * 512 + c0:T * 512 + 512],
                                       start=True, stop=last)
                        if i >= 0:
                            ins = e.matmul(B[a][:, c0:c0 + 128], ident, cb[:, CB_NEGTRI:CB_NEGTRI + 128],
                                           start=False, stop=not need_mask)
                        if need_mask:
                            ins = e.matmul(B[a][:, cm0:512], cb[:, CB_NEGSEL + j * 128:CB_NEGSEL + (j + 1) * 128],
                                           nmT[:, T * 512 - 1024 + cm0:T * 512 - 1024 + 512], start=False, stop=True)
                        return ins
                    P.op("pe", mms, r=[("kT", stl)] + K("qT", 4 * T, 4 * T + 4) + ["cb", "nmT"], w=[("B", a)])
                    return a

                def rec_exp_pv(stl, a, T=T, ob=ob, lb=lb, nst=nst, pre=None):
                    i = stl - 4 * T
                    c0 = 128 * i if i >= 0 else 0
                    pb = stl % 4
                    for (lo, hi, bi) in BIAS_PLAN[(h, T, stl)]:
                        P.op("act", lambda e, a=a, pb=pb, lo=lo, hi=hi, bi=bi: e.activation(
                            PT[:, pb * 512 + lo:pb * 512 + hi], B[a][:, lo:hi], AF.Exp,
                            bias=cf[:, CF_ABIAS + bi:CF_ABIAS + bi + 1], scale=SCALE),
                            r=[("B", a), "cf"], w=[("PTm", pb)])
                    if pre is not None:
                        pre()

                    def mmpv(e, pb=pb, c0=c0, stl=stl, ob=ob, lb=lb, nst=nst):
                        e.matmul(B[ob][:, c0:512], vtok[:, stl * 128:(stl + 1) * 128], PT[:, pb * 512 + c0:pb * 512 + 512],
                                 start=(stl == 0), stop=(stl == nst - 1))
                        return e.matmul(B[lb][:, c0:512], cb[:, CB_ONES2:CB_ONES2 + 128], PT[:, pb * 512 + c0:pb * 512 + 512],
                                        start=(stl == 0), stop=(stl == nst - 1))
                    P.op("pe", mmpv, r=[("PTm", pb), ("vtokm", stl), "cb"], w=[("B", ob), ("B", lb)])

                banks = {}
                for stl in range(min(2, nst)):
                    banks[stl] = rec_qk(stl)
                for stl in range(nst):
                    def pre(stl=stl):
                        if stl + 2 < nst:
                            banks[stl + 2] = rec_qk(stl + 2)
                    rec_exp_pv(stl, banks[stl], pre=pre)
                P.op("dve", lambda e, lb=lb: e.reciprocal(rl[:], B[lb][:, 0:512]), r=[("B", lb)], w=["rl"])
                P.op("dve", lambda e: e.tensor_tensor(rl[:], rl[:], um[:], ALU.mult), r=["rl", "um"], w=["rl"])
                P.op("dve", lambda e, ob=ob, T=T: e.tensor_tensor(GT[:, h * S + T * 512:h * S + T * 512 + 512], B[ob][:, 0:512], rl[:], ALU.mult),
                     r=[("B", ob), "rl"], w=K("GT", 4 * T, 4 * T + 4))

        def loadO(L):
            for s in range(4):
                load_w([(wout_d, L, s * 256, 256, s * 2048, 256)], [("wsl", s)], f"wX{s}")

        def phaseO(L):
            last = (L == n_layers - 1)
            if not last:
                P.retire(K("PTm", 0, 4) + ["tg0", "tg1"], K("hb", 0, 2))
            else:
                P.dma("sp", lambda e: [e.dma_start(out=hTf[:, 0:D], in_=fnw_d.ap())], "fnw", 1, r=[], w=K("hT", 0, 16))
            for tt in range(NT):
                for half in range(2):
                    a = nextA()

                    def mmo(e, a=a, tt=tt, half=half):
                        for q in range(2):
                            s = 2 * half + q
                            for ec in range(8):
                                ins = e.matmul(B[a][:, q * 256:(q + 1) * 256], yT[:, ec * S + tt * 128:ec * S + tt * 128 + 128],
                                               wsl[:, s * 2048 + ec * 256:s * 2048 + ec * 256 + 256], start=(ec == 0), stop=(ec == 7))
                        return ins
                    P.op("pe", mmo, r=K("wsl", 2 * half, 2 * half + 2) + [("yT", tt)], w=[("B", a)])
                    xs = xres[:, tt * D + half * 512:tt * D + half * 512 + 512]
                    P.op("dve", lambda e, a=a, xs=xs: e.scalar_tensor_tensor(xs, B[a][:, 0:512], 0.5, xs, ALU.mult, ALU.add),
                         r=[("B", a), ("xres", tt)], w=[("xres", tt)])
                if not last:
                    norm_front(L + 1, tt)
                    if tt >= 1:
                        norm_back(L + 1, tt - 1)
                else:
                    final_tile(tt)
            if not last:
                norm_back(L + 1, NT - 1)
            if last:
                P.final_waits = [("sp", "out0"), ("sp", "out1")]

        def final_tile(tt):
            b = tt % 2
            stg = hTf[:, D + b * D:D + (b + 1) * D]
            if final_norm:
                rms_tile(tt)
                P.op("dve", lambda e: e.scalar_tensor_tensor(stg, xres[:, tt * D:(tt + 1) * D], rstd16[:, tt:tt + 1], hTf[:, 0:D], ALU.mult, ALU.mult),
                     r=[("xres", tt), ("rstd16", tt)] + K("hT", 0, 16), w=[("stg", b)])
                P.dma("sp", lambda e: [e.dma_start(out=out_d.ap()[tt * 128:(tt + 1) * 128, :], in_=stg)],
                      f"out{b}", 1, r=[("stg", b)])
            else:
                P.dma("sp", lambda e: [e.dma_start(out=out_d.ap()[tt * 128:(tt + 1) * 128, :], in_=xres[:, tt * D:(tt + 1) * D])],
                      f"out{b}", 1, r=[("xres", tt)])

        units = []
        for L in range(n_layers):
            if L == 0:
                units.append((set(), None, lambda L=L: phaseN(L), L))
            units.append(({0, 1, 2, 3}, (lambda L=L: loadR(L, 0)), (lambda L=L: phaseRall(L)), L))
            for eb in range(4):
                units.append(({2 * (eb % 2), 2 * (eb % 2) + 1}, (lambda L=L, eb=eb: loadXO(L, eb, wro_d, C_GR)),
                              (lambda L=L, eb=eb: phaseXO(L, eb, True)), L))
            for h in range(MOBA_H):
                units.append(({2 * (h % 2), 2 * (h % 2) + 1}, (lambda L=L, h=h: loadM(L, h)), (lambda L=L, h=h: phaseM(L, h)), L))
            for eb in range(4):
                units.append(({2 * (eb % 2), 2 * (eb % 2) + 1}, (lambda L=L, eb=eb: loadXO(L, eb, wmo_d, C_GM)),
                              (lambda L=L, eb=eb: phaseXO(L, eb, False)), L))
            units.append(({0, 1, 2, 3}, (lambda L=L: loadO(L)), (lambda L=L: phaseO(L)), L))
        loaded = [False] * len(units)
        for i, (slots, ld, comp, L) in enumerate(units):
            P.epoch = L
            if ld is not None and not loaded[i]:
                ld()
                loaded[i] = True
            busy = set(slots)
            for k in range(i + 1, min(i + 3, len(units))):
                s2, ld2, _, _ = units[k]
                if ld2 is None:
                    continue
                if loaded[k]:
                    busy |= s2
                    continue
                if s2 & busy:
                    break
                ld2()
                loaded[k] = True
                busy |= s2
            comp()
        P.emit()
    return nc


_CACHE = {}


def _get_prog(n_layers, final_norm):
    key = (n_layers, final_norm)
    if key not in _CACHE:
        _CACHE[key] = build(n_layers, final_norm)
    return _CACHE[key]


def kernel(x, ln_w, w_in, ret_gn_w, w_ret_o, w_moba_o, w_out, final_norm_w):
    x = np.ascontiguousarray(np.asarray(x, dtype=np.float32))
    ln_w = np.asarray(ln_w, dtype=np.float32)
    ret_gn_w = np.asarray(ret_gn_w, dtype=np.float32)
    w_in = np.ascontiguousarray(np.asarray(w_in, dtype=np.float32))
    w_ret_o = np.ascontiguousarray(np.asarray(w_ret_o, dtype=np.float32))
    w_moba_o = np.ascontiguousarray(np.asarray(w_moba_o, dtype=np.float32))
    w_out = np.ascontiguousarray(np.asarray(w_out, dtype=np.float32))
    fnw = np.ascontiguousarray(np.broadcast_to(np.asarray(final_norm_w, dtype=np.float32)[None, :], (128, D)))
    nL = w_in.shape[0]
    cf, cb, qk = host_consts(ln_w, ret_gn_w, nL, 0)
    nc = _get_prog(nL, True)
    ncores = x.shape[0]
    in_maps = [{"x": x[b], "w_in": w_in, "w_ro": w_ret_o, "w_mo": w_moba_o, "w_out": w_out,
                "cf": cf, "cb": cb, "qksc": qk, "fnw": fnw} for b in range(ncores)]
    res = run_bass_kernel_spmd(nc, in_maps, core_ids=list(range(ncores)))
    return np.stack([np.asarray(r["out"], dtype=np.float32) for r in res.results], axis=0)
```

```python
from contextlib import ExitStack
import numpy as np
import concourse.bass as bass
import concourse.mybir as mybir
from concourse.bass_utils import run_bass_kernel_spmd

F32 = mybir.dt.float32
BF16 = mybir.dt.bfloat16
ALU = mybir.AluOpType
AF = mybir.ActivationFunctionType
AX = mybir.AxisListType

ENGS = ("pe", "act", "dve", "pool", "sp")


class _Op:
    __slots__ = ("eng", "fn", "deps", "mark", "rank", "kind", "stream", "ndma", "epoch", "idx")


class Prog:
    def __init__(self, nc):
        self.nc = nc
        self.ops = {e: [] for e in ENGS}
        self.lastw = {}
        self.readers = {}
        self.stream_cnt = {}
        self.epoch = 0
        self.final_waits = []

    def _deps(self, r, w):
        deps = set()
        for k in r:
            t = self.lastw.get(k)
            if t is not None:
                deps.add(t + ("raw",))
        for k in w:
            t = self.lastw.get(k)
            if t is not None:
                deps.add(t + ("waw",))
            for t in self.readers.get(k, ()):
                deps.add(t + ("war",))
        return deps

    def _commit(self, tok, r, w):
        for k in r:
            self.readers.setdefault(k, []).append(tok)
        for k in w:
            self.lastw[k] = tok
            self.readers[k] = []

    def retire(self, old_keys, new_keys):
        toks = []
        for k in old_keys:
            t = self.lastw.get(k)
            if t is not None:
                toks.append(t)
            toks.extend(self.readers.get(k, ()))
        toks = list(dict.fromkeys(toks))
        for k in new_keys:
            cur = list(self.readers.get(k, ()))
            t = self.lastw.get(k)
            if t is not None:
                cur.append(t)
            self.lastw[k] = None
            self.readers[k] = list(dict.fromkeys(cur + toks))

    def op(self, eng, fn, r=(), w=()):
        o = _Op()
        o.eng, o.fn, o.kind, o.mark, o.epoch = eng, fn, "c", False, self.epoch
        o.deps = self._deps(r, w)
        xk = [("Bx", k[1]) for k in list(r) + list(w) if isinstance(k, tuple) and k[0] == "B"] if eng != "pe" else []
        for k in xk:
            t = self.lastw.get(k)
            if t is not None:
                o.deps.add(t + ("x",))
        o.idx = len(self.ops[eng])
        self.ops[eng].append(o)
        tok = ("e", eng, o.idx)
        self._commit(tok, r, w)
        for k in xk:
            self.lastw[k] = tok
        return o

    def dma(self, q, fn, stream, ndma, r=(), w=()):
        o = _Op()
        o.eng, o.fn, o.kind, o.mark, o.epoch = q, fn, "d", False, self.epoch
        o.stream, o.ndma = stream, ndma
        o.deps = self._deps(r, w)
        o.idx = len(self.ops[q])
        self.ops[q].append(o)
        c = self.stream_cnt.get(stream, 0) + ndma
        self.stream_cnt[stream] = c
        self._commit(("d", stream, c), r, w)
        return o

    def emit(self):
        nc = self.nc
        ops = self.ops
        for e in ENGS:
            for o in ops[e]:
                nd = set()
                for d in o.deps:
                    if d[0] == "e":
                        pe_, idx, kind = d[1], d[2], d[3]
                        if pe_ == e and (e == "pe" or kind == "x"):
                            continue
                        ops[pe_][idx].mark = True
                        nd.add(("e", pe_, idx))
                    else:
                        nd.add(("d", d[1], d[2]))
                o.deps = nd
        nep = self.epoch + 1
        for e in ENGS:
            cnt = [0] * nep
            for o in ops[e]:
                if o.mark:
                    cnt[o.epoch] += 1
                    o.rank = cnt[o.epoch]
        with ExitStack() as st:
            esem = {}
            for e in ("pe", "act", "dve", "pool"):
                for ep in range(nep):
                    esem[(e, ep)] = st.enter_context(nc.semaphore(f"s_{e}_{ep}"))
            ssem = {s: st.enter_context(nc.semaphore(f"d_{s}")) for s in self.stream_cnt}
            block = st.enter_context(nc.Block())

            def run(e, eng):
                waited = {}
                for o in ops[e]:
                    need = {}
                    for d in o.deps:
                        if d[0] == "e":
                            po = ops[d[1]][d[2]]
                            key = ("e", d[1], po.epoch)
                            val = po.rank
                        else:
                            key = ("d", d[1])
                            val = 16 * d[2]
                        if need.get(key, 0) < val:
                            need[key] = val
                    for key, val in need.items():
                        if waited.get(key, 0) >= val:
                            continue
                        waited[key] = val
                        sem = esem[(key[1], key[2])] if key[0] == "e" else ssem[key[1]]
                        eng.wait_ge(sem, val)
                    if o.kind == "c":
                        ins = o.fn(eng)
                        if o.mark:
                            ins.then_inc(esem[(e, o.epoch)], 1)
                    else:
                        lst = o.fn(eng)
                        assert len(lst) == o.ndma, (len(lst), o.ndma)
                        for ins in lst:
                            ins.then_inc(ssem[o.stream], 16)
                for (q, stream) in self.final_waits:
                    if q == e:
                        eng.wait_ge(ssem[stream], 16 * self.stream_cnt[stream])

            @block.tensor
            def _(eng):
                run("pe", eng)

            @block.scalar
            def _(eng):
                run("act", eng)

            @block.vector
            def _(eng):
                run("dve", eng)

            @block.gpsimd
            def _(eng):
                run("pool", eng)

            @block.sync
            def _(eng):
                run("sp", eng)


def sb_ap(t, col, dims, p0=0, npart=128):
    F = 1
    for s in t.shape[1:]:
        F *= s
    return bass.AP(t, p0 * F + col, [[F, npart]] + [list(d) for d in dims])


def K(name, lo, hi):
    return [(name, i) for i in range(lo, hi)]


S = 2048
D = 1024
NT = 16
EPS = 1e-6
RET_H, MOBA_H = 4, 8
C_RQ, C_RK, C_RV, C_RG = 0, 512, 1024, 2048
C_MQ, C_MK, C_MV, C_MG = 3072, 4096, 5120, 6144
C_GR, C_GM = 7168, 8192
DIN = 9216
NEG = -30000.0
GAMMA = [1.0 - 2.0 ** (-5.0 - h) for h in range(RET_H)]
SLOPE = [2.0 ** (-8.0 * (h + 1.0) / MOBA_H) for h in range(MOBA_H)]
SCALE = 128.0 ** -0.5


def _bias_plan():
    table = {}
    plan = {}
    for h in range(MOBA_H):
        for T in range(4):
            for st in range(4 * (T + 1)):
                i = st - 4 * T
                c0 = 128 * i if i >= 0 else 0
                segs = []
                if h == 0:
                    rngs = [(max(c0, 0), 256, 512 * T + 128), (max(c0, 256), 512, 512 * T + 384)]
                else:
                    rngs = [(c0, 512, 512 * T + 256)]
                for lo, hi, ref in rngs:
                    if lo >= hi:
                        continue
                    key = (h, 128 * st - ref)
                    if key not in table:
                        table[key] = len(table)
                    segs.append((lo, hi, table[key]))
                plan[(h, T, st)] = segs
    return plan, table


BIAS_PLAN, BIAS_TABLE = _bias_plan()
NBIAS = len(BIAS_TABLE)

CF_ZCOL = 0
CF_ABIAS = CF_ZCOL + 4
CF_NEGM = CF_ABIAS + NBIAS
CF_PW = CF_NEGM + 64
CF_LNW = CF_PW + 16
CF_GNW = CF_LNW + 32
NCF = CF_GNW + 32
CB_TRI01 = 0
CB_NEGTRI = 128
CB_IDENT = 256
CB_ONES2 = 384
CB_NEGSEL = 512
NCB = CB_NEGSEL + 7 * 128


def host_consts(ln_w, ret_gn_w, n_layers, layer0):
    p = np.arange(128, dtype=np.float64)
    cf = np.zeros((128, NCF), np.float64)
    for h in range(RET_H):
        cf[:, CF_ZCOL + h] = GAMMA[h] ** (127.0 - p) * 128.0 ** -0.5
    for (h, delta), idx in BIAS_TABLE.items():
        cf[:, CF_ABIAS + idx] = SLOPE[h] * (delta + p)
    for i in range(8):
        qb = (8 + i) // 2
        for j in range(8):
            cf[:, CF_NEGM + i * 8 + j] = 0.0 if j < qb else -1e30
    cf[:, CF_PW:CF_PW + 16] = -0.5
    cf = cf.astype(np.float32)
    for l in range(n_layers):
        cf[:, CF_LNW + l * 8:CF_LNW + l * 8 + 8] = ln_w[layer0 + l].reshape(8, 128).T
        cf[:, CF_GNW + l * 8:CF_GNW + l * 8 + 8] = ret_gn_w[layer0 + l].reshape(8, 128).T
    cb = np.zeros((128, NCB), np.float32)
    m = np.arange(128)[:, None]
    n = np.arange(128)[None, :]
    cb[:, CB_TRI01:CB_TRI01 + 128] = (m <= n)
    cb[:, CB_NEGTRI:CB_NEGTRI + 128] = np.where(m > n, NEG, 0.0)
    cb[:, CB_IDENT:CB_IDENT + 128] = (m == n)
    cb[:, CB_ONES2:CB_ONES2 + 128] = 2.0
    for j in range(7):
        cb[j, CB_NEGSEL + j * 128:CB_NEGSEL + (j + 1) * 128] = NEG
    nn = np.arange(128, dtype=np.float64)
    qk = np.zeros((RET_H, 128, 256), np.float32)
    for h in range(RET_H):
        qk[h, :, 0:128] = (GAMMA[h] ** (nn + 1.0))[None, :]
        qk[h, :, 128:256] = (GAMMA[h] ** (127.0 - nn) * 128.0 ** -0.5)[None, :]
    return cf, cb, qk


def build(n_layers=4, final_norm=True):
    nc = bass.Bass("TRN2", target_bir_lowering=False)
    x_d = nc.dram_tensor("x", [S, D], F32, kind="ExternalInput")
    win_d = nc.dram_tensor("w_in", [n_layers, D, DIN], F32, kind="ExternalInput")
    wro_d = nc.dram_tensor("w_ro", [n_layers, D, D], F32, kind="ExternalInput")
    wmo_d = nc.dram_tensor("w_mo", [n_layers, D, D], F32, kind="ExternalInput")
    wout_d = nc.dram_tensor("w_out", [n_layers, D, D], F32, kind="ExternalInput")
    cf_d = nc.dram_tensor("cf", [128, NCF], F32, kind="ExternalInput")
    cb_d = nc.dram_tensor("cb", [128, NCB], F32, kind="ExternalInput")
    qk_d = nc.dram_tensor("qksc", [RET_H, 128, 256], F32, kind="ExternalInput")
    fnw_d = nc.dram_tensor("fnw", [128, D], F32, kind="ExternalInput")
    out_d = nc.dram_tensor("out", [S, D], F32, kind="ExternalOutput")

    with ExitStack() as st:
        def sb(name, shape, dt):
            return st.enter_context(nc.sbuf_tensor(name, shape, dt))

        xres = sb("xres", [128, NT * D], F32)
        hT = sb("hT", [128, 8 * S], BF16)
        GT = sb("GT", [128, 8 * S], BF16)
        yT = sb("yT", [128, 8 * S], BF16)
        wsl = sb("wsl", [128, 4 * 2048], BF16)
        qT = sb("qT", [128, S], BF16)
        kT = sb("kT", [128, S], BF16)
        vtok = sb("vtok", [128, NT * 128], BF16)
        PT = sb("PT", [128, 4 * 512], BF16)
        cf = sb("cf_sb", [128, NCF], F32)
        cb = sb("cb_sb", [128, NCB], BF16)
        qksc = sb("qksc_sb", [128, 256], F32)
        st12 = sb("st12", [128, NT * 12], F32)
        mv = sb("mv", [128, NT * 2], F32)
        ms16 = sb("ms16", [128, 16], F32)
        rstd16 = sb("rstd16", [128, 16], F32)
        gsm = sb("gsm", [128, 48], F32)
        gm = sb("gm", [128, 64], F32)
        ocp = sb("ocp", [128, 3 * 256], BF16)
        m8 = sb("m8", [128, 64], F32)
        nmb = sb("nmb", [128, 64], BF16)
        nmT = sb("nmT", [128, 1024], BF16)
        ksum = sb("ksum", [128, 8], F32)
        kmh = sb("kmh", [128, 8], BF16)
        kml = sb("kml", [128, 8], BF16)
        tmg = sb("tmg", [128, 512], BF16)
        um = sb("um", [128, 512], BF16)
        rl = sb("rl", [128, 512], F32)
        B = [st.enter_context(nc.psum_tensor(f"B{i}", [128, 512], F32)) for i in range(8)]

        Bb = [b[:].bitcast(BF16) for b in B]
        PTf = PT[:].bitcast(F32)
        VTf = vtok[:].bitcast(F32)
        hTf = hT[:].bitcast(F32)
        GTf = GT[:].bitcast(F32)

        ident = cb[:, CB_IDENT:CB_IDENT + 128]
        P = Prog(nc)
        rot = [0]

        def nextA():
            i = rot[0] % 3
            rot[0] += 1
            return i

        def wrows(dten, L):
            return dten.ap()[L].rearrange("(c p) n -> p c n", p=128)

        def load_w(pieces, skeys, stream):
            def f(e):
                r = []
                for (dten, L, c0, n, slot_col, width) in pieces:
                    src = wrows(dten, L)
                    for half in range(2):
                        r.append(e.dma_start(out=sb_ap(wsl, slot_col + half * 4 * width, [[width, 4], [1, n]]),
                                             in_=src[:, half * 4:(half + 1) * 4, c0:c0 + n]))
                return r
            P.dma("pool", f, stream, 2 * len(pieces), w=skeys)

        P.dma("sp", lambda e: [e.dma_start(out=cf[:], in_=cf_d.ap())], "cf", 1, w=["cf"])
        P.dma("sp", lambda e: [e.dma_start(out=GTf[:, 0:NCB], in_=cb_d.ap())], "cbs", 1, w=K("GT", 0, 16))
        P.op("dve", lambda e: e.tensor_copy(cb[:], GTf[:, 0:NCB]), r=K("GT", 0, 16), w=["cb"])
        P.op("pool", lambda e: e.memset(nmT[:], 0.0), w=["nmT"])
        for g in range(4):
            def f(e, g=g):
                return [e.dma_start(out=xres[:, (4 * g + i) * D:(4 * g + i + 1) * D],
                                    in_=x_d.ap()[(4 * g + i) * 128:(4 * g + i + 1) * 128, :]) for i in range(4)]
            P.dma("sp", f, f"xin{g}", 4, w=K("xres", 4 * g, 4 * g + 4))

        def rms_tile(tt):
            for hf in range(2):
                P.op("dve", lambda e, hf=hf: e.bn_stats(
                    st12[:, tt * 12 + hf * 6:tt * 12 + hf * 6 + 6],
                    xres[:, tt * D + hf * 512:tt * D + hf * 512 + 512]), r=[("xres", tt)], w=[("st12", tt)])
            P.op("dve", lambda e: e.bn_aggr(mv[:, tt * 2:tt * 2 + 2], st12[:, tt * 12:tt * 12 + 12]),
                 r=[("st12", tt)], w=[("mv", tt)])
            P.op("dve", lambda e: e.tensor_tensor(ms16[:, tt:tt + 1], mv[:, tt * 2:tt * 2 + 1], mv[:, tt * 2:tt * 2 + 1], ALU.mult),
                 r=[("mv", tt)], w=[("ms16", tt)])
            P.op("dve", lambda e: e.scalar_tensor_tensor(ms16[:, tt:tt + 1], ms16[:, tt:tt + 1], EPS, mv[:, tt * 2 + 1:tt * 2 + 2], ALU.add, ALU.add),
                 r=[("mv", tt), ("ms16", tt)], w=[("ms16", tt)])
            P.op("pool", lambda e: e.tensor_tensor(rstd16[:, tt:tt + 1], ms16[:, tt:tt + 1], cf[:, CF_PW:CF_PW + 1], ALU.pow),
                 r=[("ms16", tt), "cf"], w=[("rstd16", tt)])

        def norm_front(L, tt):
            rms_tile(tt)
            b = tt % 2
            hb = PT[:, b * 1024:(b + 1) * 1024]
            P.op("act", lambda e: e.activation(hb, xres[:, tt * D:(tt + 1) * D], AF.Copy, scale=rstd16[:, tt:tt + 1]),
                 r=[("xres", tt), ("rstd16", tt)], w=[("hb", b)])

        def norm_back(L, tt):
            b = tt % 2
            hb = PT[:, b * 1024:(b + 1) * 1024]
            bk = 6 + b

            def tr(e):
                for c in range(8):
                    ins = e.transpose(Bb[bk][:, c * 128:(c + 1) * 128], hb[:, c * 128:(c + 1) * 128], ident)
                return ins
            P.op("pe", tr, r=[("hb", b), "cb"], w=[("B", bk)])
            def ev(e):
                for c in range(8):
                    ins = e.activation(hT[:, c * S + tt * 128:c * S + (tt + 1) * 128], Bb[bk][:, c * 128:(c + 1) * 128], AF.Copy,
                                       scale=cf[:, CF_LNW + L * 8 + c:CF_LNW + L * 8 + c + 1])
                return ins
            P.op("act", ev, r=[("B", bk), "cf"], w=[("hT", tt)])

        def phaseN(L):
            P.retire(K("PTm", 0, 4) + ["tg0", "tg1"], K("hb", 0, 2))
            for tt in range(NT + 2):
                if tt >= 2:
                    norm_back(L, tt - 2)
                if tt < NT:
                    norm_front(L, tt)

        def loadRA(L, h):
            load_w([(win_d, L, C_RK + 128 * h, 128, 0, 512), (win_d, L, C_RV + 256 * h, 256, 128, 512),
                    (win_d, L, C_RQ + 128 * h, 128, 384, 512)], K("wsl", 0, 2), "wA")
            P.dma("sp", lambda e: [e.dma_start(out=qksc[:], in_=qk_d.ap()[h])], "qksc", 1, w=["qksc"])

        def loadRB(L, h):
            sB = 2 + h % 2
            load_w([(win_d, L, C_RG + 256 * h, 256, sB * 2048, 256)], [("wsl", sB)], f"wB{sB}")

        def loadR(L, h):
            loadRA(L, h)
            loadRB(L, h)

        def vT_ap(j, lo, n):
            return yT[:, j * S + lo:j * S + lo + n]

        def uT_ap(j, lo, n):
            return yT[:, 4096 + j * S + lo:4096 + j * S + lo + n]

        def kvt_ap(c, lo, n):
            return yT[:, 8192 + c * 384 + lo:8192 + c * 384 + lo + n]

        def rtg_ap(i):
            return yT[:, 14336 + i * 512:14336 + (i + 1) * 512]

        def insb_ap(i):
            return yT[:, 15360 + i * 128:15360 + (i + 1) * 128]

        def on_ap(i):
            return yT[:, 15616 + i * 256:15616 + (i + 1) * 256]

        def sbf_ap(c):
            return vtok[:, c * 256:(c + 1) * 256] if c < 8 else PT[:, (c - 8) * 256:(c - 7) * 256]
        R_S = rl[:, 0:256]
        RKEYS_Y = (K("vT", 0, 16) + K("uT", 0, 16) + K("kvt", 0, 16) + ["rtg0", "rtg1"] + K("insb", 0, 2) + K("on", 0, 3))

        def phaseRall(L):
            P.retire(K("hb", 0, 2) + K("PTm", 0, 4) + ["tg0", "tg1"], K("sbf", 8, 15))
            P.retire(K("vtokm", 0, 16), K("sbf", 0, 8))
            P.retire(K("yT", 0, 16), RKEYS_Y)
            P.retire(["rl"], K("Sst", 0, 2))
            pj = [0]
            rcnt = [0]

            def proj_tile(h, T, kind):
                sB = 2 + h % 2
                gnw0 = CF_GNW + L * 8 + 2 * h
                if kind == "q":
                    wfn, wkeys = (lambda cc: cc * 512 + 384), K("wsl", 0, 2)
                elif kind == "k":
                    wfn, wkeys = (lambda cc: cc * 512), K("wsl", 0, 2)
                elif kind in ("v0", "v1"):
                    j = int(kind[1])
                    wfn, wkeys = (lambda cc: cc * 512 + 128 + j * 128), K("wsl", 0, 2)
                else:
                    j = int(kind[1])
                    wfn, wkeys = (lambda cc: sB * 2048 + cc * 256 + j * 128), [("wsl", sB)]
                a = pj[0] % 3
                pj[0] += 1

                def mm(e):
                    for cc in range(8):
                        w0 = wfn(cc)
                        ins = e.matmul(B[a][:, 0:512], wsl[:, w0:w0 + 128],
                                       hT[:, cc * S + T * 512:cc * S + T * 512 + 512], start=(cc == 0), stop=(cc == 7))
                    return ins
                P.op("pe", mm, r=wkeys + K("hT", 4 * T, 4 * T + 4), w=[("B", a)])
                if kind in ("q", "k"):
                    which = 0 if kind == "q" else 1
                    dst = qT if which == 0 else kT
                    dkey = "qT" if which == 0 else "kT"
                    P.op("dve", lambda e: e.tensor_tensor(
                        sb_ap(dst, T * 512, [[128, 4], [1, 128]]),
                        B[a][:, 0:512].rearrange("p (c n) -> p c n", c=4),
                        sb_ap(qksc, which * 128, [[0, 4], [1, 128]]), ALU.mult),
                        r=[("B", a), "qksc"], w=K(dkey, 4 * T, 4 * T + 4))
                elif kind in ("v0", "v1"):
                    P.op("act", lambda e: e.activation(vT_ap(j, T * 512, 512), B[a][:, 0:512], AF.Copy),
                         r=[("B", a)], w=K("vT", 4 * T, 4 * T + 4))
                else:
                    ri = rcnt[0] % 2
                    rcnt[0] += 1
                    P.op("act", lambda e: e.activation(rtg_ap(ri), B[a][:, 0:512], AF.Tanh, scale=0.5),
                         r=[("B", a)], w=[f"rtg{ri}"])
                    P.op("dve", lambda e: e.scalar_tensor_tensor(uT_ap(j, T * 512, 512), rtg_ap(ri), 1.0, B[a][:, 0:512], ALU.add, ALU.mult),
                         r=[("B", a), f"rtg{ri}"], w=K("uT", 4 * T, 4 * T + 4))
                    P.op("act", lambda e: e.activation(uT_ap(j, T * 512, 512), uT_ap(j, T * 512, 512), AF.Copy,
                                                        scale=cf[:, gnw0 + j:gnw0 + j + 1]),
                         r=K("uT", 4 * T, 4 * T + 4) + ["cf"], w=K("uT", 4 * T, 4 * T + 4))

            def st1(h, pr):
                def trkv(e):
                    for q in range(2):
                        c = 2 * pr + q
                        e.transpose(Bb[3][:, q * 384:q * 384 + 128], kT[:, c * 128:(c + 1) * 128], ident)
                        e.transpose(Bb[3][:, q * 384 + 128:q * 384 + 256], vT_ap(0, c * 128, 128), ident)
                        ins = e.transpose(Bb[3][:, q * 384 + 256:q * 384 + 384], vT_ap(1, c * 128, 128), ident)
                    return ins
                P.op("pe", trkv, r=K("kT", 2 * pr, 2 * pr + 2) + K("vT", 2 * pr, 2 * pr + 2) + ["cb"], w=[("B", 3)])
                P.op("act", lambda e: e.activation(kvt_ap(2 * pr, 0, 768), Bb[3][:, 0:768], AF.Copy),
                     r=[("B", 3)], w=K("kvt", 2 * pr, 2 * pr + 2))

            def st2A1(h2, c2, hA, cA):
                do2 = c2 is not None and c2 < NT - 1
                doA = cA is not None
                if not (do2 or doA):
                    return
                r = []
                if do2:
                    r += [("kvt", c2)]
                if doA:
                    r += [("kT", cA), ("qT", cA)]

                def mm(e):
                    ins = None
                    if do2:
                        ins = e.matmul(B[4][:, 0:256], kvt_ap(c2, 0, 128), kvt_ap(c2, 128, 256), start=True, stop=True)
                    if doA:
                        tok = slice(cA * 128, (cA + 1) * 128)
                        ins = e.matmul(B[4][:, 256:384], kT[:, tok], qT[:, tok], start=True, stop=True)
                    return ins
                P.op("pe", mm, r=r, w=[("B", 4)])
                if do2:
                    g2 = GAMMA[h2]
                    Sc = rl[:, (c2 % 2) * 256:(c2 % 2) * 256 + 256]
                    Sp = rl[:, ((c2 + 1) % 2) * 256:((c2 + 1) % 2) * 256 + 256]
                    if c2 == 0:
                        P.op("dve", lambda e: e.tensor_copy(Sc, B[4][:, 0:256]), r=[("B", 4)], w=[("Sst", c2 % 2)])
                    else:
                        P.op("dve", lambda e: e.scalar_tensor_tensor(Sc, Sp, float(g2 ** 128.0), B[4][:, 0:256], ALU.mult, ALU.add),
                             r=[("B", 4), ("Sst", (c2 + 1) % 2)], w=[("Sst", c2 % 2)])
                if doA:
                    gA = GAMMA[hA]
                    i2 = cA % 2
                    P.op("dve", lambda e: e.scalar_tensor_tensor(insb_ap(i2), B[4][:, 256:384], float(gA ** -128.0), cb[:, CB_TRI01:CB_TRI01 + 128], ALU.mult, ALU.mult),
                         r=[("B", 4), "cb"], w=[("insb", i2)])
                if do2:
                    P.op("dve", lambda e: e.tensor_copy(sbf_ap(c2), Sc), r=[("Sst", c2 % 2)], w=[("sbf", c2)])

            def stA2(h, c):
                tok = slice(c * 128, (c + 1) * 128)
                i2 = c % 2
                ob = 5 + c % 2

                def mmo(e):
                    if c > 0:
                        e.matmul(B[ob][:, 0:256], qT[:, tok], sbf_ap(c - 1), start=True, stop=False)
                    return e.matmul(B[ob][:, 0:256], insb_ap(i2), kvt_ap(c, 128, 256), start=(c == 0), stop=True)
                P.op("pe", mmo, r=[("qT", c), ("insb", i2), ("kvt", c)] + ([("sbf", c - 1)] if c > 0 else []), w=[("B", ob)])

            def stB1(h, c):
                ob = 5 + c % 2
                g3 = c % 3
                g0 = g3 * 16
                P.op("dve", lambda e: e.bn_stats(gsm[:, g0:g0 + 6], B[ob][:, 0:256]), r=[("B", ob)], w=[("gsm", g3)])
                P.op("dve", lambda e: e.bn_aggr(gsm[:, g0 + 6:g0 + 8], gsm[:, g0:g0 + 6]), r=[("gsm", g3)], w=[("gsm", g3)])
                P.op("act", lambda e: e.activation(ocp[:, g3 * 256:(g3 + 1) * 256], B[ob][:, 0:256], AF.Copy),
                     r=[("B", ob)], w=[("ocp", g3)])
                P.op("pool", lambda e: e.tensor_scalar(gsm[:, g0 + 8:g0 + 9], gsm[:, g0 + 7:g0 + 8], EPS, 4.0, ALU.add, ALU.mult),
                     r=[("gsm", g3)], w=[("gsm", g3)])
                P.op("pool", lambda e: e.tensor_tensor(gsm[:, g0 + 9:g0 + 10], gsm[:, g0 + 8:g0 + 9], cf[:, CF_PW:CF_PW + 1], ALU.pow),
                     r=[("gsm", g3), "cf"], w=[("gsm", g3)])
                P.op("pool", lambda e: e.tensor_scalar(gsm[:, g0 + 10:g0 + 11], gsm[:, g0 + 6:g0 + 7], -1.0, gsm[:, g0 + 9:g0 + 10], ALU.mult, ALU.mult),
                     r=[("gsm", g3)], w=[("gsm", g3)])

            def stB2a(h, c):
                ob = 5 + c % 2
                g3 = c % 3
                g0 = g3 * 16
                P.op("act", lambda e: e.activation(on_ap(g3), ocp[:, g3 * 256:(g3 + 1) * 256], AF.Identity,
                                                    bias=gsm[:, g0 + 10:g0 + 11], scale=gsm[:, g0 + 9:g0 + 10]),
                     r=[("ocp", g3), ("gsm", g3)], w=[("on", g3)])

            def stB2b(h, c):
                g3 = c % 3

                def trr(e):
                    e.transpose(Bb[7][:, 0:128], on_ap(g3)[:, 0:128], ident)
                    return e.transpose(Bb[7][:, 128:256], on_ap(g3)[:, 128:256], ident)
                P.op("pe", trr, r=[("on", g3), "cb"], w=[("B", 7)])

            def stB3(h, c):
                P.op("dve", lambda e: e.tensor_tensor(
                    sb_ap(GT, 2 * h * S + c * 128, [[S, 2], [1, 128]]),
                    Bb[7][:, 0:256].rearrange("p (j n) -> p j n", j=2),
                    sb_ap(yT, 4096 + c * 128, [[S, 2], [1, 128]]), ALU.mult),
                    r=[("B", 7), ("uT", c)], w=[("GT", c)])

            NG = RET_H * NT
            PROJ_ORDER = [("q", "k"), ("v0", "v1"), ("r0",), ("r1",)]
            stages = [(9, stA2), (10, stB1), (11, stB2a), (12, stB2b), (13, stB3)]
            for gstep in range(NG + 14):
                for (dly, fn) in reversed(stages):
                    gc = gstep - dly
                    if 0 <= gc < NG:
                        fn(gc // NT, gc % NT)
                g2, gA = gstep - 6, gstep - 8
                st2A1(g2 // NT if 0 <= g2 < NG else None, g2 % NT if 0 <= g2 < NG else None,
                      gA // NT if 0 <= gA < NG else None, gA % NT if 0 <= gA < NG else None)
                if gstep % 2 == 0:
                    gc = gstep - 4
                    if 0 <= gc < NG:
                        st1(gc // NT, (gc % NT) // 2)
                if gstep < NG:
                    h, cc_ = gstep // NT, gstep % NT
                    for kind in PROJ_ORDER[cc_ % 4]:
                        proj_tile(h, cc_ // 4, kind)
                    if cc_ == 0 and h + 1 < RET_H:
                        loadRB(L, h + 1)
                    if cc_ == 13 and h + 1 < RET_H:
                        loadRA(L, h + 1)

        def loadXO(L, eb, wd, gcol):
            p = eb % 2
            load_w([(wd, L, eb * 256, 256, (2 * p) * 2048, 256)], [("wsl", 2 * p)], f"wX{2 * p}")
            load_w([(win_d, L, gcol + eb * 256, 256, (2 * p + 1) * 2048, 256)], [("wsl", 2 * p + 1)], f"wX{2 * p + 1}")

        def phaseXO(L, eb, first):
            p = eb % 2
            if eb == 0:
                if first:
                    P.retire(K("sbf", 8, 15), ["tg0", "tg1"])
                    P.retire(RKEYS_Y, K("yT", 0, 16))
                else:
                    P.retire(K("PTm", 0, 4), ["tg0", "tg1"])
            for ec in range(2):
                echunk = 2 * eb + ec
                for T in range(4):
                    aa = nextA()

                    def mma(e, aa=aa, T=T, ec=ec):
                        for vc_ in range(8):
                            ins = e.matmul(B[aa][:, 0:512], wsl[:, (2 * p) * 2048 + vc_ * 256 + ec * 128:(2 * p) * 2048 + vc_ * 256 + ec * 128 + 128],
                                           GT[:, vc_ * S + T * 512:vc_ * S + T * 512 + 512], start=(vc_ == 0), stop=(vc_ == 7))
                        return ins
                    P.op("pe", mma, r=[("wsl", 2 * p)] + K("GT", 4 * T, 4 * T + 4), w=[("B", aa)])
                    ag = nextA()

                    def mmg(e, ag=ag, T=T, ec=ec):
                        for cc in range(8):
                            ins = e.matmul(B[ag][:, 0:512], wsl[:, (2 * p + 1) * 2048 + cc * 256 + ec * 128:(2 * p + 1) * 2048 + cc * 256 + ec * 128 + 128],
                                           hT[:, cc * S + T * 512:cc * S + T * 512 + 512], start=(cc == 0), stop=(cc == 7))
                        return ins
                    P.op("pe", mmg, r=[("wsl", 2 * p + 1)] + K("hT", 4 * T, 4 * T + 4), w=[("B", ag)])
                    tb = (ec * 4 + T) % 2
                    tg = PT[:, tb * 512:(tb + 1) * 512]
                    tkey = f"tg{tb}"
                    P.op("act", lambda e, ag=ag, tg=tg: e.activation(tg, B[ag][:, 0:512], AF.Tanh, scale=0.5),
                         r=[("B", ag)], w=[tkey])
                    ydst = yT[:, echunk * S + T * 512:echunk * S + T * 512 + 512]
                    if first:
                        P.op("dve", lambda e, aa=aa, tg=tg, ydst=ydst: e.scalar_tensor_tensor(ydst, tg, 1.0, B[aa][:, 0:512], ALU.add, ALU.mult),
                             r=[("B", aa), tkey], w=K("yT", 4 * T, 4 * T + 4))
                    else:
                        P.op("dve", lambda e, aa=aa, tg=tg: e.scalar_tensor_tensor(rl[:], tg, 1.0, B[aa][:, 0:512], ALU.add, ALU.mult),
                             r=[("B", aa), tkey], w=["rl"])
                        P.op("dve", lambda e, ydst=ydst: e.tensor_tensor(ydst, rl[:], ydst, ALU.add),
                             r=["rl"] + K("yT", 4 * T, 4 * T + 4), w=K("yT", 4 * T, 4 * T + 4))

        def loadM(L, h):
            p = h % 2
            load_w([(win_d, L, C_MQ + 128 * h, 128, (2 * p) * 2048, 256), (win_d, L, C_MK + 128 * h, 128, (2 * p) * 2048 + 128, 256)],
                   [("wsl", 2 * p)], f"wX{2 * p}")
            load_w([(win_d, L, C_MV + 128 * h, 128, (2 * p + 1) * 2048, 256), (win_d, L, C_MG + 128 * h, 128, (2 * p + 1) * 2048 + 128, 256)],
                   [("wsl", 2 * p + 1)], f"wX{2 * p + 1}")

        def phaseM(L, h):
            p = h % 2
            sQK = (2 * p) * 2048
            sVG = (2 * p + 1) * 2048
            if h == 0:
                P.retire(["tg0", "tg1"], K("PTm", 0, 4))
                P.retire(K("sbf", 0, 8), K("vtokm", 0, 16))
                P.retire(K("Sst", 0, 2), ["rl"])
            for T in range(4):
                a = nextA()

                def mmq(e, a=a, T=T):
                    for cc in range(8):
                        ins = e.matmul(B[a][:, 0:512], wsl[:, sQK + cc * 256:sQK + cc * 256 + 128],
                                       hT[:, cc * S + T * 512:cc * S + T * 512 + 512], start=(cc == 0), stop=(cc == 7))
                    return ins
                P.op("pe", mmq, r=[("wsl", 2 * p)] + K("hT", 4 * T, 4 * T + 4), w=[("B", a)])
                P.op("act", lambda e, a=a, T=T: e.activation(qT[:, T * 512:(T + 1) * 512], B[a][:, 0:512], AF.Copy),
                     r=[("B", a)], w=K("qT", 4 * T, 4 * T + 4))
                a2 = nextA()

                def mmk(e, a2=a2, T=T):
                    for cc in range(8):
                        ins = e.matmul(B[a2][:, 0:512], wsl[:, sQK + cc * 256 + 128:sQK + cc * 256 + 256],
                                       hT[:, cc * S + T * 512:cc * S + T * 512 + 512], start=(cc == 0), stop=(cc == 7))
                    return ins
                P.op("pe", mmk, r=[("wsl", 2 * p)] + K("hT", 4 * T, 4 * T + 4), w=[("B", a2)])
                P.op("act", lambda e, a2=a2, T=T: e.activation(kT[:, T * 512:(T + 1) * 512], B[a2][:, 0:512], AF.Copy),
                     r=[("B", a2)], w=K("kT", 4 * T, 4 * T + 4))
                P.op("dve", lambda e, a2=a2, T=T: e.tensor_reduce(ksum[:, 2 * T:2 * T + 2], B[a2][:, 0:512].rearrange("p (b n) -> p b n", b=2), AX.X, ALU.add),
                     r=[("B", a2)], w=["ksum"])
            for T in range(4):
                a = nextA()

                def mmv(e, a=a, T=T):
                    for cc in range(8):
                        ins = e.matmul(B[a][:, 0:512], wsl[:, sVG + cc * 256:sVG + cc * 256 + 128],
                                       hT[:, cc * S + T * 512:cc * S + T * 512 + 512], start=(cc == 0), stop=(cc == 7))
                    return ins
                P.op("pe", mmv, r=[("wsl", 2 * p + 1)] + K("hT", 4 * T, 4 * T + 4), w=[("B", a)])
                P.op("act", lambda e, a=a: e.activation(tmg[:], B[a][:, 0:512], AF.Copy), r=[("B", a)], w=["tmg"])
                bk = 6 if T % 2 == 0 else 7

                def trv(e, bk=bk):
                    for i in range(4):
                        ins = e.transpose(Bb[bk][:, i * 128:(i + 1) * 128], tmg[:, i * 128:(i + 1) * 128], ident)
                    return ins
                P.op("pe", trv, r=["tmg", "cb"], w=[("B", bk)])
                P.op("dve", lambda e, bk=bk, T=T: e.tensor_copy(vtok[:, T * 512:(T + 1) * 512], Bb[bk][:, 0:512]),
                     r=[("B", bk)], w=K("vtokm", 4 * T, 4 * T + 4))
            def gate1():
                P.op("dve", lambda e: e.tensor_copy(kmh[:], ksum[:]), r=["ksum"], w=["kmh"])
                P.op("dve", lambda e: e.tensor_tensor(kml[:], ksum[:], kmh[:], ALU.subtract), r=["ksum", "kmh"], w=["kml"])

                def mmgate(e):
                    for i in range(8):
                        tt = 8 + i
                        e.matmul(B[7][:, i * 8:(i + 1) * 8], qT[:, tt * 128:(tt + 1) * 128], kmh[:], start=True, stop=False)
                        ins = e.matmul(B[7][:, i * 8:(i + 1) * 8], qT[:, tt * 128:(tt + 1) * 128], kml[:], start=False, stop=True)
                    return ins
                P.op("pe", mmgate, r=K("qT", 8, 16) + ["kmh", "kml"], w=[("B", 7)])
                P.op("dve", lambda e: e.tensor_tensor(gm[:], B[7][:, 0:64], cf[:, CF_NEGM:CF_NEGM + 64], ALU.add),
                     r=[("B", 7), "cf"], w=["gm"])
                for i in range(8):
                    P.op("dve", lambda e, i=i: e.max(m8[:, i * 8:(i + 1) * 8], gm[:, i * 8:(i + 1) * 8]), r=["gm"], w=["m8"])
                P.op("dve", lambda e: e.tensor_tensor(sb_ap(nmb, 0, [[8, 8], [1, 8]]), sb_ap(gm, 0, [[8, 8], [1, 8]]),
                                                       sb_ap(m8, 2, [[8, 8], [0, 8]]), ALU.is_lt),
                     r=["gm", "m8"], w=["nmb"])


            def gate2():
                def trm(e):
                    for i in range(8):
                        ins = e.transpose(Bb[7][0:8, i * 128:(i + 1) * 128], nmb[:, i * 8:(i + 1) * 8], ident)
                    return ins
                P.op("pe", trm, r=["nmb", "cb"], w=[("B", 7)])
                P.op("dve", lambda e: e.tensor_copy(nmT[0:8, :], Bb[7][0:8, 0:1024]), r=[("B", 7)], w=["nmT"])


            def t_start(T):
                def mmmg(e):
                    for cc in range(8):
                        ins = e.matmul(B[7][:, 0:512], wsl[:, sVG + cc * 256 + 128:sVG + cc * 256 + 256],
                                       hT[:, cc * S + T * 512:cc * S + T * 512 + 512], start=(cc == 0), stop=(cc == 7))
                    return ins
                P.op("pe", mmmg, r=[("wsl", 2 * p + 1)] + K("hT", 4 * T, 4 * T + 4), w=[("B", 7)])
                P.op("act", lambda e: e.activation(tmg[:], B[7][:, 0:512], AF.Tanh, scale=0.5), r=[("B", 7)], w=["tmg"])
                P.op("dve", lambda e: e.scalar_tensor_tensor(um[:], tmg[:], 1.0, B[7][:, 0:512], ALU.add, ALU.mult),
                     r=[("B", 7), "tmg"], w=["um"])

            def rec_qk(T, stl):
                i = stl - 4 * T
                c0 = 128 * i if i >= 0 else 0
                j = stl // 2
                a = nextA()
                need_mask = (T >= 2) and (j <= 2 * T)
                cm0 = 0 if j < 2 * T else 256

                def mms(e):
                    last = not (i >= 0 or need_mask)
                    ins = e.matmul(B[a][:, c0:512], kT[:, stl * 128:(stl + 1) * 128], qT[:, T * 512 + c0:T * 512 + 512],
                                   start=True, stop=last)
                    if i >= 0:
                        ins = e.matmul(B[a][:, c0:c0 + 128], ident, cb[:, CB_NEGTRI:CB_NEGTRI + 128],
                                       start=False, stop=not need_mask)
                    if need_mask:
                        ins = e.matmul(B[a][:, cm0:512], cb[:, CB_NEGSEL + j * 128:CB_NEGSEL + (j + 1) * 128],
                                       nmT[:, T * 512 - 1024 + cm0:T * 512 - 1024 + 512], start=False, stop=True)
                    return ins
                P.op("pe", mms, r=[("kT", stl)] + K("qT", 4 * T, 4 * T + 4) + ["cb", "nmT"], w=[("B", a)])
                return a

            def rec_exp(T, stl, a):
                pb = stl % 4
                for (lo, hi, bi) in BIAS_PLAN[(h, T, stl)]:
                    P.op("act", lambda e, lo=lo, hi=hi, bi=bi: e.activation(
                        PT[:, pb * 512 + lo:pb * 512 + hi], B[a][:, lo:hi], AF.Exp,
                        bias=cf[:, CF_ABIAS + bi:CF_ABIAS + bi + 1], scale=SCALE),
                        r=[("B", a), "cf"], w=[("PTm", pb)])

            def rec_pv(T, stl):
                i = stl - 4 * T
                c0 = 128 * i if i >= 0 else 0
                pb = stl % 4
                ob = 3 + T % 2
                lb = 5 + T % 2
                nst = 4 * (T + 1)

                def mmpv(e):
                    e.matmul(B[ob][:, c0:512], vtok[:, stl * 128:(stl + 1) * 128], PT[:, pb * 512 + c0:pb * 512 + 512],
                             start=(stl == 0), stop=(stl == nst - 1))
                    return e.matmul(B[lb][:, c0:512], cb[:, CB_ONES2:CB_ONES2 + 128], PT[:, pb * 512 + c0:pb * 512 + 512],
                                    start=(stl == 0), stop=(stl == nst - 1))
                P.op("pe", mmpv, r=[("PTm", pb), ("vtokm", stl), "cb"], w=[("B", ob), ("B", lb)])

            def epilogue(T):
                ob = 3 + T % 2
                lb = 5 + T % 2
                P.op("dve", lambda e: e.reciprocal(rl[:], B[lb][:, 0:512]), r=[("B", lb)], w=["rl"])
                P.op("dve", lambda e: e.tensor_tensor(rl[:], rl[:], um[:], ALU.mult), r=["rl", "um"], w=["rl"])
                P.op("dve", lambda e: e.tensor_tensor(GT[:, h * S + T * 512:h * S + T * 512 + 512], B[ob][:, 0:512], rl[:], ALU.mult),
                     r=[("B", ob), "rl"], w=K("GT", 4 * T, 4 * T + 4))

            tiles = [(T, stl) for T in range(4) for stl in range(4 * (T + 1))]
            banks = {}
            for i in range(2):
                banks[i] = rec_qk(*tiles[i])
            for i, (T, stl) in enumerate(tiles):
                if stl == 0:
                    if T == 1:
                        gate1()
                    t_start(T)
                if T == 1 and stl == 6:
                    gate2()
                rec_exp(T, stl, banks[i])
                if i + 2 < len(tiles):
                    banks[i + 2] = rec_qk(*tiles[i + 2])
                rec_pv(T, stl)
                if stl == 4 * (T + 1) - 1:
                    epilogue(T)

        def loadO(L):
            for s in range(4):
                load_w([(wout_d, L, s * 256, 256, s * 2048, 256)], [("wsl", s)], f"wX{s}")

        def phaseO(L):
            last = (L == n_layers - 1)
            if not last:
                P.retire(K("PTm", 0, 4) + ["tg0", "tg1"], K("hb", 0, 2))
            else:
                P.dma("sp", lambda e: [e.dma_start(out=hTf[:, 0:D], in_=fnw_d.ap())], "fnw", 1, r=[], w=K("hT", 0, 16))
            for tt in range(NT):
                for half in range(2):
                    a = nextA()

                    def mmo(e, a=a, tt=tt, half=half):
                        for q in range(2):
                            s = 2 * half + q
                            for ec in range(8):
                                ins = e.matmul(B[a][:, q * 256:(q + 1) * 256], yT[:, ec * S + tt * 128:ec * S + tt * 128 + 128],
                                               wsl[:, s * 2048 + ec * 256:s * 2048 + ec * 256 + 256], start=(ec == 0), stop=(ec == 7))
                        return ins
                    P.op("pe", mmo, r=K("wsl", 2 * half, 2 * half + 2) + [("yT", tt)], w=[("B", a)])
                    xs = xres[:, tt * D + half * 512:tt * D + half * 512 + 512]
                    P.op("dve", lambda e, a=a, xs=xs: e.scalar_tensor_tensor(xs, B[a][:, 0:512], 0.5, xs, ALU.mult, ALU.add),
                         r=[("B", a), ("xres", tt)], w=[("xres", tt)])
                if not last:
                    if tt >= 2:
                        norm_back(L + 1, tt - 2)
                    norm_front(L + 1, tt)
                else:
                    final_tile(tt)
            if not last:
                norm_back(L + 1, NT - 2)
                norm_back(L + 1, NT - 1)
            if last:
                P.final_waits = [("sp", "out0"), ("sp", "out1")]

        def final_tile(tt):
            b = tt % 2
            stg = hTf[:, D + b * D:D + (b + 1) * D]
            if final_norm:
                rms_tile(tt)
                P.op("dve", lambda e: e.scalar_tensor_tensor(stg, xres[:, tt * D:(tt + 1) * D], rstd16[:, tt:tt + 1], hTf[:, 0:D], ALU.mult, ALU.mult),
                     r=[("xres", tt), ("rstd16", tt)] + K("hT", 0, 16), w=[("stg", b)])
                P.dma("sp", lambda e: [e.dma_start(out=out_d.ap()[tt * 128:(tt + 1) * 128, :], in_=stg)],
                      f"out{b}", 1, r=[("stg", b)])
            else:
                P.dma("sp", lambda e: [e.dma_start(out=out_d.ap()[tt * 128:(tt + 1) * 128, :], in_=xres[:, tt * D:(tt + 1) * D])],
                      f"out{b}", 1, r=[("xres", tt)])

        units = []
        for L in range(n_layers):
            if L == 0:
                units.append((set(), None, lambda L=L: phaseN(L), L))
            units.append(({0, 1, 2, 3}, (lambda L=L: loadR(L, 0)), (lambda L=L: phaseRall(L)), L))
            for eb in range(4):
                units.append(({2 * (eb % 2), 2 * (eb % 2) + 1}, (lambda L=L, eb=eb: loadXO(L, eb, wro_d, C_GR)),
                              (lambda L=L, eb=eb: phaseXO(L, eb, True)), L))
            for h in range(MOBA_H):
                units.append(({2 * (h % 2), 2 * (h % 2) + 1}, (lambda L=L, h=h: loadM(L, h)), (lambda L=L, h=h: phaseM(L, h)), L))
            for eb in range(4):
                units.append(({2 * (eb % 2), 2 * (eb % 2) + 1}, (lambda L=L, eb=eb: loadXO(L, eb, wmo_d, C_GM)),
                              (lambda L=L, eb=eb: phaseXO(L, eb, False)), L))
            units.append(({0, 1, 2, 3}, (lambda L=L: loadO(L)), (lambda L=L: phaseO(L)), L))
        loaded = [False] * len(units)
        for i, (slots, ld, comp, L) in enumerate(units):
            P.epoch = L
            if ld is not None and not loaded[i]:
                ld()
                loaded[i] = True
            busy = set(slots)
            for k in range(i + 1, min(i + 3, len(units))):
                s2, ld2, _, _ = units[k]
                if ld2 is None:
                    continue
                if loaded[k]:
                    busy |= s2
                    continue
                if s2 & busy:
                    break
                ld2()
                loaded[k] = True
                busy |= s2
            comp()
        P.emit()
    return nc


_CACHE = {}


def _get_prog(n_layers, final_norm):
    key = (n_layers, final_norm)
    if key not in _CACHE:
        _CACHE[key] = build(n_layers, final_norm)
    return _CACHE[key]


def kernel(x, ln_w, w_in, ret_gn_w, w_ret_o, w_moba_o, w_out, final_norm_w):
    x = np.ascontiguousarray(np.asarray(x, dtype=np.float32))
    ln_w = np.asarray(ln_w, dtype=np.float32)
    ret_gn_w = np.asarray(ret_gn_w, dtype=np.float32)
    w_in = np.ascontiguousarray(np.asarray(w_in, dtype=np.float32))
    w_ret_o = np.ascontiguousarray(np.asarray(w_ret_o, dtype=np.float32))
    w_moba_o = np.ascontiguousarray(np.asarray(w_moba_o, dtype=np.float32))
    w_out = np.ascontiguousarray(np.asarray(w_out, dtype=np.float32))
    fnw = np.ascontiguousarray(np.broadcast_to(np.asarray(final_norm_w, dtype=np.float32)[None, :], (128, D)))
    nL = w_in.shape[0]
    cf, cb, qk = host_consts(ln_w, ret_gn_w, nL, 0)
    nc = _get_prog(nL, True)
    ncores = x.shape[0]
    in_maps = [{"x": x[b], "w_in": w_in, "w_ro": w_ret_o, "w_mo": w_moba_o, "w_out": w_out,
                "cf": cf, "cb": cb, "qksc": qk, "fnw": fnw} for b in range(ncores)]
    res = run_bass_kernel_spmd(nc, in_maps, core_ids=list(range(ncores)))
    return np.stack([np.asarray(r["out"], dtype=np.float32) for r in res.results], axis=0)
```

```python
from contextlib import ExitStack
import numpy as np
import concourse.bass as bass
import concourse.mybir as mybir
from concourse.bass_utils import run_bass_kernel_spmd

F32 = mybir.dt.float32
BF16 = mybir.dt.bfloat16
ALU = mybir.AluOpType
AF = mybir.ActivationFunctionType
AX = mybir.AxisListType

ENGS = ("pe", "act", "dve", "pool", "sp")


class _Op:
    __slots__ = ("eng", "fn", "deps", "mark", "rank", "kind", "stream", "ndma", "epoch", "idx")


class Prog:
    def __init__(self, nc):
        self.nc = nc
        self.ops = {e: [] for e in ENGS}
        self.lastw = {}
        self.readers = {}
        self.stream_cnt = {}
        self.epoch = 0
        self.final_waits = []

    def _deps(self, r, w):
        deps = set()
        for k in r:
            t = self.lastw.get(k)
            if t is not None:
                deps.add(t + ("raw",))
        for k in w:
            t = self.lastw.get(k)
            if t is not None:
                deps.add(t + ("waw",))
            for t in self.readers.get(k, ()):
                deps.add(t + ("war",))
        return deps

    def _commit(self, tok, r, w):
        for k in r:
            self.readers.setdefault(k, []).append(tok)
        for k in w:
            self.lastw[k] = tok
            self.readers[k] = []

    def retire(self, old_keys, new_keys):
        toks = []
        for k in old_keys:
            t = self.lastw.get(k)
            if t is not None:
                toks.append(t)
            toks.extend(self.readers.get(k, ()))
        toks = list(dict.fromkeys(toks))
        for k in new_keys:
            cur = list(self.readers.get(k, ()))
            t = self.lastw.get(k)
            if t is not None:
                cur.append(t)
            self.lastw[k] = None
            self.readers[k] = list(dict.fromkeys(cur + toks))

    def op(self, eng, fn, r=(), w=()):
        o = _Op()
        o.eng, o.fn, o.kind, o.mark, o.epoch = eng, fn, "c", False, self.epoch
        o.deps = self._deps(r, w)
        xk = [("Bx", k[1]) for k in list(r) + list(w) if isinstance(k, tuple) and k[0] == "B"] if eng != "pe" else []
        for k in xk:
            t = self.lastw.get(k)
            if t is not None:
                o.deps.add(t + ("x",))
        o.idx = len(self.ops[eng])
        self.ops[eng].append(o)
        tok = ("e", eng, o.idx)
        self._commit(tok, r, w)
        for k in xk:
            self.lastw[k] = tok
        return o

    def dma(self, q, fn, stream, ndma, r=(), w=()):
        o = _Op()
        o.eng, o.fn, o.kind, o.mark, o.epoch = q, fn, "d", False, self.epoch
        o.stream, o.ndma = stream, ndma
        o.deps = self._deps(r, w)
        o.idx = len(self.ops[q])
        self.ops[q].append(o)
        c = self.stream_cnt.get(stream, 0) + ndma
        self.stream_cnt[stream] = c
        self._commit(("d", stream, c), r, w)
        return o

    def emit(self):
        nc = self.nc
        ops = self.ops
        for e in ENGS:
            for o in ops[e]:
                nd = set()
                for d in o.deps:
                    if d[0] == "e":
                        pe_, idx, kind = d[1], d[2], d[3]
                        if pe_ == e and (e == "pe" or kind == "x"):
                            continue
                        ops[pe_][idx].mark = True
                        nd.add(("e", pe_, idx))
                    else:
                        nd.add(("d", d[1], d[2]))
                o.deps = nd
        nep = self.epoch + 1
        for e in ENGS:
            cnt = [0] * nep
            for o in ops[e]:
                if o.mark:
                    cnt[o.epoch] += 1
                    o.rank = cnt[o.epoch]
        with ExitStack() as st:
            esem = {}
            for e in ("pe", "act", "dve", "pool"):
                for ep in range(nep):
                    esem[(e, ep)] = st.enter_context(nc.semaphore(f"s_{e}_{ep}"))
            ssem = {s: st.enter_context(nc.semaphore(f"d_{s}")) for s in self.stream_cnt}
            block = st.enter_context(nc.Block())

            def run(e, eng):
                waited = {}
                for o in ops[e]:
                    need = {}
                    for d in o.deps:
                        if d[0] == "e":
                            po = ops[d[1]][d[2]]
                            key = ("e", d[1], po.epoch)
                            val = po.rank
                        else:
                            key = ("d", d[1])
                            val = 16 * d[2]
                        if need.get(key, 0) < val:
                            need[key] = val
                    for key, val in need.items():
                        if waited.get(key, 0) >= val:
                            continue
                        waited[key] = val
                        sem = esem[(key[1], key[2])] if key[0] == "e" else ssem[key[1]]
                        eng.wait_ge(sem, val)
                    if o.kind == "c":
                        ins = o.fn(eng)
                        if o.mark:
                            ins.then_inc(esem[(e, o.epoch)], 1)
                    else:
                        lst = o.fn(eng)
                        assert len(lst) == o.ndma, (len(lst), o.ndma)
                        for ins in lst:
                            ins.then_inc(ssem[o.stream], 16)
                for (q, stream) in self.final_waits:
                    if q == e:
                        eng.wait_ge(ssem[stream], 16 * self.stream_cnt[stream])

            @block.tensor
            def _(eng):
                run("pe", eng)

            @block.scalar
            def _(eng):
                run("act", eng)

            @block.vector
            def _(eng):
                run("dve", eng)

            @block.gpsimd
            def _(eng):
                run("pool", eng)

            @block.sync
            def _(eng):
                run("sp", eng)


def sb_ap(t, col, dims, p0=0, npart=128):
    F = 1
    for s in t.shape[1:]:
        F *= s
    return bass.AP(t, p0 * F + col, [[F, npart]] + [list(d) for d in dims])


def K(name, lo, hi):
    return [(name, i) for i in range(lo, hi)]


S = 2048
D = 1024
NT = 16
EPS = 1e-6
RET_H, MOBA_H = 4, 8
C_RQ, C_RK, C_RV, C_RG = 0, 512, 1024, 2048
C_MQ, C_MK, C_MV, C_MG = 3072, 4096, 5120, 6144
C_GR, C_GM = 7168, 8192
DIN = 9216
NEG = -30000.0
GAMMA = [1.0 - 2.0 ** (-5.0 - h) for h in range(RET_H)]
SLOPE = [2.0 ** (-8.0 * (h + 1.0) / MOBA_H) for h in range(MOBA_H)]
SCALE = 128.0 ** -0.5


def _bias_plan():
    table = {}
    plan = {}
    for h in range(MOBA_H):
        for T in range(4):
            for st in range(4 * (T + 1)):
                i = st - 4 * T
                c0 = 128 * i if i >= 0 else 0
                segs = []
                if h == 0:
                    rngs = [(max(c0, 0), 256, 512 * T + 128), (max(c0, 256), 512, 512 * T + 384)]
                else:
                    rngs = [(c0, 512, 512 * T + 256)]
                for lo, hi, ref in rngs:
                    if lo >= hi:
                        continue
                    key = (h, 128 * st - ref)
                    if key not in table:
                        table[key] = len(table)
                    segs.append((lo, hi, table[key]))
                plan[(h, T, st)] = segs
    return plan, table


BIAS_PLAN, BIAS_TABLE = _bias_plan()
NBIAS = len(BIAS_TABLE)

CF_ZCOL = 0
CF_ABIAS = CF_ZCOL + 4
CF_NEGM = CF_ABIAS + NBIAS
CF_PW = CF_NEGM + 64
CF_LNW = CF_PW + 16
CF_GNW = CF_LNW + 32
NCF = CF_GNW + 32
CB_TRI01 = 0
CB_NEGTRI = 128
CB_IDENT = 256
CB_ONES2 = 384
CB_NEGSEL = 512
NCB = CB_NEGSEL + 7 * 128


def host_consts(ln_w, ret_gn_w, n_layers, layer0):
    p = np.arange(128, dtype=np.float64)
    cf = np.zeros((128, NCF), np.float64)
    for h in range(RET_H):
        cf[:, CF_ZCOL + h] = GAMMA[h] ** (127.0 - p) * 128.0 ** -0.5
    for (h, delta), idx in BIAS_TABLE.items():
        cf[:, CF_ABIAS + idx] = SLOPE[h] * (delta + p)
    for i in range(8):
        qb = (8 + i) // 2
        for j in range(8):
            cf[:, CF_NEGM + i * 8 + j] = 0.0 if j < qb else -1e30
    cf[:, CF_PW:CF_PW + 16] = -0.5
    cf = cf.astype(np.float32)
    for l in range(n_layers):
        cf[:, CF_LNW + l * 8:CF_LNW + l * 8 + 8] = ln_w[layer0 + l].reshape(8, 128).T
        cf[:, CF_GNW + l * 8:CF_GNW + l * 8 + 8] = ret_gn_w[layer0 + l].reshape(8, 128).T
    cb = np.zeros((128, NCB), np.float32)
    m = np.arange(128)[:, None]
    n = np.arange(128)[None, :]
    cb[:, CB_TRI01:CB_TRI01 + 128] = (m <= n)
    cb[:, CB_NEGTRI:CB_NEGTRI + 128] = np.where(m > n, NEG, 0.0)
    cb[:, CB_IDENT:CB_IDENT + 128] = (m == n)
    cb[:, CB_ONES2:CB_ONES2 + 128] = 2.0
    for j in range(7):
        cb[j, CB_NEGSEL + j * 128:CB_NEGSEL + (j + 1) * 128] = NEG
    nn = np.arange(128, dtype=np.float64)
    qk = np.zeros((RET_H, 128, 256), np.float32)
    for h in range(RET_H):
        qk[h, :, 0:128] = (GAMMA[h] ** (nn + 1.0))[None, :]
        qk[h, :, 128:256] = (GAMMA[h] ** (127.0 - nn) * 128.0 ** -0.5)[None, :]
    return cf, cb, qk


def build(n_layers=4, final_norm=True):
    nc = bass.Bass("TRN2", target_bir_lowering=False)
    x_d = nc.dram_tensor("x", [S, D], F32, kind="ExternalInput")
    win_d = nc.dram_tensor("w_in", [n_layers, D, DIN], F32, kind="ExternalInput")
    wro_d = nc.dram_tensor("w_ro", [n_layers, D, D], F32, kind="ExternalInput")
    wmo_d = nc.dram_tensor("w_mo", [n_layers, D, D], F32, kind="ExternalInput")
    wout_d = nc.dram_tensor("w_out", [n_layers, D, D], F32, kind="ExternalInput")
    cf_d = nc.dram_tensor("cf", [128, NCF], F32, kind="ExternalInput")
    cb_d = nc.dram_tensor("cb", [128, NCB], F32, kind="ExternalInput")
    qk_d = nc.dram_tensor("qksc", [RET_H, 128, 256], F32, kind="ExternalInput")
    fnw_d = nc.dram_tensor("fnw", [128, D], F32, kind="ExternalInput")
    out_d = nc.dram_tensor("out", [S, D], F32, kind="ExternalOutput")

    with ExitStack() as st:
        def sb(name, shape, dt):
            return st.enter_context(nc.sbuf_tensor(name, shape, dt))

        xres = sb("xres", [128, NT * D], F32)
        hT = sb("hT", [128, 8 * S], BF16)
        GT = sb("GT", [128, 8 * S], BF16)
        yT = sb("yT", [128, 8 * S], BF16)
        wsl = sb("wsl", [128, 4 * 2048], BF16)
        qT = sb("qT", [128, S], BF16)
        kT = sb("kT", [128, S], BF16)
        vtok = sb("vtok", [128, NT * 128], BF16)
        PT = sb("PT", [128, 4 * 512], BF16)
        cf = sb("cf_sb", [128, NCF], F32)
        cb = sb("cb_sb", [128, NCB], BF16)
        qksc = sb("qksc_sb", [128, 256], F32)
        st12 = sb("st12", [128, NT * 12], F32)
        mv = sb("mv", [128, NT * 2], F32)
        ms16 = sb("ms16", [128, 16], F32)
        rstd16 = sb("rstd16", [128, 16], F32)
        gsm = sb("gsm", [128, 48], F32)
        gm = sb("gm", [128, 64], F32)
        ocp = sb("ocp", [128, 3 * 256], BF16)
        m8 = sb("m8", [128, 64], F32)
        nmb = sb("nmb", [128, 64], BF16)
        nmT = sb("nmT", [128, 1024], BF16)
        ksum = sb("ksum", [128, 8], F32)
        kmh = sb("kmh", [128, 8], BF16)
        kml = sb("kml", [128, 8], BF16)
        tmg = sb("tmg", [128, 512], BF16)
        um = sb("um", [128, 512], BF16)
        rl = sb("rl", [128, 512], F32)
        B = [st.enter_context(nc.psum_tensor(f"B{i}", [128, 512], F32)) for i in range(8)]

        Bb = [b[:].bitcast(BF16) for b in B]
        PTf = PT[:].bitcast(F32)
        VTf = vtok[:].bitcast(F32)
        hTf = hT[:].bitcast(F32)
        GTf = GT[:].bitcast(F32)

        ident = cb[:, CB_IDENT:CB_IDENT + 128]
        P = Prog(nc)
        rot = [0]

        def nextA():
            i = rot[0] % 3
            rot[0] += 1
            return i

        def wrows(dten, L):
            return dten.ap()[L].rearrange("(c p) n -> p c n", p=128)

        def load_w(pieces, skeys, stream):
            def f(e):
                r = []
                for (dten, L, c0, n, slot_col, width) in pieces:
                    src = wrows(dten, L)
                    for half in range(2):
                        r.append(e.dma_start(out=sb_ap(wsl, slot_col + half * 4 * width, [[width, 4], [1, n]]),
                                             in_=src[:, half * 4:(half + 1) * 4, c0:c0 + n]))
                return r
            P.dma("pool", f, stream, 2 * len(pieces), w=skeys)

        P.dma("sp", lambda e: [e.dma_start(out=cf[:], in_=cf_d.ap())], "cf", 1, w=["cf"])
        P.dma("sp", lambda e: [e.dma_start(out=GTf[:, 0:NCB], in_=cb_d.ap())], "cbs", 1, w=K("GT", 0, 16))
        P.op("dve", lambda e: e.tensor_copy(cb[:], GTf[:, 0:NCB]), r=K("GT", 0, 16), w=["cb"])
        P.op("pool", lambda e: e.memset(nmT[:], 0.0), w=["nmT"])
        for g in range(4):
            def f(e, g=g):
                return [e.dma_start(out=xres[:, (4 * g + i) * D:(4 * g + i + 1) * D],
                                    in_=x_d.ap()[(4 * g + i) * 128:(4 * g + i + 1) * 128, :]) for i in range(4)]
            P.dma("sp", f, f"xin{g}", 4, w=K("xres", 4 * g, 4 * g + 4))

        def rms_tile(tt):
            for hf in range(2):
                P.op("dve", lambda e, hf=hf: e.bn_stats(
                    st12[:, tt * 12 + hf * 6:tt * 12 + hf * 6 + 6],
                    xres[:, tt * D + hf * 512:tt * D + hf * 512 + 512]), r=[("xres", tt)], w=[("st12", tt)])
            P.op("dve", lambda e: e.bn_aggr(mv[:, tt * 2:tt * 2 + 2], st12[:, tt * 12:tt * 12 + 12]),
                 r=[("st12", tt)], w=[("mv", tt)])
            P.op("dve", lambda e: e.tensor_tensor(ms16[:, tt:tt + 1], mv[:, tt * 2:tt * 2 + 1], mv[:, tt * 2:tt * 2 + 1], ALU.mult),
                 r=[("mv", tt)], w=[("ms16", tt)])
            P.op("dve", lambda e: e.scalar_tensor_tensor(ms16[:, tt:tt + 1], ms16[:, tt:tt + 1], EPS, mv[:, tt * 2 + 1:tt * 2 + 2], ALU.add, ALU.add),
                 r=[("mv", tt), ("ms16", tt)], w=[("ms16", tt)])
            P.op("pool", lambda e: e.tensor_tensor(rstd16[:, tt:tt + 1], ms16[:, tt:tt + 1], cf[:, CF_PW:CF_PW + 1], ALU.pow),
                 r=[("ms16", tt), "cf"], w=[("rstd16", tt)])

        def norm_front(L, tt):
            rms_tile(tt)
            b = tt % 2
            hb = PT[:, b * 1024:(b + 1) * 1024]
            P.op("act", lambda e: e.activation(hb, xres[:, tt * D:(tt + 1) * D], AF.Copy, scale=rstd16[:, tt:tt + 1]),
                 r=[("xres", tt), ("rstd16", tt)], w=[("hb", b)])

        def norm_back(L, tt):
            b = tt % 2
            hb = PT[:, b * 1024:(b + 1) * 1024]
            bk = 6 + b

            def tr(e):
                for c in range(8):
                    ins = e.transpose(Bb[bk][:, c * 128:(c + 1) * 128], hb[:, c * 128:(c + 1) * 128], ident)
                return ins
            P.op("pe", tr, r=[("hb", b), "cb"], w=[("B", bk)])
            def ev(e):
                for c in range(8):
                    ins = e.activation(hT[:, c * S + tt * 128:c * S + (tt + 1) * 128], Bb[bk][:, c * 128:(c + 1) * 128], AF.Copy,
                                       scale=cf[:, CF_LNW + L * 8 + c:CF_LNW + L * 8 + c + 1])
                return ins
            P.op("act", ev, r=[("B", bk), "cf"], w=[("hT", tt)])

        def phaseN(L):
            P.retire(K("PTm", 0, 4) + ["tg0", "tg1"], K("hb", 0, 2))
            for tt in range(NT + 2):
                if tt >= 2:
                    norm_back(L, tt - 2)
                if tt < NT:
                    norm_front(L, tt)

        def loadRA(L, h):
            load_w([(win_d, L, C_RK + 128 * h, 128, 0, 512), (win_d, L, C_RV + 256 * h, 256, 128, 512),
                    (win_d, L, C_RQ + 128 * h, 128, 384, 512)], K("wsl", 0, 2), "wA")
            P.dma("sp", lambda e: [e.dma_start(out=qksc[:], in_=qk_d.ap()[h])], "qksc", 1, w=["qksc"])

        def loadRB(L, h):
            sB = 2 + h % 2
            load_w([(win_d, L, C_RG + 256 * h, 256, sB * 2048, 256)], [("wsl", sB)], f"wB{sB}")

        def loadR(L, h):
            loadRA(L, h)
            loadRB(L, h)

        def vT_ap(j, lo, n):
            return yT[:, j * S + lo:j * S + lo + n]

        def uT_ap(j, lo, n):
            return yT[:, 4096 + j * S + lo:4096 + j * S + lo + n]

        def kvt_ap(c, lo, n):
            return yT[:, 8192 + c * 384 + lo:8192 + c * 384 + lo + n]

        def rtg_ap(i):
            return yT[:, 14336 + i * 512:14336 + (i + 1) * 512]

        def insb_ap(i):
            return yT[:, 15360 + i * 128:15360 + (i + 1) * 128]

        def on_ap(i):
            return yT[:, 15616 + i * 256:15616 + (i + 1) * 256]

        def sbf_ap(c):
            return vtok[:, c * 256:(c + 1) * 256] if c < 8 else PT[:, (c - 8) * 256:(c - 7) * 256]
        R_S = rl[:, 0:256]
        RKEYS_Y = (K("vT", 0, 16) + K("uT", 0, 16) + K("kvt", 0, 16) + ["rtg0", "rtg1"] + K("insb", 0, 2) + K("on", 0, 3))

        def phaseRall(L):
            P.retire(K("hb", 0, 2) + K("PTm", 0, 4) + ["tg0", "tg1"], K("sbf", 8, 15))
            P.retire(K("vtokm", 0, 16), K("sbf", 0, 8))
            P.retire(K("yT", 0, 16), RKEYS_Y)
            P.retire(["rl"], K("Sst", 0, 2))
            pj = [0]
            rcnt = [0]

            def proj_tile(h, T, kind):
                sB = 2 + h % 2
                gnw0 = CF_GNW + L * 8 + 2 * h
                if kind == "q":
                    wfn, wkeys = (lambda cc: cc * 512 + 384), K("wsl", 0, 2)
                elif kind == "k":
                    wfn, wkeys = (lambda cc: cc * 512), K("wsl", 0, 2)
                elif kind in ("v0", "v1"):
                    j = int(kind[1])
                    wfn, wkeys = (lambda cc: cc * 512 + 128 + j * 128), K("wsl", 0, 2)
                else:
                    j = int(kind[1])
                    wfn, wkeys = (lambda cc: sB * 2048 + cc * 256 + j * 128), [("wsl", sB)]
                a = pj[0] % 3
                pj[0] += 1

                def mm(e):
                    for cc in range(8):
                        w0 = wfn(cc)
                        ins = e.matmul(B[a][:, 0:512], wsl[:, w0:w0 + 128],
                                       hT[:, cc * S + T * 512:cc * S + T * 512 + 512], start=(cc == 0), stop=(cc == 7))
                    return ins
                P.op("pe", mm, r=wkeys + K("hT", 4 * T, 4 * T + 4), w=[("B", a)])
                if kind in ("q", "k"):
                    which = 0 if kind == "q" else 1
                    dst = qT if which == 0 else kT
                    dkey = "qT" if which == 0 else "kT"
                    P.op("dve", lambda e: e.tensor_tensor(
                        sb_ap(dst, T * 512, [[128, 4], [1, 128]]),
                        B[a][:, 0:512].rearrange("p (c n) -> p c n", c=4),
                        sb_ap(qksc, which * 128, [[0, 4], [1, 128]]), ALU.mult),
                        r=[("B", a), "qksc"], w=K(dkey, 4 * T, 4 * T + 4))
                elif kind in ("v0", "v1"):
                    P.op("act", lambda e: e.activation(vT_ap(j, T * 512, 512), B[a][:, 0:512], AF.Copy),
                         r=[("B", a)], w=K("vT", 4 * T, 4 * T + 4))
                else:
                    ri = rcnt[0] % 2
                    rcnt[0] += 1
                    P.op("act", lambda e: e.activation(rtg_ap(ri), B[a][:, 0:512], AF.Tanh, scale=0.5),
                         r=[("B", a)], w=[f"rtg{ri}"])
                    P.op("dve", lambda e: e.scalar_tensor_tensor(uT_ap(j, T * 512, 512), rtg_ap(ri), 1.0, B[a][:, 0:512], ALU.add, ALU.mult),
                         r=[("B", a), f"rtg{ri}"], w=K("uT", 4 * T, 4 * T + 4))
                    P.op("act", lambda e: e.activation(uT_ap(j, T * 512, 512), uT_ap(j, T * 512, 512), AF.Copy,
                                                        scale=cf[:, gnw0 + j:gnw0 + j + 1]),
                         r=K("uT", 4 * T, 4 * T + 4) + ["cf"], w=K("uT", 4 * T, 4 * T + 4))

            def st1(h, pr):
                def trkv(e):
                    for q in range(2):
                        c = 2 * pr + q
                        e.transpose(Bb[3][:, q * 384:q * 384 + 128], kT[:, c * 128:(c + 1) * 128], ident)
                        e.transpose(Bb[3][:, q * 384 + 128:q * 384 + 256], vT_ap(0, c * 128, 128), ident)
                        ins = e.transpose(Bb[3][:, q * 384 + 256:q * 384 + 384], vT_ap(1, c * 128, 128), ident)
                    return ins
                P.op("pe", trkv, r=K("kT", 2 * pr, 2 * pr + 2) + K("vT", 2 * pr, 2 * pr + 2) + ["cb"], w=[("B", 3)])
                P.op("act", lambda e: e.activation(kvt_ap(2 * pr, 0, 768), Bb[3][:, 0:768], AF.Copy),
                     r=[("B", 3)], w=K("kvt", 2 * pr, 2 * pr + 2))

            def st2A1(h2, c2, hA, cA):
                do2 = c2 is not None and c2 < NT - 1
                doA = cA is not None
                if not (do2 or doA):
                    return
                r = []
                if do2:
                    r += [("kvt", c2)]
                if doA:
                    r += [("kT", cA), ("qT", cA)]

                def mm(e):
                    ins = None
                    if do2:
                        ins = e.matmul(B[4][:, 0:256], kvt_ap(c2, 0, 128), kvt_ap(c2, 128, 256), start=True, stop=True)
                    if doA:
                        tok = slice(cA * 128, (cA + 1) * 128)
                        ins = e.matmul(B[4][:, 256:384], kT[:, tok], qT[:, tok], start=True, stop=True)
                    return ins
                P.op("pe", mm, r=r, w=[("B", 4)])
                if do2:
                    g2 = GAMMA[h2]
                    Sc = rl[:, (c2 % 2) * 256:(c2 % 2) * 256 + 256]
                    Sp = rl[:, ((c2 + 1) % 2) * 256:((c2 + 1) % 2) * 256 + 256]
                    if c2 == 0:
                        P.op("dve", lambda e: e.tensor_copy(Sc, B[4][:, 0:256]), r=[("B", 4)], w=[("Sst", c2 % 2)])
                    else:
                        P.op("dve", lambda e: e.scalar_tensor_tensor(Sc, Sp, float(g2 ** 128.0), B[4][:, 0:256], ALU.mult, ALU.add),
                             r=[("B", 4), ("Sst", (c2 + 1) % 2)], w=[("Sst", c2 % 2)])
                if doA:
                    gA = GAMMA[hA]
                    i2 = cA % 2
                    P.op("dve", lambda e: e.scalar_tensor_tensor(insb_ap(i2), B[4][:, 256:384], float(gA ** -128.0), cb[:, CB_TRI01:CB_TRI01 + 128], ALU.mult, ALU.mult),
                         r=[("B", 4), "cb"], w=[("insb", i2)])
                if do2:
                    P.op("dve", lambda e: e.tensor_copy(sbf_ap(c2), Sc), r=[("Sst", c2 % 2)], w=[("sbf", c2)])

            def stA2(h, c):
                tok = slice(c * 128, (c + 1) * 128)
                i2 = c % 2
                ob = 5 + c % 2

                def mmo(e):
                    if c > 0:
                        e.matmul(B[ob][:, 0:256], qT[:, tok], sbf_ap(c - 1), start=True, stop=False)
                    return e.matmul(B[ob][:, 0:256], insb_ap(i2), kvt_ap(c, 128, 256), start=(c == 0), stop=True)
                P.op("pe", mmo, r=[("qT", c), ("insb", i2), ("kvt", c)] + ([("sbf", c - 1)] if c > 0 else []), w=[("B", ob)])

            def stB1(h, c):
                ob = 5 + c % 2
                g3 = c % 3
                g0 = g3 * 16
                P.op("dve", lambda e: e.bn_stats(gsm[:, g0:g0 + 6], B[ob][:, 0:256]), r=[("B", ob)], w=[("gsm", g3)])
                P.op("dve", lambda e: e.bn_aggr(gsm[:, g0 + 6:g0 + 8], gsm[:, g0:g0 + 6]), r=[("gsm", g3)], w=[("gsm", g3)])
                P.op("act", lambda e: e.activation(ocp[:, g3 * 256:(g3 + 1) * 256], B[ob][:, 0:256], AF.Copy),
                     r=[("B", ob)], w=[("ocp", g3)])
                P.op("pool", lambda e: e.tensor_scalar(gsm[:, g0 + 8:g0 + 9], gsm[:, g0 + 7:g0 + 8], EPS, 4.0, ALU.add, ALU.mult),
                     r=[("gsm", g3)], w=[("gsm", g3)])
                P.op("pool", lambda e: e.tensor_tensor(gsm[:, g0 + 9:g0 + 10], gsm[:, g0 + 8:g0 + 9], cf[:, CF_PW:CF_PW + 1], ALU.pow),
                     r=[("gsm", g3), "cf"], w=[("gsm", g3)])
                P.op("pool", lambda e: e.tensor_scalar(gsm[:, g0 + 10:g0 + 11], gsm[:, g0 + 6:g0 + 7], -1.0, gsm[:, g0 + 9:g0 + 10], ALU.mult, ALU.mult),
                     r=[("gsm", g3)], w=[("gsm", g3)])

            def stB2a(h, c):
                ob = 5 + c % 2
                g3 = c % 3
                g0 = g3 * 16
                P.op("act", lambda e: e.activation(on_ap(g3), ocp[:, g3 * 256:(g3 + 1) * 256], AF.Identity,
                                                    bias=gsm[:, g0 + 10:g0 + 11], scale=gsm[:, g0 + 9:g0 + 10]),
                     r=[("ocp", g3), ("gsm", g3)], w=[("on", g3)])

            def stB2b(h, c):
                g3 = c % 3

                def trr(e):
                    e.transpose(Bb[7][:, 0:128], on_ap(g3)[:, 0:128], ident)
                    return e.transpose(Bb[7][:, 128:256], on_ap(g3)[:, 128:256], ident)
                P.op("pe", trr, r=[("on", g3), "cb"], w=[("B", 7)])

            def stB3(h, c):
                P.op("dve", lambda e: e.tensor_tensor(
                    sb_ap(GT, 2 * h * S + c * 128, [[S, 2], [1, 128]]),
                    Bb[7][:, 0:256].rearrange("p (j n) -> p j n", j=2),
                    sb_ap(yT, 4096 + c * 128, [[S, 2], [1, 128]]), ALU.mult),
                    r=[("B", 7), ("uT", c)], w=[("GT", c)])

            NG = RET_H * NT
            PROJ_ORDER = [("q", "k"), ("v0", "v1"), ("r0",), ("r1",)]
            stages = [(9, stA2), (10, stB1), (11, stB2a), (12, stB2b), (13, stB3)]
            for gstep in range(NG + 14):
                for (dly, fn) in reversed(stages):
                    gc = gstep - dly
                    if 0 <= gc < NG:
                        fn(gc // NT, gc % NT)
                g2, gA = gstep - 6, gstep - 8
                st2A1(g2 // NT if 0 <= g2 < NG else None, g2 % NT if 0 <= g2 < NG else None,
                      gA // NT if 0 <= gA < NG else None, gA % NT if 0 <= gA < NG else None)
                if gstep % 2 == 0:
                    gc = gstep - 4
                    if 0 <= gc < NG:
                        st1(gc // NT, (gc % NT) // 2)
                if gstep < NG:
                    h, cc_ = gstep // NT, gstep % NT
                    for kind in PROJ_ORDER[cc_ % 4]:
                        proj_tile(h, cc_ // 4, kind)
                    if cc_ == 0 and h + 1 < RET_H:
                        loadRB(L, h + 1)
                    if cc_ == 13 and h + 1 < RET_H:
                        loadRA(L, h + 1)

        def loadXO(L, eb, wd, gcol):
            p = eb % 2
            load_w([(wd, L, eb * 256, 256, (2 * p) * 2048, 256)], [("wsl", 2 * p)], f"wX{2 * p}")
            load_w([(win_d, L, gcol + eb * 256, 256, (2 * p + 1) * 2048, 256)], [("wsl", 2 * p + 1)], f"wX{2 * p + 1}")

        def phaseXO(L, eb, first):
            p = eb % 2
            if eb == 0:
                if first:
                    P.retire(K("sbf", 8, 15), ["tg0", "tg1"])
                    P.retire(RKEYS_Y, K("yT", 0, 16))
                else:
                    P.retire(K("PTm", 0, 4), ["tg0", "tg1"])
            for ec in range(2):
                echunk = 2 * eb + ec
                for T in range(4):
                    aa = nextA()

                    def mma(e, aa=aa, T=T, ec=ec):
                        for vc_ in range(8):
                            ins = e.matmul(B[aa][:, 0:512], wsl[:, (2 * p) * 2048 + vc_ * 256 + ec * 128:(2 * p) * 2048 + vc_ * 256 + ec * 128 + 128],
                                           GT[:, vc_ * S + T * 512:vc_ * S + T * 512 + 512], start=(vc_ == 0), stop=(vc_ == 7))
                        return ins
                    P.op("pe", mma, r=[("wsl", 2 * p)] + K("GT", 4 * T, 4 * T + 4), w=[("B", aa)])
                    ag = nextA()

                    def mmg(e, ag=ag, T=T, ec=ec):
                        for cc in range(8):
                            ins = e.matmul(B[ag][:, 0:512], wsl[:, (2 * p + 1) * 2048 + cc * 256 + ec * 128:(2 * p + 1) * 2048 + cc * 256 + ec * 128 + 128],
                                           hT[:, cc * S + T * 512:cc * S + T * 512 + 512], start=(cc == 0), stop=(cc == 7))
                        return ins
                    P.op("pe", mmg, r=[("wsl", 2 * p + 1)] + K("hT", 4 * T, 4 * T + 4), w=[("B", ag)])
                    tb = (ec * 4 + T) % 2
                    tg = PT[:, tb * 512:(tb + 1) * 512]
                    tkey = f"tg{tb}"
                    P.op("act", lambda e, ag=ag, tg=tg: e.activation(tg, B[ag][:, 0:512], AF.Tanh, scale=0.5),
                         r=[("B", ag)], w=[tkey])
                    ydst = yT[:, echunk * S + T * 512:echunk * S + T * 512 + 512]
                    if first:
                        P.op("dve", lambda e, aa=aa, tg=tg, ydst=ydst: e.scalar_tensor_tensor(ydst, tg, 1.0, B[aa][:, 0:512], ALU.add, ALU.mult),
                             r=[("B", aa), tkey], w=K("yT", 4 * T, 4 * T + 4))
                    else:
                        P.op("dve", lambda e, aa=aa, tg=tg: e.scalar_tensor_tensor(rl[:], tg, 1.0, B[aa][:, 0:512], ALU.add, ALU.mult),
                             r=[("B", aa), tkey], w=["rl"])
                        P.op("dve", lambda e, ydst=ydst: e.tensor_tensor(ydst, rl[:], ydst, ALU.add),
                             r=["rl"] + K("yT", 4 * T, 4 * T + 4), w=K("yT", 4 * T, 4 * T + 4))

        def loadM(L, h):
            p = h % 2
            load_w([(win_d, L, C_MQ + 128 * h, 128, (2 * p) * 2048, 256), (win_d, L, C_MK + 128 * h, 128, (2 * p) * 2048 + 128, 256)],
                   [("wsl", 2 * p)], f"wX{2 * p}")
            load_w([(win_d, L, C_MV + 128 * h, 128, (2 * p + 1) * 2048, 256), (win_d, L, C_MG + 128 * h, 128, (2 * p + 1) * 2048 + 128, 256)],
                   [("wsl", 2 * p + 1)], f"wX{2 * p + 1}")

        def phaseM(L, h):
            p = h % 2
            sQK = (2 * p) * 2048
            sVG = (2 * p + 1) * 2048
            if h == 0:
                P.retire(["tg0", "tg1"], K("PTm", 0, 4))
                P.retire(K("sbf", 0, 8), K("vtokm", 0, 16))
                P.retire(K("Sst", 0, 2), ["rl"])
            for T in range(4):
                a = nextA()

                def mmq(e, a=a, T=T):
                    for cc in range(8):
                        ins = e.matmul(B[a][:, 0:512], wsl[:, sQK + cc * 256:sQK + cc * 256 + 128],
                                       hT[:, cc * S + T * 512:cc * S + T * 512 + 512], start=(cc == 0), stop=(cc == 7))
                    return ins
                P.op("pe", mmq, r=[("wsl", 2 * p)] + K("hT", 4 * T, 4 * T + 4), w=[("B", a)])
                P.op("act", lambda e, a=a, T=T: e.activation(qT[:, T * 512:(T + 1) * 512], B[a][:, 0:512], AF.Copy),
                     r=[("B", a)], w=K("qT", 4 * T, 4 * T + 4))
                a2 = nextA()

                def mmk(e, a2=a2, T=T):
                    for cc in range(8):
                        ins = e.matmul(B[a2][:, 0:512], wsl[:, sQK + cc * 256 + 128:sQK + cc * 256 + 256],
                                       hT[:, cc * S + T * 512:cc * S + T * 512 + 512], start=(cc == 0), stop=(cc == 7))
                    return ins
                P.op("pe", mmk, r=[("wsl", 2 * p)] + K("hT", 4 * T, 4 * T + 4), w=[("B", a2)])
                P.op("act", lambda e, a2=a2, T=T: e.activation(kT[:, T * 512:(T + 1) * 512], B[a2][:, 0:512], AF.Copy),
                     r=[("B", a2)], w=K("kT", 4 * T, 4 * T + 4))
                P.op("dve", lambda e, a2=a2, T=T: e.tensor_reduce(ksum[:, 2 * T:2 * T + 2], B[a2][:, 0:512].rearrange("p (b n) -> p b n", b=2), AX.X, ALU.add),
                     r=[("B", a2)], w=["ksum"])
            for T in range(4):
                a = nextA()

                def mmv(e, a=a, T=T):
                    for cc in range(8):
                        ins = e.matmul(B[a][:, 0:512], wsl[:, sVG + cc * 256:sVG + cc * 256 + 128],
                                       hT[:, cc * S + T * 512:cc * S + T * 512 + 512], start=(cc == 0), stop=(cc == 7))
                    return ins
                P.op("pe", mmv, r=[("wsl", 2 * p + 1)] + K("hT", 4 * T, 4 * T + 4), w=[("B", a)])
                P.op("act", lambda e, a=a: e.activation(tmg[:], B[a][:, 0:512], AF.Copy), r=[("B", a)], w=["tmg"])
                bk = 6 if T % 2 == 0 else 7

                def trv(e, bk=bk):
                    for i in range(4):
                        ins = e.transpose(Bb[bk][:, i * 128:(i + 1) * 128], tmg[:, i * 128:(i + 1) * 128], ident)
                    return ins
                P.op("pe", trv, r=["tmg", "cb"], w=[("B", bk)])
                P.op("dve", lambda e, bk=bk, T=T: e.tensor_copy(vtok[:, T * 512:(T + 1) * 512], Bb[bk][:, 0:512]),
                     r=[("B", bk)], w=K("vtokm", 4 * T, 4 * T + 4))
            def gate1():
                P.op("dve", lambda e: e.tensor_copy(kmh[:], ksum[:]), r=["ksum"], w=["kmh"])
                P.op("dve", lambda e: e.tensor_tensor(kml[:], ksum[:], kmh[:], ALU.subtract), r=["ksum", "kmh"], w=["kml"])

                def mmgate(e):
                    for i in range(8):
                        tt = 8 + i
                        e.matmul(B[7][:, i * 8:(i + 1) * 8], qT[:, tt * 128:(tt + 1) * 128], kmh[:], start=True, stop=False)
                        ins = e.matmul(B[7][:, i * 8:(i + 1) * 8], qT[:, tt * 128:(tt + 1) * 128], kml[:], start=False, stop=True)
                    return ins
                P.op("pe", mmgate, r=K("qT", 8, 16) + ["kmh", "kml"], w=[("B", 7)])
                P.op("dve", lambda e: e.tensor_tensor(gm[:], B[7][:, 0:64], cf[:, CF_NEGM:CF_NEGM + 64], ALU.add),
                     r=[("B", 7), "cf"], w=["gm"])
                for i in range(8):
                    P.op("dve", lambda e, i=i: e.max(m8[:, i * 8:(i + 1) * 8], gm[:, i * 8:(i + 1) * 8]), r=["gm"], w=["m8"])
                P.op("dve", lambda e: e.tensor_tensor(sb_ap(nmb, 0, [[8, 8], [1, 8]]), sb_ap(gm, 0, [[8, 8], [1, 8]]),
                                                       sb_ap(m8, 2, [[8, 8], [0, 8]]), ALU.is_lt),
                     r=["gm", "m8"], w=["nmb"])


            def gate2():
                def trm(e):
                    for i in range(8):
                        ins = e.transpose(Bb[7][0:8, i * 128:(i + 1) * 128], nmb[:, i * 8:(i + 1) * 8], ident)
                    return ins
                P.op("pe", trm, r=["nmb", "cb"], w=[("B", 7)])
                P.op("dve", lambda e: e.tensor_copy(nmT[0:8, :], Bb[7][0:8, 0:1024]), r=[("B", 7)], w=["nmT"])


            def t_mm(T):
                def mmmg(e):
                    for cc in range(8):
                        ins = e.matmul(B[7][:, 0:512], wsl[:, sVG + cc * 256 + 128:sVG + cc * 256 + 256],
                                       hT[:, cc * S + T * 512:cc * S + T * 512 + 512], start=(cc == 0), stop=(cc == 7))
                    return ins
                P.op("pe", mmmg, r=[("wsl", 2 * p + 1)] + K("hT", 4 * T, 4 * T + 4), w=[("B", 7)])

            def t_act(T):
                P.op("act", lambda e: e.activation(tmg[:], B[7][:, 0:512], AF.Tanh, scale=0.5), r=[("B", 7)], w=["tmg"])
                P.op("dve", lambda e: e.scalar_tensor_tensor(um[:], tmg[:], 1.0, B[7][:, 0:512], ALU.add, ALU.mult),
                     r=[("B", 7), "tmg"], w=["um"])

            def rec_qk(T, stl):
                i = stl - 4 * T
                c0 = 128 * i if i >= 0 else 0
                j = stl // 2
                a = nextA()
                need_mask = (T >= 2) and (j <= 2 * T)
                cm0 = 0 if j < 2 * T else 256

                def mms(e):
                    last = not (i >= 0 or need_mask)
                    ins = e.matmul(B[a][:, c0:512], kT[:, stl * 128:(stl + 1) * 128], qT[:, T * 512 + c0:T * 512 + 512],
                                   start=True, stop=last)
                    if i >= 0:
                        ins = e.matmul(B[a][:, c0:c0 + 128], ident, cb[:, CB_NEGTRI:CB_NEGTRI + 128],
                                       start=False, stop=not need_mask)
                    if need_mask:
                        ins = e.matmul(B[a][:, cm0:512], cb[:, CB_NEGSEL + j * 128:CB_NEGSEL + (j + 1) * 128],
                                       nmT[:, T * 512 - 1024 + cm0:T * 512 - 1024 + 512], start=False, stop=True)
                    return ins
                P.op("pe", mms, r=[("kT", stl)] + K("qT", 4 * T, 4 * T + 4) + ["cb", "nmT"], w=[("B", a)])
                return a

            def rec_exp(T, stl, a):
                pb = stl % 4
                for (lo, hi, bi) in BIAS_PLAN[(h, T, stl)]:
                    P.op("act", lambda e, lo=lo, hi=hi, bi=bi: e.activation(
                        PT[:, pb * 512 + lo:pb * 512 + hi], B[a][:, lo:hi], AF.Exp,
                        bias=cf[:, CF_ABIAS + bi:CF_ABIAS + bi + 1], scale=SCALE),
                        r=[("B", a), "cf"], w=[("PTm", pb)])

            def rec_pv(T, stl):
                i = stl - 4 * T
                c0 = 128 * i if i >= 0 else 0
                pb = stl % 4
                ob = 3 + T % 2
                lb = 5 + T % 2
                nst = 4 * (T + 1)

                def mmpv(e):
                    e.matmul(B[ob][:, c0:512], vtok[:, stl * 128:(stl + 1) * 128], PT[:, pb * 512 + c0:pb * 512 + 512],
                             start=(stl == 0), stop=(stl == nst - 1))
                    return e.matmul(B[lb][:, c0:512], cb[:, CB_ONES2:CB_ONES2 + 128], PT[:, pb * 512 + c0:pb * 512 + 512],
                                    start=(stl == 0), stop=(stl == nst - 1))
                P.op("pe", mmpv, r=[("PTm", pb), ("vtokm", stl), "cb"], w=[("B", ob), ("B", lb)])

            def epilogue(T):
                ob = 3 + T % 2
                lb = 5 + T % 2
                P.op("dve", lambda e: e.reciprocal(rl[:], B[lb][:, 0:512]), r=[("B", lb)], w=["rl"])
                P.op("dve", lambda e: e.tensor_tensor(rl[:], rl[:], um[:], ALU.mult), r=["rl", "um"], w=["rl"])
                P.op("dve", lambda e: e.tensor_tensor(GT[:, h * S + T * 512:h * S + T * 512 + 512], B[ob][:, 0:512], rl[:], ALU.mult),
                     r=[("B", ob), "rl"], w=K("GT", 4 * T, 4 * T + 4))

            tiles = [(T, stl) for T in range(4) for stl in range(4 * (T + 1))]
            banks = {}
            for i in range(2):
                banks[i] = rec_qk(*tiles[i])
            t_mm(0)
            for i, (T, stl) in enumerate(tiles):
                if stl == 0:
                    t_act(T)
                    if T == 1:
                        gate1()
                if T == 1 and stl == 6:
                    gate2()
                if stl == 4 * (T + 1) - 2 and T + 1 < 4:
                    t_mm(T + 1)
                rec_exp(T, stl, banks[i])
                if i + 2 < len(tiles):
                    banks[i + 2] = rec_qk(*tiles[i + 2])
                rec_pv(T, stl)
                if stl == 4 * (T + 1) - 1:
                    epilogue(T)

        def loadO(L):
            for s in range(4):
                load_w([(wout_d, L, s * 256, 256, s * 2048, 256)], [("wsl", s)], f"wX{s}")

        def phaseO(L):
            last = (L == n_layers - 1)
            if not last:
                P.retire(K("PTm", 0, 4) + ["tg0", "tg1"], K("hb", 0, 2))
            else:
                P.dma("sp", lambda e: [e.dma_start(out=hTf[:, 0:D], in_=fnw_d.ap())], "fnw", 1, r=[], w=K("hT", 0, 16))
            for tt in range(NT):
                for half in range(2):
                    a = nextA()

                    def mmo(e, a=a, tt=tt, half=half):
                        for q in range(2):
                            s = 2 * half + q
                            for ec in range(8):
                                ins = e.matmul(B[a][:, q * 256:(q + 1) * 256], yT[:, ec * S + tt * 128:ec * S + tt * 128 + 128],
                                               wsl[:, s * 2048 + ec * 256:s * 2048 + ec * 256 + 256], start=(ec == 0), stop=(ec == 7))
                        return ins
                    P.op("pe", mmo, r=K("wsl", 2 * half, 2 * half + 2) + [("yT", tt)], w=[("B", a)])
                    xs = xres[:, tt * D + half * 512:tt * D + half * 512 + 512]
                    P.op("dve", lambda e, a=a, xs=xs: e.scalar_tensor_tensor(xs, B[a][:, 0:512], 0.5, xs, ALU.mult, ALU.add),
                         r=[("B", a), ("xres", tt)], w=[("xres", tt)])
                if not last:
                    if tt >= 2:
                        norm_back(L + 1, tt - 2)
                    norm_front(L + 1, tt)
                else:
                    final_tile(tt)
            if not last:
                norm_back(L + 1, NT - 2)
                norm_back(L + 1, NT - 1)
            if last:
                P.final_waits = [("sp", "out0"), ("sp", "out1")]

        def final_tile(tt):
            b = tt % 2
            stg = hTf[:, D + b * D:D + (b + 1) * D]
            if final_norm:
                rms_tile(tt)
                P.op("dve", lambda e: e.scalar_tensor_tensor(stg, xres[:, tt * D:(tt + 1) * D], rstd16[:, tt:tt + 1], hTf[:, 0:D], ALU.mult, ALU.mult),
                     r=[("xres", tt), ("rstd16", tt)] + K("hT", 0, 16), w=[("stg", b)])
                P.dma("sp", lambda e: [e.dma_start(out=out_d.ap()[tt * 128:(tt + 1) * 128, :], in_=stg)],
                      f"out{b}", 1, r=[("stg", b)])
            else:
                P.dma("sp", lambda e: [e.dma_start(out=out_d.ap()[tt * 128:(tt + 1) * 128, :], in_=xres[:, tt * D:(tt + 1) * D])],
                      f"out{b}", 1, r=[("xres", tt)])

        units = []
        for L in range(n_layers):
            if L == 0:
                units.append((set(), None, lambda L=L: phaseN(L), L))
            units.append(({0, 1, 2, 3}, (lambda L=L: loadR(L, 0)), (lambda L=L: phaseRall(L)), L))
            for eb in range(4):
                units.append(({2 * (eb % 2), 2 * (eb % 2) + 1}, (lambda L=L, eb=eb: loadXO(L, eb, wro_d, C_GR)),
                              (lambda L=L, eb=eb: phaseXO(L, eb, True)), L))
            for h in range(MOBA_H):
                units.append(({2 * (h % 2), 2 * (h % 2) + 1}, (lambda L=L, h=h: loadM(L, h)), (lambda L=L, h=h: phaseM(L, h)), L))
            for eb in range(4):
                units.append(({2 * (eb % 2), 2 * (eb % 2) + 1}, (lambda L=L, eb=eb: loadXO(L, eb, wmo_d, C_GM)),
                              (lambda L=L, eb=eb: phaseXO(L, eb, False)), L))
            units.append(({0, 1, 2, 3}, (lambda L=L: loadO(L)), (lambda L=L: phaseO(L)), L))
        loaded = [False] * len(units)
        for i, (slots, ld, comp, L) in enumerate(units):
            P.epoch = L
            if ld is not None and not loaded[i]:
                ld()
                loaded[i] = True
            busy = set(slots)
            for k in range(i + 1, min(i + 3, len(units))):
                s2, ld2, _, _ = units[k]
                if ld2 is None:
                    continue
                if loaded[k]:
                    busy |= s2
                    continue
                if s2 & busy:
                    break
                ld2()
                loaded[k] = True
                busy |= s2
            comp()
        P.emit()
    return nc


_CACHE = {}


def _get_prog(n_layers, final_norm):
    key = (n_layers, final_norm)
    if key not in _CACHE:
        _CACHE[key] = build(n_layers, final_norm)
    return _CACHE[key]


def kernel(x, ln_w, w_in, ret_gn_w, w_ret_o, w_moba_o, w_out, final_norm_w):
    x = np.ascontiguousarray(np.asarray(x, dtype=np.float32))
    ln_w = np.asarray(ln_w, dtype=np.float32)
    ret_gn_w = np.asarray(ret_gn_w, dtype=np.float32)
    w_in = np.ascontiguousarray(np.asarray(w_in, dtype=np.float32))
    w_ret_o = np.ascontiguousarray(np.asarray(w_ret_o, dtype=np.float32))
    w_moba_o = np.ascontiguousarray(np.asarray(w_moba_o, dtype=np.float32))
    w_out = np.ascontiguousarray(np.asarray(w_out, dtype=np.float32))
    fnw = np.ascontiguousarray(np.broadcast_to(np.asarray(final_norm_w, dtype=np.float32)[None, :], (128, D)))
    nL = w_in.shape[0]
    cf, cb, qk = host_consts(ln_w, ret_gn_w, nL, 0)
    nc = _get_prog(nL, True)
    ncores = x.shape[0]
    in_maps = [{"x": x[b], "w_in": w_in, "w_ro": w_ret_o, "w_mo": w_moba_o, "w_out": w_out,
                "cf": cf, "cb": cb, "qksc": qk, "fnw": fnw} for b in range(ncores)]
    res = run_bass_kernel_spmd(nc, in_maps, core_ids=list(range(ncores)))
    return np.stack([np.asarray(r["out"], dtype=np.float32) for r in res.results], axis=0)
```

```python
from contextlib import ExitStack
import numpy as np
import concourse.bass as bass
import concourse.mybir as mybir
from concourse.bass_utils import run_bass_kernel_spmd

F32 = mybir.dt.float32
BF16 = mybir.dt.bfloat16
ALU = mybir.AluOpType
AF = mybir.ActivationFunctionType
AX = mybir.AxisListType

ENGS = ("pe", "act", "dve", "pool", "sp")


class _Op:
    __slots__ = ("eng", "fn", "deps", "mark", "rank", "kind", "stream", "ndma", "epoch", "idx")


class Prog:
    def __init__(self, nc):
        self.nc = nc
        self.ops = {e: [] for e in ENGS}
        self.lastw = {}
        self.readers = {}
        self.stream_cnt = {}
        self.epoch = 0
        self.final_waits = []

    def _deps(self, r, w):
        deps = set()
        for k in r:
            t = self.lastw.get(k)
            if t is not None:
                deps.add(t + ("raw",))
        for k in w:
            t = self.lastw.get(k)
            if t is not None:
                deps.add(t + ("waw",))
            for t in self.readers.get(k, ()):
                deps.add(t + ("war",))
        return deps

    def _commit(self, tok, r, w):
        for k in r:
            self.readers.setdefault(k, []).append(tok)
        for k in w:
            self.lastw[k] = tok
            self.readers[k] = []

    def retire(self, old_keys, new_keys):
        toks = []
        for k in old_keys:
            t = self.lastw.get(k)
            if t is not None:
                toks.append(t)
            toks.extend(self.readers.get(k, ()))
        toks = list(dict.fromkeys(toks))
        for k in new_keys:
            cur = list(self.readers.get(k, ()))
            t = self.lastw.get(k)
            if t is not None:
                cur.append(t)
            self.lastw[k] = None
            self.readers[k] = list(dict.fromkeys(cur + toks))

    def op(self, eng, fn, r=(), w=()):
        o = _Op()
        o.eng, o.fn, o.kind, o.mark, o.epoch = eng, fn, "c", False, self.epoch
        o.deps = self._deps(r, w)
        xk = [("Bx", k[1]) for k in list(r) + list(w) if isinstance(k, tuple) and k[0] == "B"] if eng != "pe" else []
        for k in xk:
            t = self.lastw.get(k)
            if t is not None:
                o.deps.add(t + ("x",))
        o.idx = len(self.ops[eng])
        self.ops[eng].append(o)
        tok = ("e", eng, o.idx)
        self._commit(tok, r, w)
        for k in xk:
            self.lastw[k] = tok
        return o

    def dma(self, q, fn, stream, ndma, r=(), w=()):
        o = _Op()
        o.eng, o.fn, o.kind, o.mark, o.epoch = q, fn, "d", False, self.epoch
        o.stream, o.ndma = stream, ndma
        o.deps = self._deps(r, w)
        o.idx = len(self.ops[q])
        self.ops[q].append(o)
        c = self.stream_cnt.get(stream, 0) + ndma
        self.stream_cnt[stream] = c
        self._commit(("d", stream, c), r, w)
        return o

    def emit(self):
        nc = self.nc
        ops = self.ops
        for e in ENGS:
            for o in ops[e]:
                nd = set()
                for d in o.deps:
                    if d[0] == "e":
                        pe_, idx, kind = d[1], d[2], d[3]
                        if pe_ == e and (e == "pe" or kind == "x"):
                            continue
                        ops[pe_][idx].mark = True
                        nd.add(("e", pe_, idx))
                    else:
                        nd.add(("d", d[1], d[2]))
                o.deps = nd
        nep = self.epoch + 1
        for e in ENGS:
            cnt = [0] * nep
            for o in ops[e]:
                if o.mark:
                    cnt[o.epoch] += 1
                    o.rank = cnt[o.epoch]
        with ExitStack() as st:
            esem = {}
            for e in ("pe", "act", "dve", "pool"):
                for ep in range(nep):
                    esem[(e, ep)] = st.enter_context(nc.semaphore(f"s_{e}_{ep}"))
            ssem = {s: st.enter_context(nc.semaphore(f"d_{s}")) for s in self.stream_cnt}
            block = st.enter_context(nc.Block())

            def run(e, eng):
                waited = {}
                for o in ops[e]:
                    need = {}
                    for d in o.deps:
                        if d[0] == "e":
                            po = ops[d[1]][d[2]]
                            key = ("e", d[1], po.epoch)
                            val = po.rank
                        else:
                            key = ("d", d[1])
                            val = 16 * d[2]
                        if need.get(key, 0) < val:
                            need[key] = val
                    for key, val in need.items():
                        if waited.get(key, 0) >= val:
                            continue
                        waited[key] = val
                        sem = esem[(key[1], key[2])] if key[0] == "e" else ssem[key[1]]
                        eng.wait_ge(sem, val)
                    if o.kind == "c":
                        ins = o.fn(eng)
                        if o.mark:
                            ins.then_inc(esem[(e, o.epoch)], 1)
                    else:
                        lst = o.fn(eng)
                        assert len(lst) == o.ndma, (len(lst), o.ndma)
                        for ins in lst:
                            ins.then_inc(ssem[o.stream], 16)
                for (q, stream) in self.final_waits:
                    if q == e:
                        eng.wait_ge(ssem[stream], 16 * self.stream_cnt[stream])

            @block.tensor
            def _(eng):
                run("pe", eng)

            @block.scalar
            def _(eng):
                run("act", eng)

            @block.vector
            def _(eng):
                run("dve", eng)

            @block.gpsimd
            def _(eng):
                run("pool", eng)

            @block.sync
            def _(eng):
                run("sp", eng)


def sb_ap(t, col, dims, p0=0, npart=128):
    F = 1
    for s in t.shape[1:]:
        F *= s
    return bass.AP(t, p0 * F + col, [[F, npart]] + [list(d) for d in dims])


def K(name, lo, hi):
    return [(name, i) for i in range(lo, hi)]


S = 2048
D = 1024
NT = 16
EPS = 1e-6
RET_H, MOBA_H = 4, 8
C_RQ, C_RK, C_RV, C_RG = 0, 512, 1024, 2048
C_MQ, C_MK, C_MV, C_MG = 3072, 4096, 5120, 6144
C_GR, C_GM = 7168, 8192
DIN = 9216
NEG = -30000.0
GAMMA = [1.0 - 2.0 ** (-5.0 - h) for h in range(RET_H)]
SLOPE = [2.0 ** (-8.0 * (h + 1.0) / MOBA_H) for h in range(MOBA_H)]
SCALE = 128.0 ** -0.5


def _bias_plan():
    table = {}
    plan = {}
    for h in range(MOBA_H):
        for T in range(4):
            for st in range(4 * (T + 1)):
                i = st - 4 * T
                c0 = 128 * i if i >= 0 else 0
                segs = []
                if h == 0:
                    rngs = [(max(c0, 0), 256, 512 * T + 128), (max(c0, 256), 512, 512 * T + 384)]
                else:
                    rngs = [(c0, 512, 512 * T + 256)]
                for lo, hi, ref in rngs:
                    if lo >= hi:
                        continue
                    key = (h, 128 * st - ref)
                    if key not in table:
                        table[key] = len(table)
                    segs.append((lo, hi, table[key]))
                plan[(h, T, st)] = segs
    return plan, table


BIAS_PLAN, BIAS_TABLE = _bias_plan()
NBIAS = len(BIAS_TABLE)

CF_ZCOL = 0
CF_ABIAS = CF_ZCOL + 4
CF_NEGM = CF_ABIAS + NBIAS
CF_PW = CF_NEGM + 64
CF_LNW = CF_PW + 16
CF_GNW = CF_LNW + 32
NCF = CF_GNW + 32
CB_TRI01 = 0
CB_NEGTRI = 128
CB_IDENT = 256
CB_ONES2 = 384
CB_NEGSEL = 512
NCB = CB_NEGSEL + 7 * 128


def host_consts(ln_w, ret_gn_w, n_layers, layer0):
    p = np.arange(128, dtype=np.float64)
    cf = np.zeros((128, NCF), np.float64)
    for h in range(RET_H):
        cf[:, CF_ZCOL + h] = GAMMA[h] ** (127.0 - p) * 128.0 ** -0.5
    for (h, delta), idx in BIAS_TABLE.items():
        cf[:, CF_ABIAS + idx] = SLOPE[h] * (delta + p)
    for i in range(8):
        qb = (8 + i) // 2
        for j in range(8):
            cf[:, CF_NEGM + i * 8 + j] = 0.0 if j < qb else -1e30
    cf[:, CF_PW:CF_PW + 16] = -0.5
    cf = cf.astype(np.float32)
    for l in range(n_layers):
        cf[:, CF_LNW + l * 8:CF_LNW + l * 8 + 8] = ln_w[layer0 + l].reshape(8, 128).T
        cf[:, CF_GNW + l * 8:CF_GNW + l * 8 + 8] = ret_gn_w[layer0 + l].reshape(8, 128).T
    cb = np.zeros((128, NCB), np.float32)
    m = np.arange(128)[:, None]
    n = np.arange(128)[None, :]
    cb[:, CB_TRI01:CB_TRI01 + 128] = (m <= n)
    cb[:, CB_NEGTRI:CB_NEGTRI + 128] = np.where(m > n, NEG, 0.0)
    cb[:, CB_IDENT:CB_IDENT + 128] = (m == n)
    cb[:, CB_ONES2:CB_ONES2 + 128] = 2.0
    for j in range(7):
        cb[j, CB_NEGSEL + j * 128:CB_NEGSEL + (j + 1) * 128] = NEG
    nn = np.arange(128, dtype=np.float64)
    qk = np.zeros((RET_H, 128, 256), np.float32)
    for h in range(RET_H):
        qk[h, :, 0:128] = (GAMMA[h] ** (nn + 1.0))[None, :]
        qk[h, :, 128:256] = (GAMMA[h] ** (127.0 - nn) * 128.0 ** -0.5)[None, :]
    return cf, cb, qk


def build(n_layers=4, final_norm=True):
    nc = bass.Bass("TRN2", target_bir_lowering=False)
    x_d = nc.dram_tensor("x", [S, D], F32, kind="ExternalInput")
    win_d = nc.dram_tensor("w_in", [n_layers, D, DIN], F32, kind="ExternalInput")
    wro_d = nc.dram_tensor("w_ro", [n_layers, D, D], F32, kind="ExternalInput")
    wmo_d = nc.dram_tensor("w_mo", [n_layers, D, D], F32, kind="ExternalInput")
    wout_d = nc.dram_tensor("w_out", [n_layers, D, D], F32, kind="ExternalInput")
    cf_d = nc.dram_tensor("cf", [128, NCF], F32, kind="ExternalInput")
    cb_d = nc.dram_tensor("cb", [128, NCB], F32, kind="ExternalInput")
    qk_d = nc.dram_tensor("qksc", [RET_H, 128, 256], F32, kind="ExternalInput")
    fnw_d = nc.dram_tensor("fnw", [128, D], F32, kind="ExternalInput")
    out_d = nc.dram_tensor("out", [S, D], F32, kind="ExternalOutput")

    with ExitStack() as st:
        def sb(name, shape, dt):
            return st.enter_context(nc.sbuf_tensor(name, shape, dt))

        xres = sb("xres", [128, NT * D], F32)
        hT = sb("hT", [128, 8 * S], BF16)
        GT = sb("GT", [128, 8 * S], BF16)
        yT = sb("yT", [128, 8 * S], BF16)
        wsl = sb("wsl", [128, 4 * 2048], BF16)
        qT = sb("qT", [128, S], BF16)
        kT = sb("kT", [128, S], BF16)
        vtok = sb("vtok", [128, NT * 128], BF16)
        PT = sb("PT", [128, 4 * 512], BF16)
        cf = sb("cf_sb", [128, NCF], F32)
        cb = sb("cb_sb", [128, NCB], BF16)
        qksc = sb("qksc_sb", [128, 256], F32)
        st12 = sb("st12", [128, NT * 12], F32)
        mv = sb("mv", [128, NT * 2], F32)
        ms16 = sb("ms16", [128, 16], F32)
        rstd16 = sb("rstd16", [128, 16], F32)
        gsm = sb("gsm", [128, 48], F32)
        gm = sb("gm", [128, 64], F32)
        ocp = sb("ocp", [128, 3 * 256], BF16)
        m8 = sb("m8", [128, 64], F32)
        nmb = sb("nmb", [128, 64], BF16)
        nmT = sb("nmT", [128, 1024], BF16)
        ksum = sb("ksum", [128, 8], F32)
        kmh = sb("kmh", [128, 8], BF16)
        kml = sb("kml", [128, 8], BF16)
        tmg = sb("tmg", [128, 512], BF16)
        um = sb("um", [128, 512], BF16)
        rl = sb("rl", [128, 512], F32)
        B = [st.enter_context(nc.psum_tensor(f"B{i}", [128, 512], F32)) for i in range(8)]

        Bb = [b[:].bitcast(BF16) for b in B]
        PTf = PT[:].bitcast(F32)
        VTf = vtok[:].bitcast(F32)
        hTf = hT[:].bitcast(F32)
        GTf = GT[:].bitcast(F32)

        ident = cb[:, CB_IDENT:CB_IDENT + 128]
        P = Prog(nc)
        rot = [0]

        def nextA():
            i = rot[0] % 3
            rot[0] += 1
            return i

        def wrows(dten, L):
            return dten.ap()[L].rearrange("(c p) n -> p c n", p=128)

        def load_w(pieces, skeys, stream):
            def f(e):
                r = []
                for (dten, L, c0, n, slot_col, width) in pieces:
                    src = wrows(dten, L)
                    for half in range(2):
                        r.append(e.dma_start(out=sb_ap(wsl, slot_col + half * 4 * width, [[width, 4], [1, n]]),
                                             in_=src[:, half * 4:(half + 1) * 4, c0:c0 + n]))
                return r
            P.dma("pool", f, stream, 2 * len(pieces), w=skeys)

        P.dma("sp", lambda e: [e.dma_start(out=cf[:], in_=cf_d.ap())], "cf", 1, w=["cf"])
        P.dma("sp", lambda e: [e.dma_start(out=GTf[:, 0:NCB], in_=cb_d.ap())], "cbs", 1, w=K("GT", 0, 16))
        P.op("dve", lambda e: e.tensor_copy(cb[:], GTf[:, 0:NCB]), r=K("GT", 0, 16), w=["cb"])
        P.op("pool", lambda e: e.memset(nmT[:], 0.0), w=["nmT"])
        for g in range(4):
            def f(e, g=g):
                return [e.dma_start(out=xres[:, (4 * g + i) * D:(4 * g + i + 1) * D],
                                    in_=x_d.ap()[(4 * g + i) * 128:(4 * g + i + 1) * 128, :]) for i in range(4)]
            P.dma("sp", f, f"xin{g}", 4, w=K("xres", 4 * g, 4 * g + 4))

        def rms_tile(tt):
            for hf in range(2):
                P.op("dve", lambda e, hf=hf: e.bn_stats(
                    st12[:, tt * 12 + hf * 6:tt * 12 + hf * 6 + 6],
                    xres[:, tt * D + hf * 512:tt * D + hf * 512 + 512]), r=[("xres", tt)], w=[("st12", tt)])
            P.op("dve", lambda e: e.bn_aggr(mv[:, tt * 2:tt * 2 + 2], st12[:, tt * 12:tt * 12 + 12]),
                 r=[("st12", tt)], w=[("mv", tt)])
            P.op("dve", lambda e: e.tensor_tensor(ms16[:, tt:tt + 1], mv[:, tt * 2:tt * 2 + 1], mv[:, tt * 2:tt * 2 + 1], ALU.mult),
                 r=[("mv", tt)], w=[("ms16", tt)])
            P.op("dve", lambda e: e.scalar_tensor_tensor(ms16[:, tt:tt + 1], ms16[:, tt:tt + 1], EPS, mv[:, tt * 2 + 1:tt * 2 + 2], ALU.add, ALU.add),
                 r=[("mv", tt), ("ms16", tt)], w=[("ms16", tt)])
            P.op("pool", lambda e: e.tensor_tensor(rstd16[:, tt:tt + 1], ms16[:, tt:tt + 1], cf[:, CF_PW:CF_PW + 1], ALU.pow),
                 r=[("ms16", tt), "cf"], w=[("rstd16", tt)])

        def norm_front(L, tt):
            rms_tile(tt)
            b = tt % 2
            hb = PT[:, b * 1024:(b + 1) * 1024]
            P.op("act", lambda e: e.activation(hb, xres[:, tt * D:(tt + 1) * D], AF.Copy, scale=rstd16[:, tt:tt + 1]),
                 r=[("xres", tt), ("rstd16", tt)], w=[("hb", b)])

        def norm_back(L, tt):
            b = tt % 2
            hb = PT[:, b * 1024:(b + 1) * 1024]
            bk = 6 + b

            def tr(e):
                for c in range(8):
                    ins = e.transpose(Bb[bk][:, c * 128:(c + 1) * 128], hb[:, c * 128:(c + 1) * 128], ident)
                return ins
            P.op("pe", tr, r=[("hb", b), "cb"], w=[("B", bk)])
            def ev(e):
                for c in range(8):
                    ins = e.activation(hT[:, c * S + tt * 128:c * S + (tt + 1) * 128], Bb[bk][:, c * 128:(c + 1) * 128], AF.Copy,
                                       scale=cf[:, CF_LNW + L * 8 + c:CF_LNW + L * 8 + c + 1])
                return ins
            P.op("act", ev, r=[("B", bk), "cf"], w=[("hT", tt)])

        def phaseN(L):
            P.retire(K("PTm", 0, 4) + ["tg0", "tg1"], K("hb", 0, 2))
            for tt in range(NT + 2):
                if tt >= 2:
                    norm_back(L, tt - 2)
                if tt < NT:
                    norm_front(L, tt)

        def loadRA(L, h):
            load_w([(win_d, L, C_RK + 128 * h, 128, 0, 512), (win_d, L, C_RV + 256 * h, 256, 128, 512),
                    (win_d, L, C_RQ + 128 * h, 128, 384, 512)], K("wsl", 0, 2), "wA")
            P.dma("sp", lambda e: [e.dma_start(out=qksc[:], in_=qk_d.ap()[h])], "qksc", 1, w=["qksc"])

        def loadRB(L, h):
            sB = 2 + h % 2
            load_w([(win_d, L, C_RG + 256 * h, 256, sB * 2048, 256)], [("wsl", sB)], f"wB{sB}")

        def loadR(L, h):
            loadRA(L, h)
            loadRB(L, h)

        def vT_ap(j, lo, n):
            return yT[:, j * S + lo:j * S + lo + n]

        def uT_ap(j, lo, n):
            return yT[:, 4096 + j * S + lo:4096 + j * S + lo + n]

        def kvt_ap(c, lo, n):
            return yT[:, 8192 + c * 384 + lo:8192 + c * 384 + lo + n]

        def rtg_ap(i):
            return yT[:, 14336 + i * 512:14336 + (i + 1) * 512]

        def insb_ap(i):
            return yT[:, 15360 + i * 128:15360 + (i + 1) * 128]

        def on_ap(i):
            return yT[:, 15616 + i * 256:15616 + (i + 1) * 256]

        def sbf_ap(c):
            return vtok[:, c * 256:(c + 1) * 256] if c < 8 else PT[:, (c - 8) * 256:(c - 7) * 256]
        R_S = rl[:, 0:256]
        RKEYS_Y = (K("vT", 0, 16) + K("uT", 0, 16) + K("kvt", 0, 16) + ["rtg0", "rtg1"] + K("insb", 0, 2) + K("on", 0, 3))

        def phaseRall(L):
            P.retire(K("hb", 0, 2) + K("PTm", 0, 4) + ["tg0", "tg1"], K("sbf", 8, 15))
            P.retire(K("vtokm", 0, 16), K("sbf", 0, 8))
            P.retire(K("yT", 0, 16), RKEYS_Y)
            P.retire(["rl"], K("Sst", 0, 2))
            pj = [0]
            rcnt = [0]

            def proj_tile(h, T, kind):
                sB = 2 + h % 2
                gnw0 = CF_GNW + L * 8 + 2 * h
                if kind == "q":
                    wfn, wkeys = (lambda cc: cc * 512 + 384), K("wsl", 0, 2)
                elif kind == "k":
                    wfn, wkeys = (lambda cc: cc * 512), K("wsl", 0, 2)
                elif kind in ("v0", "v1"):
                    j = int(kind[1])
                    wfn, wkeys = (lambda cc: cc * 512 + 128 + j * 128), K("wsl", 0, 2)
                else:
                    j = int(kind[1])
                    wfn, wkeys = (lambda cc: sB * 2048 + cc * 256 + j * 128), [("wsl", sB)]
                a = pj[0] % 3
                pj[0] += 1

                def mm(e):
                    for cc in range(8):
                        w0 = wfn(cc)
                        ins = e.matmul(B[a][:, 0:512], wsl[:, w0:w0 + 128],
                                       hT[:, cc * S + T * 512:cc * S + T * 512 + 512], start=(cc == 0), stop=(cc == 7))
                    return ins
                P.op("pe", mm, r=wkeys + K("hT", 4 * T, 4 * T + 4), w=[("B", a)])
                if kind in ("q", "k"):
                    which = 0 if kind == "q" else 1
                    dst = qT if which == 0 else kT
                    dkey = "qT" if which == 0 else "kT"
                    P.op("dve", lambda e: e.tensor_tensor(
                        sb_ap(dst, T * 512, [[128, 4], [1, 128]]),
                        B[a][:, 0:512].rearrange("p (c n) -> p c n", c=4),
                        sb_ap(qksc, which * 128, [[0, 4], [1, 128]]), ALU.mult),
                        r=[("B", a), "qksc"], w=K(dkey, 4 * T, 4 * T + 4))
                elif kind in ("v0", "v1"):
                    P.op("act", lambda e: e.activation(vT_ap(j, T * 512, 512), B[a][:, 0:512], AF.Copy),
                         r=[("B", a)], w=K("vT", 4 * T, 4 * T + 4))
                else:
                    ri = rcnt[0] % 2
                    rcnt[0] += 1
                    P.op("act", lambda e: e.activation(rtg_ap(ri), B[a][:, 0:512], AF.Tanh, scale=0.5),
                         r=[("B", a)], w=[f"rtg{ri}"])
                    P.op("dve", lambda e: e.scalar_tensor_tensor(uT_ap(j, T * 512, 512), rtg_ap(ri), 1.0, B[a][:, 0:512], ALU.add, ALU.mult),
                         r=[("B", a), f"rtg{ri}"], w=K("uT", 4 * T, 4 * T + 4))
                    P.op("act", lambda e: e.activation(uT_ap(j, T * 512, 512), uT_ap(j, T * 512, 512), AF.Copy,
                                                        scale=cf[:, gnw0 + j:gnw0 + j + 1]),
                         r=K("uT", 4 * T, 4 * T + 4) + ["cf"], w=K("uT", 4 * T, 4 * T + 4))

            def st1(h, pr):
                def trkv(e):
                    for q in range(2):
                        c = 2 * pr + q
                        e.transpose(Bb[3][:, q * 384:q * 384 + 128], kT[:, c * 128:(c + 1) * 128], ident)
                        e.transpose(Bb[3][:, q * 384 + 128:q * 384 + 256], vT_ap(0, c * 128, 128), ident)
                        ins = e.transpose(Bb[3][:, q * 384 + 256:q * 384 + 384], vT_ap(1, c * 128, 128), ident)
                    return ins
                P.op("pe", trkv, r=K("kT", 2 * pr, 2 * pr + 2) + K("vT", 2 * pr, 2 * pr + 2) + ["cb"], w=[("B", 3)])
                P.op("act", lambda e: e.activation(kvt_ap(2 * pr, 0, 768), Bb[3][:, 0:768], AF.Copy),
                     r=[("B", 3)], w=K("kvt", 2 * pr, 2 * pr + 2))

            def st2A1(h2, c2, hA, cA):
                do2 = c2 is not None and c2 < NT - 1
                doA = cA is not None
                if not (do2 or doA):
                    return
                r = []
                if do2:
                    r += [("kvt", c2)]
                if doA:
                    r += [("kT", cA), ("qT", cA)]

                def mm(e):
                    ins = None
                    if do2:
                        ins = e.matmul(B[4][:, 0:256], kvt_ap(c2, 0, 128), kvt_ap(c2, 128, 256), start=True, stop=True)
                    if doA:
                        tok = slice(cA * 128, (cA + 1) * 128)
                        ins = e.matmul(B[4][:, 256:384], kT[:, tok], qT[:, tok], start=True, stop=True)
                    return ins
                P.op("pe", mm, r=r, w=[("B", 4)])
                if do2:
                    g2 = GAMMA[h2]
                    Sc = rl[:, (c2 % 2) * 256:(c2 % 2) * 256 + 256]
                    Sp = rl[:, ((c2 + 1) % 2) * 256:((c2 + 1) % 2) * 256 + 256]
                    if c2 == 0:
                        P.op("dve", lambda e: e.tensor_copy(Sc, B[4][:, 0:256]), r=[("B", 4)], w=[("Sst", c2 % 2)])
                    else:
                        P.op("dve", lambda e: e.scalar_tensor_tensor(Sc, Sp, float(g2 ** 128.0), B[4][:, 0:256], ALU.mult, ALU.add),
                             r=[("B", 4), ("Sst", (c2 + 1) % 2)], w=[("Sst", c2 % 2)])
                if doA:
                    gA = GAMMA[hA]
                    i2 = cA % 2
                    P.op("dve", lambda e: e.scalar_tensor_tensor(insb_ap(i2), B[4][:, 256:384], float(gA ** -128.0), cb[:, CB_TRI01:CB_TRI01 + 128], ALU.mult, ALU.mult),
                         r=[("B", 4), "cb"], w=[("insb", i2)])
                if do2:
                    P.op("dve", lambda e: e.tensor_copy(sbf_ap(c2), Sc), r=[("Sst", c2 % 2)], w=[("sbf", c2)])

            def stA2(h, c):
                tok = slice(c * 128, (c + 1) * 128)
                i2 = c % 2
                ob = 5 + c % 2

                def mmo(e):
                    if c > 0:
                        e.matmul(B[ob][:, 0:256], qT[:, tok], sbf_ap(c - 1), start=True, stop=False)
                    return e.matmul(B[ob][:, 0:256], insb_ap(i2), kvt_ap(c, 128, 256), start=(c == 0), stop=True)
                P.op("pe", mmo, r=[("qT", c), ("insb", i2), ("kvt", c)] + ([("sbf", c - 1)] if c > 0 else []), w=[("B", ob)])

            def stB1(h, c):
                ob = 5 + c % 2
                g3 = c % 3
                g0 = g3 * 16
                P.op("dve", lambda e: e.bn_stats(gsm[:, g0:g0 + 6], B[ob][:, 0:256]), r=[("B", ob)], w=[("gsm", g3)])
                P.op("dve", lambda e: e.bn_aggr(gsm[:, g0 + 6:g0 + 8], gsm[:, g0:g0 + 6]), r=[("gsm", g3)], w=[("gsm", g3)])
                P.op("act", lambda e: e.activation(ocp[:, g3 * 256:(g3 + 1) * 256], B[ob][:, 0:256], AF.Copy),
                     r=[("B", ob)], w=[("ocp", g3)])
                P.op("pool", lambda e: e.tensor_scalar(gsm[:, g0 + 8:g0 + 9], gsm[:, g0 + 7:g0 + 8], EPS, 4.0, ALU.add, ALU.mult),
                     r=[("gsm", g3)], w=[("gsm", g3)])
                P.op("pool", lambda e: e.tensor_tensor(gsm[:, g0 + 9:g0 + 10], gsm[:, g0 + 8:g0 + 9], cf[:, CF_PW:CF_PW + 1], ALU.pow),
                     r=[("gsm", g3), "cf"], w=[("gsm", g3)])
                P.op("pool", lambda e: e.tensor_scalar(gsm[:, g0 + 10:g0 + 11], gsm[:, g0 + 6:g0 + 7], -1.0, gsm[:, g0 + 9:g0 + 10], ALU.mult, ALU.mult),
                     r=[("gsm", g3)], w=[("gsm", g3)])

            def stB2a(h, c):
                ob = 5 + c % 2
                g3 = c % 3
                g0 = g3 * 16
                P.op("act", lambda e: e.activation(on_ap(g3), ocp[:, g3 * 256:(g3 + 1) * 256], AF.Identity,
                                                    bias=gsm[:, g0 + 10:g0 + 11], scale=gsm[:, g0 + 9:g0 + 10]),
                     r=[("ocp", g3), ("gsm", g3)], w=[("on", g3)])

            def stB2b(h, c):
                g3 = c % 3

                def trr(e):
                    e.transpose(Bb[7][:, 0:128], on_ap(g3)[:, 0:128], ident)
                    return e.transpose(Bb[7][:, 128:256], on_ap(g3)[:, 128:256], ident)
                P.op("pe", trr, r=[("on", g3), "cb"], w=[("B", 7)])

            def stB3(h, c):
                P.op("dve", lambda e: e.tensor_tensor(
                    sb_ap(GT, 2 * h * S + c * 128, [[S, 2], [1, 128]]),
                    Bb[7][:, 0:256].rearrange("p (j n) -> p j n", j=2),
                    sb_ap(yT, 4096 + c * 128, [[S, 2], [1, 128]]), ALU.mult),
                    r=[("B", 7), ("uT", c)], w=[("GT", c)])

            NG = RET_H * NT
            PROJ_ORDER = [("q", "k"), ("v0", "v1"), ("r0",), ("r1",)]
            stages = [(9, stA2), (10, stB1), (11, stB2a), (12, stB2b), (13, stB3)]
            for gstep in range(NG + 14):
                for (dly, fn) in reversed(stages):
                    gc = gstep - dly
                    if 0 <= gc < NG:
                        fn(gc // NT, gc % NT)
                g2, gA = gstep - 6, gstep - 8
                st2A1(g2 // NT if 0 <= g2 < NG else None, g2 % NT if 0 <= g2 < NG else None,
                      gA // NT if 0 <= gA < NG else None, gA % NT if 0 <= gA < NG else None)
                if gstep % 2 == 0:
                    gc = gstep - 4
                    if 0 <= gc < NG:
                        st1(gc // NT, (gc % NT) // 2)
                if gstep < NG:
                    h, cc_ = gstep // NT, gstep % NT
                    for kind in PROJ_ORDER[cc_ % 4]:
                        proj_tile(h, cc_ // 4, kind)
                    if cc_ == 0 and h + 1 < RET_H:
                        loadRB(L, h + 1)
                    if cc_ == 13 and h + 1 < RET_H:
                        loadRA(L, h + 1)

        def loadXO(L, eb, wd, gcol):
            p = eb % 2
            load_w([(wd, L, eb * 256, 256, (2 * p) * 2048, 256)], [("wsl", 2 * p)], f"wX{2 * p}")
            load_w([(win_d, L, gcol + eb * 256, 256, (2 * p + 1) * 2048, 256)], [("wsl", 2 * p + 1)], f"wX{2 * p + 1}")

        def phaseXO(L, eb, first):
            p = eb % 2
            if eb == 0:
                if first:
                    P.retire(K("sbf", 8, 15), ["tg0", "tg1"])
                    P.retire(RKEYS_Y, K("yT", 0, 16))
                else:
                    P.retire(K("PTm", 0, 4), ["tg0", "tg1"])
            for ec in range(2):
                echunk = 2 * eb + ec
                for T in range(4):
                    aa = nextA()

                    def mma(e, aa=aa, T=T, ec=ec):
                        for vc_ in range(8):
                            ins = e.matmul(B[aa][:, 0:512], wsl[:, (2 * p) * 2048 + vc_ * 256 + ec * 128:(2 * p) * 2048 + vc_ * 256 + ec * 128 + 128],
                                           GT[:, vc_ * S + T * 512:vc_ * S + T * 512 + 512], start=(vc_ == 0), stop=(vc_ == 7))
                        return ins
                    P.op("pe", mma, r=[("wsl", 2 * p)] + K("GT", 4 * T, 4 * T + 4), w=[("B", aa)])
                    ag = nextA()

                    def mmg(e, ag=ag, T=T, ec=ec):
                        for cc in range(8):
                            ins = e.matmul(B[ag][:, 0:512], wsl[:, (2 * p + 1) * 2048 + cc * 256 + ec * 128:(2 * p + 1) * 2048 + cc * 256 + ec * 128 + 128],
                                           hT[:, cc * S + T * 512:cc * S + T * 512 + 512], start=(cc == 0), stop=(cc == 7))
                        return ins
                    P.op("pe", mmg, r=[("wsl", 2 * p + 1)] + K("hT", 4 * T, 4 * T + 4), w=[("B", ag)])
                    tb = (ec * 4 + T) % 2
                    tg = PT[:, tb * 512:(tb + 1) * 512]
                    tkey = f"tg{tb}"
                    P.op("act", lambda e, ag=ag, tg=tg: e.activation(tg, B[ag][:, 0:512], AF.Tanh, scale=0.5),
                         r=[("B", ag)], w=[tkey])
                    ydst = yT[:, echunk * S + T * 512:echunk * S + T * 512 + 512]
                    if first:
                        P.op("dve", lambda e, aa=aa, tg=tg, ydst=ydst: e.scalar_tensor_tensor(ydst, tg, 1.0, B[aa][:, 0:512], ALU.add, ALU.mult),
                             r=[("B", aa), tkey], w=K("yT", 4 * T, 4 * T + 4))
                    else:
                        P.op("dve", lambda e, aa=aa, tg=tg: e.scalar_tensor_tensor(rl[:], tg, 1.0, B[aa][:, 0:512], ALU.add, ALU.mult),
                             r=[("B", aa), tkey], w=["rl"])
                        P.op("dve", lambda e, ydst=ydst: e.tensor_tensor(ydst, rl[:], ydst, ALU.add),
                             r=["rl"] + K("yT", 4 * T, 4 * T + 4), w=K("yT", 4 * T, 4 * T + 4))

        def loadM(L, h):
            p = h % 2
            load_w([(win_d, L, C_MQ + 128 * h, 128, (2 * p) * 2048, 256), (win_d, L, C_MK + 128 * h, 128, (2 * p) * 2048 + 128, 256)],
                   [("wsl", 2 * p)], f"wX{2 * p}")
            load_w([(win_d, L, C_MV + 128 * h, 128, (2 * p + 1) * 2048, 256), (win_d, L, C_MG + 128 * h, 128, (2 * p + 1) * 2048 + 128, 256)],
                   [("wsl", 2 * p + 1)], f"wX{2 * p + 1}")

        def phaseM(L, h):
            p = h % 2
            sQK = (2 * p) * 2048
            sVG = (2 * p + 1) * 2048
            if h == 0:
                P.retire(["tg0", "tg1"], K("PTm", 0, 4))
                P.retire(K("sbf", 0, 8), K("vtokm", 0, 16))
                P.retire(K("Sst", 0, 2), ["rl"])
            for T in range(4):
                a = nextA()

                def mmq(e, a=a, T=T):
                    for cc in range(8):
                        ins = e.matmul(B[a][:, 0:512], wsl[:, sQK + cc * 256:sQK + cc * 256 + 128],
                                       hT[:, cc * S + T * 512:cc * S + T * 512 + 512], start=(cc == 0), stop=(cc == 7))
                    return ins
                P.op("pe", mmq, r=[("wsl", 2 * p)] + K("hT", 4 * T, 4 * T + 4), w=[("B", a)])
                P.op("act", lambda e, a=a, T=T: e.activation(qT[:, T * 512:(T + 1) * 512], B[a][:, 0:512], AF.Copy),
                     r=[("B", a)], w=K("qT", 4 * T, 4 * T + 4))
                a2 = nextA()

                def mmk(e, a2=a2, T=T):
                    for cc in range(8):
                        ins = e.matmul(B[a2][:, 0:512], wsl[:, sQK + cc * 256 + 128:sQK + cc * 256 + 256],
                                       hT[:, cc * S + T * 512:cc * S + T * 512 + 512], start=(cc == 0), stop=(cc == 7))
                    return ins
                P.op("pe", mmk, r=[("wsl", 2 * p)] + K("hT", 4 * T, 4 * T + 4), w=[("B", a2)])
                P.op("act", lambda e, a2=a2, T=T: e.activation(kT[:, T * 512:(T + 1) * 512], B[a2][:, 0:512], AF.Copy),
                     r=[("B", a2)], w=K("kT", 4 * T, 4 * T + 4))
                P.op("dve", lambda e, a2=a2, T=T: e.tensor_reduce(ksum[:, 2 * T:2 * T + 2], B[a2][:, 0:512].rearrange("p (b n) -> p b n", b=2), AX.X, ALU.add),
                     r=[("B", a2)], w=["ksum"])
            vbank = {}

            def v_mm(T):
                a = nextA()
                vbank[T] = a

                def mmv(e):
                    for cc in range(8):
                        ins = e.matmul(B[a][:, 0:512], wsl[:, sVG + cc * 256:sVG + cc * 256 + 128],
                                       hT[:, cc * S + T * 512:cc * S + T * 512 + 512], start=(cc == 0), stop=(cc == 7))
                    return ins
                P.op("pe", mmv, r=[("wsl", 2 * p + 1)] + K("hT", 4 * T, 4 * T + 4), w=[("B", a)])

            def v_copy(T):
                a = vbank[T]
                P.op("act", lambda e: e.activation(tmg[:], B[a][:, 0:512], AF.Copy), r=[("B", a)], w=["tmg"])

            def v_tr(T):
                bk = 6 if T % 2 == 0 else 7

                def trv(e):
                    for i in range(4):
                        ins = e.transpose(Bb[bk][:, i * 128:(i + 1) * 128], tmg[:, i * 128:(i + 1) * 128], ident)
                    return ins
                P.op("pe", trv, r=["tmg", "cb"], w=[("B", bk)])
                P.op("dve", lambda e: e.tensor_copy(vtok[:, T * 512:(T + 1) * 512], Bb[bk][:, 0:512]),
                     r=[("B", bk)], w=K("vtokm", 4 * T, 4 * T + 4))

            v_mm(0)
            v_copy(0)
            for T in range(4):
                if T + 1 < 4:
                    v_mm(T + 1)
                v_tr(T)
                if T + 1 < 4:
                    v_copy(T + 1)
            def gate1():
                P.op("dve", lambda e: e.tensor_copy(kmh[:], ksum[:]), r=["ksum"], w=["kmh"])
                P.op("dve", lambda e: e.tensor_tensor(kml[:], ksum[:], kmh[:], ALU.subtract), r=["ksum", "kmh"], w=["kml"])

                def mmgate(e):
                    for i in range(8):
                        tt = 8 + i
                        e.matmul(B[7][:, i * 8:(i + 1) * 8], qT[:, tt * 128:(tt + 1) * 128], kmh[:], start=True, stop=False)
                        ins = e.matmul(B[7][:, i * 8:(i + 1) * 8], qT[:, tt * 128:(tt + 1) * 128], kml[:], start=False, stop=True)
                    return ins
                P.op("pe", mmgate, r=K("qT", 8, 16) + ["kmh", "kml"], w=[("B", 7)])
                P.op("dve", lambda e: e.tensor_tensor(gm[:], B[7][:, 0:64], cf[:, CF_NEGM:CF_NEGM + 64], ALU.add),
                     r=[("B", 7), "cf"], w=["gm"])
                for i in range(8):
                    P.op("dve", lambda e, i=i: e.max(m8[:, i * 8:(i + 1) * 8], gm[:, i * 8:(i + 1) * 8]), r=["gm"], w=["m8"])
                P.op("dve", lambda e: e.tensor_tensor(sb_ap(nmb, 0, [[8, 8], [1, 8]]), sb_ap(gm, 0, [[8, 8], [1, 8]]),
                                                       sb_ap(m8, 2, [[8, 8], [0, 8]]), ALU.is_lt),
                     r=["gm", "m8"], w=["nmb"])


            def gate2():
                def trm(e):
                    for i in range(8):
                        ins = e.transpose(Bb[7][0:8, i * 128:(i + 1) * 128], nmb[:, i * 8:(i + 1) * 8], ident)
                    return ins
                P.op("pe", trm, r=["nmb", "cb"], w=[("B", 7)])
                P.op("dve", lambda e: e.tensor_copy(nmT[0:8, :], Bb[7][0:8, 0:1024]), r=[("B", 7)], w=["nmT"])


            def t_mm(T):
                def mmmg(e):
                    for cc in range(8):
                        ins = e.matmul(B[7][:, 0:512], wsl[:, sVG + cc * 256 + 128:sVG + cc * 256 + 256],
                                       hT[:, cc * S + T * 512:cc * S + T * 512 + 512], start=(cc == 0), stop=(cc == 7))
                    return ins
                P.op("pe", mmmg, r=[("wsl", 2 * p + 1)] + K("hT", 4 * T, 4 * T + 4), w=[("B", 7)])

            def t_act(T):
                P.op("act", lambda e: e.activation(tmg[:], B[7][:, 0:512], AF.Tanh, scale=0.5), r=[("B", 7)], w=["tmg"])
                P.op("dve", lambda e: e.scalar_tensor_tensor(um[:], tmg[:], 1.0, B[7][:, 0:512], ALU.add, ALU.mult),
                     r=[("B", 7), "tmg"], w=["um"])

            def rec_qk(T, stl):
                i = stl - 4 * T
                c0 = 128 * i if i >= 0 else 0
                j = stl // 2
                a = nextA()
                need_mask = (T >= 2) and (j <= 2 * T)
                cm0 = 0 if j < 2 * T else 256

                def mms(e):
                    last = not (i >= 0 or need_mask)
                    ins = e.matmul(B[a][:, c0:512], kT[:, stl * 128:(stl + 1) * 128], qT[:, T * 512 + c0:T * 512 + 512],
                                   start=True, stop=last)
                    if i >= 0:
                        ins = e.matmul(B[a][:, c0:c0 + 128], ident, cb[:, CB_NEGTRI:CB_NEGTRI + 128],
                                       start=False, stop=not need_mask)
                    if need_mask:
                        ins = e.matmul(B[a][:, cm0:512], cb[:, CB_NEGSEL + j * 128:CB_NEGSEL + (j + 1) * 128],
                                       nmT[:, T * 512 - 1024 + cm0:T * 512 - 1024 + 512], start=False, stop=True)
                    return ins
                P.op("pe", mms, r=[("kT", stl)] + K("qT", 4 * T, 4 * T + 4) + ["cb", "nmT"], w=[("B", a)])
                return a

            def rec_exp(T, stl, a):
                pb = stl % 4
                for (lo, hi, bi) in BIAS_PLAN[(h, T, stl)]:
                    P.op("act", lambda e, lo=lo, hi=hi, bi=bi: e.activation(
                        PT[:, pb * 512 + lo:pb * 512 + hi], B[a][:, lo:hi], AF.Exp,
                        bias=cf[:, CF_ABIAS + bi:CF_ABIAS + bi + 1], scale=SCALE),
                        r=[("B", a), "cf"], w=[("PTm", pb)])

            def rec_pv(T, stl):
                i = stl - 4 * T
                c0 = 128 * i if i >= 0 else 0
                pb = stl % 4
                ob = 3 + T % 2
                lb = 5 + T % 2
                nst = 4 * (T + 1)

                def mmpv(e):
                    e.matmul(B[ob][:, c0:512], vtok[:, stl * 128:(stl + 1) * 128], PT[:, pb * 512 + c0:pb * 512 + 512],
                             start=(stl == 0), stop=(stl == nst - 1))
                    return e.matmul(B[lb][:, c0:512], cb[:, CB_ONES2:CB_ONES2 + 128], PT[:, pb * 512 + c0:pb * 512 + 512],
                                    start=(stl == 0), stop=(stl == nst - 1))
                P.op("pe", mmpv, r=[("PTm", pb), ("vtokm", stl), "cb"], w=[("B", ob), ("B", lb)])

            def epilogue(T):
                ob = 3 + T % 2
                lb = 5 + T % 2
                P.op("dve", lambda e: e.reciprocal(rl[:], B[lb][:, 0:512]), r=[("B", lb)], w=["rl"])
                P.op("dve", lambda e: e.tensor_tensor(rl[:], rl[:], um[:], ALU.mult), r=["rl", "um"], w=["rl"])
                P.op("dve", lambda e: e.tensor_tensor(GT[:, h * S + T * 512:h * S + T * 512 + 512], B[ob][:, 0:512], rl[:], ALU.mult),
                     r=[("B", ob), "rl"], w=K("GT", 4 * T, 4 * T + 4))

            tiles = [(T, stl) for T in range(4) for stl in range(4 * (T + 1))]
            banks = {}
            for i in range(2):
                banks[i] = rec_qk(*tiles[i])
            t_mm(0)
            for i, (T, stl) in enumerate(tiles):
                if stl == 0:
                    t_act(T)
                    if T == 1:
                        gate1()
                if T == 1 and stl == 6:
                    gate2()
                if stl == 4 * (T + 1) - 2 and T + 1 < 4:
                    t_mm(T + 1)
                rec_exp(T, stl, banks[i])
                if i + 2 < len(tiles):
                    banks[i + 2] = rec_qk(*tiles[i + 2])
                rec_pv(T, stl)
                if stl == 4 * (T + 1) - 1:
                    epilogue(T)

        def loadO(L):
            for s in range(4):
                load_w([(wout_d, L, s * 256, 256, s * 2048, 256)], [("wsl", s)], f"wX{s}")

        def phaseO(L):
            last = (L == n_layers - 1)
            if not last:
                P.retire(K("PTm", 0, 4) + ["tg0", "tg1"], K("hb", 0, 2))
            else:
                P.dma("sp", lambda e: [e.dma_start(out=hTf[:, 0:D], in_=fnw_d.ap())], "fnw", 1, r=[], w=K("hT", 0, 16))
            for tt in range(NT):
                for half in range(2):
                    a = nextA()

                    def mmo(e, a=a, tt=tt, half=half):
                        for q in range(2):
                            s = 2 * half + q
                            for ec in range(8):
                                ins = e.matmul(B[a][:, q * 256:(q + 1) * 256], yT[:, ec * S + tt * 128:ec * S + tt * 128 + 128],
                                               wsl[:, s * 2048 + ec * 256:s * 2048 + ec * 256 + 256], start=(ec == 0), stop=(ec == 7))
                        return ins
                    P.op("pe", mmo, r=K("wsl", 2 * half, 2 * half + 2) + [("yT", tt)], w=[("B", a)])
                    xs = xres[:, tt * D + half * 512:tt * D + half * 512 + 512]
                    P.op("dve", lambda e, a=a, xs=xs: e.scalar_tensor_tensor(xs, B[a][:, 0:512], 0.5, xs, ALU.mult, ALU.add),
                         r=[("B", a), ("xres", tt)], w=[("xres", tt)])
                if not last:
                    if tt >= 2:
                        norm_back(L + 1, tt - 2)
                    norm_front(L + 1, tt)
                else:
                    final_tile(tt)
            if not last:
                norm_back(L + 1, NT - 2)
                norm_back(L + 1, NT - 1)
            if last:
                P.final_waits = [("sp", "out0"), ("sp", "out1")]

        def final_tile(tt):
            b = tt % 2
            stg = hTf[:, D + b * D:D + (b + 1) * D]
            if final_norm:
                rms_tile(tt)
                P.op("dve", lambda e: e.scalar_tensor_tensor(stg, xres[:, tt * D:(tt + 1) * D], rstd16[:, tt:tt + 1], hTf[:, 0:D], ALU.mult, ALU.mult),
                     r=[("xres", tt), ("rstd16", tt)] + K("hT", 0, 16), w=[("stg", b)])
                P.dma("sp", lambda e: [e.dma_start(out=out_d.ap()[tt * 128:(tt + 1) * 128, :], in_=stg)],
                      f"out{b}", 1, r=[("stg", b)])
            else:
                P.dma("sp", lambda e: [e.dma_start(out=out_d.ap()[tt * 128:(tt + 1) * 128, :], in_=xres[:, tt * D:(tt + 1) * D])],
                      f"out{b}", 1, r=[("xres", tt)])

        units = []
        for L in range(n_layers):
            if L == 0:
                units.append((set(), None, lambda L=L: phaseN(L), L))
            units.append(({0, 1, 2, 3}, (lambda L=L: loadR(L, 0)), (lambda L=L: phaseRall(L)), L))
            for eb in range(4):
                units.append(({2 * (eb % 2), 2 * (eb % 2) + 1}, (lambda L=L, eb=eb: loadXO(L, eb, wro_d, C_GR)),
                              (lambda L=L, eb=eb: phaseXO(L, eb, True)), L))
            for h in range(MOBA_H):
                units.append(({2 * (h % 2), 2 * (h % 2) + 1}, (lambda L=L, h=h: loadM(L, h)), (lambda L=L, h=h: phaseM(L, h)), L))
            for eb in range(4):
                units.append(({2 * (eb % 2), 2 * (eb % 2) + 1}, (lambda L=L, eb=eb: loadXO(L, eb, wmo_d, C_GM)),
                              (lambda L=L, eb=eb: phaseXO(L, eb, False)), L))
            units.append(({0, 1, 2, 3}, (lambda L=L: loadO(L)), (lambda L=L: phaseO(L)), L))
        loaded = [False] * len(units)
        for i, (slots, ld, comp, L) in enumerate(units):
            P.epoch = L
            if ld is not None and not loaded[i]:
                ld()
                loaded[i] = True
            busy = set(slots)
            for k in range(i + 1, min(i + 3, len(units))):
                s2, ld2, _, _ = units[k]
                if ld2 is None:
                    continue
                if loaded[k]:
                    busy |= s2
                    continue
                if s2 & busy:
                    break
                ld2()
                loaded[k] = True
                busy |= s2
            comp()
        P.emit()
    return nc


_CACHE = {}


def _get_prog(n_layers, final_norm):
    key = (n_layers, final_norm)
    if key not in _CACHE:
        _CACHE[key] = build(n_layers, final_norm)
    return _CACHE[key]


def kernel(x, ln_w, w_in, ret_gn_w, w_ret_o, w_moba_o, w_out, final_norm_w):
    x = np.ascontiguousarray(np.asarray(x, dtype=np.float32))
    ln_w = np.asarray(ln_w, dtype=np.float32)
    ret_gn_w = np.asarray(ret_gn_w, dtype=np.float32)
    w_in = np.ascontiguousarray(np.asarray(w_in, dtype=np.float32))
    w_ret_o = np.ascontiguousarray(np.asarray(w_ret_o, dtype=np.float32))
    w_moba_o = np.ascontiguousarray(np.asarray(w_moba_o, dtype=np.float32))
    w_out = np.ascontiguousarray(np.asarray(w_out, dtype=np.float32))
    fnw = np.ascontiguousarray(np.broadcast_to(np.asarray(final_norm_w, dtype=np.float32)[None, :], (128, D)))
    nL = w_in.shape[0]
    cf, cb, qk = host_consts(ln_w, ret_gn_w, nL, 0)
    nc = _get_prog(nL, True)
    ncores = x.shape[0]
    in_maps = [{"x": x[b], "w_in": w_in, "w_ro": w_ret_o, "w_mo": w_moba_o, "w_out": w_out,
                "cf": cf, "cb": cb, "qksc": qk, "fnw": fnw} for b in range(ncores)]
    res = run_bass_kernel_spmd(nc, in_maps, core_ids=list(range(ncores)))
    return np.stack([np.asarray(r["out"], dtype=np.float32) for r in res.results], axis=0)
```
